# Optimizing a Trainium2 kernel written in Bass

```python
import math
import jax, jax.numpy as jnp
from jax import lax
import numpy as np

D_MODEL = 1024
BATCH = 16
SEQ = 2048
DEPTH = 4

HEAD_DIM = 64
EPS = 1e-6
Q_BLOCK = 128
MOBA_HEADS = D_MODEL // 2 // HEAD_DIM
FOX_HEADS = D_MODEL // 2 // HEAD_DIM
MOBA_BLOCK = 256
MOBA_TOPK = 3
MOBA_Q_CHUNK = 16
DIFF_V_DIM = 2 * HEAD_DIM
DIFF_HEADS = D_MODEL // 2 // DIFF_V_DIM
SWA_Q_HEADS = D_MODEL // 2 // HEAD_DIM
SWA_KV_HEADS = SWA_Q_HEADS // 4
SWA_WINDOW = 128

N_AB = (DEPTH + 1) // 2
N_CD = DEPTH // 2

MOBA_W = MOBA_HEADS * HEAD_DIM
FOX_W = FOX_HEADS * HEAD_DIM
AB_SIZES = [MOBA_W, MOBA_W, MOBA_W, MOBA_W, FOX_W, FOX_W, FOX_W, FOX_W, FOX_HEADS]
AB_IN = sum(AB_SIZES)
DIFF_QK_W = DIFF_HEADS * 2 * HEAD_DIM
DIFF_W = DIFF_HEADS * DIFF_V_DIM
SWA_W = SWA_Q_HEADS * HEAD_DIM
SWA_KV_W = SWA_KV_HEADS * HEAD_DIM
CD_SIZES = [DIFF_QK_W, DIFF_QK_W, DIFF_W, DIFF_W, SWA_W, SWA_KV_W, SWA_KV_W, SWA_W]
CD_IN = sum(CD_SIZES)
MIX_W = D_MODEL

kernel_name = "hybrid_moba_fox_diff_swa_gated"


def rmsnorm(x, gain):
    xf = x.astype(jnp.float32)
    y = xf * lax.rsqrt(jnp.mean(xf * xf, axis=-1, keepdims=True) + EPS)
    return (y * gain.astype(jnp.float32)).astype(x.dtype)


def alibi_slopes(n):
    return jnp.asarray(2.0 ** (-8.0 * np.arange(1, n + 1) / n), dtype=jnp.float32)


def split_cols(t, sizes):
    idx = np.cumsum(sizes)[:-1].tolist()
    return jnp.split(t, idx, axis=-1)


def to_heads(t, n):
    b, s, _ = t.shape
    return t.reshape(b, s, n, -1).transpose(0, 2, 1, 3)


def from_heads(t):
    b, h, s, d = t.shape
    return t.transpose(0, 2, 1, 3).reshape(b, s, h * d)


def moba_attention(q, k, v, slopes):
    B, H, S, Dh = q.shape
    L = MOBA_BLOCK
    nb = -(-S // L)
    pad = nb * L - S
    kp = jnp.pad(k, ((0, 0), (0, 0), (0, pad), (0, 0)))
    vp = jnp.pad(v, ((0, 0), (0, 0), (0, pad), (0, 0)))
    kb = kp.reshape(B, H, nb, L, Dh)
    vb = vp.reshape(B, H, nb, L, Dh)
    kmean = jnp.mean(kb.astype(jnp.float32), axis=3)
    pos = jnp.arange(S)
    qblk = pos // L
    gate = jnp.einsum('bhsd,bhnd->bhsn', q.astype(jnp.float32), kmean)
    past = jnp.arange(nb)[None, :] < qblk[:, None]
    gate = jnp.where(past, gate, -jnp.inf)
    topk = min(MOBA_TOPK, nb)
    _, sel = lax.top_k(gate, topk)
    sel_valid = sel < qblk[None, None, :, None]
    scale = Dh ** -0.5
    bi = jnp.arange(B)[:, None, None, None]
    hi = jnp.arange(H)[None, :, None, None]
    C = MOBA_Q_CHUNK

    def chunk(c):
        start = c * C
        qc = lax.dynamic_slice_in_dim(q, start, C, axis=2)
        selc = lax.dynamic_slice_in_dim(sel, start, C, axis=2)
        validc = lax.dynamic_slice_in_dim(sel_valid, start, C, axis=2)
        tpos = start + jnp.arange(C)
        kg = kb[bi, hi, selc]
        vg = vb[bi, hi, selc]
        s_past = jnp.einsum('bhqd,bhqnld->bhqnl', qc, kg).astype(jnp.float32) * scale
        spos = selc[..., None] * L + jnp.arange(L)
        rel_p = (tpos[None, None, :, None, None] - spos).astype(jnp.float32)
        s_past = jnp.where(validc[..., None],
                           s_past - slopes[None, :, None, None, None] * rel_p, -jnp.inf)
        s_past = s_past.reshape(B, H, C, topk * L)
        own_start = (start // L) * L
        ko = lax.dynamic_slice_in_dim(kp, own_start, L, axis=2)
        vo = lax.dynamic_slice_in_dim(vp, own_start, L, axis=2)
        s_own = jnp.einsum('bhqd,bhld->bhql', qc, ko).astype(jnp.float32) * scale
        rel_o = tpos[:, None] - (own_start + jnp.arange(L))[None, :]
        s_own = jnp.where(rel_o >= 0,
                          s_own - slopes[None, :, None, None] * rel_o.astype(jnp.float32),
                          -jnp.inf)
        p = jax.nn.softmax(jnp.concatenate([s_past, s_own], axis=-1), axis=-1).astype(v.dtype)
        p_past = p[..., :topk * L].reshape(B, H, C, topk, L)
        p_own = p[..., topk * L:]
        return (jnp.einsum('bhqnl,bhqnld->bhqd', p_past, vg)
                + jnp.einsum('bhql,bhld->bhqd', p_own, vo))

    outs = lax.map(chunk, jnp.arange(S // C))
    return outs.transpose(1, 2, 0, 3, 4).reshape(B, H, S, Dh)


def fox_attention(q, k, v, log_f):
    B, H, S, Dh = q.shape
    c = jnp.cumsum(log_f, axis=-1)
    scale = Dh ** -0.5
    kpos = jnp.arange(S)

    def block(i):
        start = i * Q_BLOCK
        qb = lax.dynamic_slice_in_dim(q, start, Q_BLOCK, axis=2)
        cb = lax.dynamic_slice_in_dim(c, start, Q_BLOCK, axis=2)
        s = (jnp.einsum('bhqd,bhsd->bhqs', qb, k).astype(jnp.float32) * scale
             + cb[..., None] - c[:, :, None, :])
        tpos = start + jnp.arange(Q_BLOCK)
        s = jnp.where(tpos[:, None] >= kpos[None, :], s, -jnp.inf)
        p = jax.nn.softmax(s, axis=-1).astype(v.dtype)
        return jnp.einsum('bhqs,bhsd->bhqd', p, v)

    outs = lax.map(block, jnp.arange(S // Q_BLOCK))
    return outs.transpose(1, 2, 0, 3, 4).reshape(B, H, S, Dh)


def diff_attention(q, k, v, lam, slopes):
    B, H, S, _, Dh = q.shape
    scale = Dh ** -0.5
    kpos = jnp.arange(S)

    def block(i):
        start = i * Q_BLOCK
        qb = lax.dynamic_slice_in_dim(q, start, Q_BLOCK, axis=2)
        s = jnp.einsum('bhqcd,bhscd->bhcqs', qb, k).astype(jnp.float32) * scale
        rel = (start + jnp.arange(Q_BLOCK))[:, None] - kpos[None, :]
        s = jnp.where(rel >= 0,
                      s - slopes[None, :, None, None, None] * rel.astype(jnp.float32),
                      -jnp.inf)
        p = jax.nn.softmax(s, axis=-1)
        a = (p[:, :, 0] - lam * p[:, :, 1]).astype(v.dtype)
        return jnp.einsum('bhqs,bhse->bhqe', a, v)

    outs = lax.map(block, jnp.arange(S // Q_BLOCK))
    return outs.transpose(1, 2, 0, 3, 4).reshape(B, H, S, v.shape[-1])


def swa_attention(q, k, v, sinks, slopes):
    B, S, Hq, Dh = q.shape
    Hkv = k.shape[2]
    G = Hq // Hkv
    nb = S // Q_BLOCK
    qb = q.reshape(B, nb, Q_BLOCK, Hkv, G, Dh)
    kb = k.reshape(B, nb, Q_BLOCK, Hkv, Dh)
    vb = v.reshape(B, nb, Q_BLOCK, Hkv, Dh)
    pad = ((0, 0), (1, 0), (0, 0), (0, 0), (0, 0))
    kband = jnp.concatenate([jnp.pad(kb, pad)[:, :-1], kb], axis=2)
    vband = jnp.concatenate([jnp.pad(vb, pad)[:, :-1], vb], axis=2)
    s = jnp.einsum('bnqgrd,bnkgd->bngrqk', qb, kband).astype(jnp.float32) * (Dh ** -0.5)
    qi = jnp.arange(Q_BLOCK)
    ki = jnp.arange(2 * Q_BLOCK)
    rel = (Q_BLOCK + qi)[:, None] - ki[None, :]
    in_win = (rel >= 0) & (rel < SWA_WINDOW)
    has_prev = (jnp.arange(nb) > 0)[:, None, None] | (ki >= Q_BLOCK)[None, None, :]
    mask = in_win[None] & has_prev
    bias = -slopes.reshape(Hkv, G)[:, :, None, None] * rel.astype(jnp.float32)
    s = jnp.where(mask[None, :, None, None], s + bias, -jnp.inf)
    sink = jnp.broadcast_to(
        sinks.astype(jnp.float32).reshape(Hkv, G)[None, None, :, :, None, None],
        s.shape[:-1] + (1,))
    p = jax.nn.softmax(jnp.concatenate([s, sink], axis=-1), axis=-1)[..., :-1]
    out = jnp.einsum('bngrqk,bnkgd->bnqgrd', p.astype(v.dtype), vband)
    return out.reshape(B, S, Hq * Dh)


def setup_inputs(seed: int = 0) -> dict:
    key = jax.random.key(seed)
    ks = jax.random.split(key, 20)
    f32 = jnp.float32
    nrm = lambda k, shp: jax.random.normal(k, shp, f32)
    gain = lambda k, shp: 1.0 + 0.05 * nrm(k, shp)
    return {
        "x": nrm(ks[0], (BATCH, SEQ, D_MODEL)),
        "norm_gain": gain(ks[1], (DEPTH, D_MODEL)),
        "w_in_ab": nrm(ks[2], (N_AB, D_MODEL, AB_IN)) * D_MODEL ** -0.5,
        "b_forget": 3.0 + 0.3 * nrm(ks[3], (N_AB, FOX_HEADS)),
        "moba_q_gain": gain(ks[4], (N_AB, HEAD_DIM)),
        "moba_k_gain": gain(ks[5], (N_AB, HEAD_DIM)),
        "fox_q_gain": gain(ks[6], (N_AB, HEAD_DIM)),
        "fox_k_gain": gain(ks[7], (N_AB, HEAD_DIM)),
        "w_out_ab": nrm(ks[8], (N_AB, MIX_W, D_MODEL)) * MIX_W ** -0.5,
        "w_in_cd": nrm(ks[9], (N_CD, D_MODEL, CD_IN)) * D_MODEL ** -0.5,
        "diff_q_gain": gain(ks[10], (N_CD, HEAD_DIM)),
        "diff_k_gain": gain(ks[11], (N_CD, HEAD_DIM)),
        "diff_lambda": 0.1 * nrm(ks[12], (N_CD, 4, HEAD_DIM)),
        "diff_subln_gain": gain(ks[13], (N_CD, DIFF_V_DIM)),
        "swa_q_gain": gain(ks[14], (N_CD, HEAD_DIM)),
        "swa_k_gain": gain(ks[15], (N_CD, HEAD_DIM)),
        "swa_sinks": nrm(ks[16], (N_CD, SWA_Q_HEADS)),
        "w_out_cd": nrm(ks[17], (N_CD, MIX_W, D_MODEL)) * MIX_W ** -0.5,
    }


def reference(x, norm_gain, w_in_ab, b_forget, moba_q_gain, moba_k_gain, fox_q_gain,
              fox_k_gain, w_out_ab, w_in_cd, diff_q_gain, diff_k_gain, diff_lambda,
              diff_subln_gain, swa_q_gain, swa_k_gain, swa_sinks, w_out_cd):
    B, S, _ = x.shape
    moba_slopes = alibi_slopes(MOBA_HEADS)
    diff_slopes = alibi_slopes(DIFF_HEADS)
    swa_slopes = alibi_slopes(SWA_Q_HEADS)
    for layer in range(DEPTH):
        h = rmsnorm(x, norm_gain[layer])
        j = layer // 2
        if layer % 2 == 0:
            proj = h @ w_in_ab[j]
            qa, ka, va, ga, qb, kb_, vb_, gb, fb = split_cols(proj, AB_SIZES)
            qa = rmsnorm(to_heads(qa, MOBA_HEADS), moba_q_gain[j])
            ka = rmsnorm(to_heads(ka, MOBA_HEADS), moba_k_gain[j])
            ya = from_heads(moba_attention(qa, ka, to_heads(va, MOBA_HEADS), moba_slopes))
            qb = rmsnorm(to_heads(qb, FOX_HEADS), fox_q_gain[j])
            kb_ = rmsnorm(to_heads(kb_, FOX_HEADS), fox_k_gain[j])
            log_f = jax.nn.log_sigmoid(fb.astype(jnp.float32)
                                       + b_forget[j].astype(jnp.float32)).transpose(0, 2, 1)
            yb = from_heads(fox_attention(qb, kb_, to_heads(vb_, FOX_HEADS), log_f))
            y = jnp.concatenate([ya * jax.nn.silu(ga), yb * jax.nn.silu(gb)], axis=-1)
            x = x + y @ w_out_ab[j]
        else:
            proj = h @ w_in_cd[j]
            qc, kc, vc, gc, qd, kd, vd, gd = split_cols(proj, CD_SIZES)
            qc = rmsnorm(qc.reshape(B, S, DIFF_HEADS, 2, HEAD_DIM).transpose(0, 2, 1, 3, 4),
                         diff_q_gain[j])
            kc = rmsnorm(kc.reshape(B, S, DIFF_HEADS, 2, HEAD_DIM).transpose(0, 2, 1, 3, 4),
                         diff_k_gain[j])
            lam_init = 0.8 - 0.6 * math.exp(-0.3 * layer)
            lp = diff_lambda[j].astype(jnp.float32)
            lam = (jnp.exp(jnp.sum(lp[0] * lp[1])) - jnp.exp(jnp.sum(lp[2] * lp[3]))
                   + lam_init)
            oc = diff_attention(qc, kc, to_heads(vc, DIFF_HEADS), lam, diff_slopes)
            oc = rmsnorm(oc, diff_subln_gain[j]) * (1.0 - lam_init)
            yc = from_heads(oc)
            qd = rmsnorm(qd.reshape(B, S, SWA_Q_HEADS, HEAD_DIM), swa_q_gain[j])
            kd = rmsnorm(kd.reshape(B, S, SWA_KV_HEADS, HEAD_DIM), swa_k_gain[j])
            vd = vd.reshape(B, S, SWA_KV_HEADS, HEAD_DIM)
            yd = swa_attention(qd, kd, vd, swa_sinks[j], swa_slopes)
            y = jnp.concatenate([yc * jax.nn.silu(gc), yd * jax.nn.silu(gd)], axis=-1)
            x = x + y @ w_out_cd[j]
    return x
```

```python
import math
import numpy as np
from contextlib import ExitStack
import concourse.bass as bass
import concourse.mybir as mybir
from concourse.ap import AP
from concourse.bass_utils import run_bass_kernel_spmd

F32 = mybir.dt.float32
BF16 = mybir.dt.bfloat16
ALU = mybir.AluOpType
AF = mybir.ActivationFunctionType
AX = mybir.AxisListType

ENGS = ['pe', 'act', 'dve', 'pool', 'sp']
S = 2048
D = 1024
NT = 16
KR = 76
EPS = 1e-6
NEG = -30000.0


class Prog:
    def __init__(self, nc, stack):
        self.nc = nc
        self.stack = stack
        self.q = {k: [] for k in ENGS}
        self.cnt = {}
        self.sems = {}
        self.seen = {k: {} for k in ENGS}
        self.w = {}
        self.r = {}
        for k in ENGS:
            self._sem('c_' + k)

    def _sem(self, name):
        if name not in self.sems:
            self.sems[name] = self.stack.enter_context(self.nc.semaphore(name))
            self.cnt[name] = 0
        return self.sems[name]

    def sb(self, name, shape, dt):
        return self.stack.enter_context(self.nc.sbuf_tensor(name, shape, dt))

    def ps(self, name, shape, dt=F32):
        return self.stack.enter_context(self.nc.psum_tensor(name, shape, dt))

    def _deps(self, eng, reads, writes):
        deps = {}

        def add(sig):
            if sig is None:
                return
            s, v = sig
            if deps.get(s, 0) < v:
                deps[s] = v
        for r in reads:
            add(self.w.get(r))
        for w_ in writes:
            add(self.w.get(w_))
            for sig in self.r.get(w_, ()):
                add(sig)
        out = []
        own = 'c_' + eng
        for s, v in deps.items():
            if eng == 'pe' and s == own:
                continue
            if self.seen[eng].get(s, 0) < v:
                self.seen[eng][s] = v
                out.append((s, v))
        return out

    def _record(self, sig, reads, writes):
        for r in reads:
            lst = self.r.setdefault(r, [])
            lst[:] = [x for x in lst if x[0] != sig[0]] + [sig]
        for w_ in writes:
            self.w[w_] = sig
            self.r[w_] = []

    def op(self, eng, fn, reads=(), writes=()):
        waits = self._deps(eng, reads, writes)
        name = 'c_' + eng
        self.cnt[name] += 1
        sig = (name, self.cnt[name])
        self.q[eng].append((waits, fn, name, 1))
        self._record(sig, reads, writes)
        return sig

    def dma(self, eng, out, in_, sem, reads=(), writes=()):
        waits = self._deps(eng, reads, writes)
        self._sem(sem)
        self.cnt[sem] += 16
        sig = (sem, self.cnt[sem])
        self.q[eng].append((waits, lambda e: e.dma_start(out=out, in_=in_), sem, 16))
        self._record(sig, reads, writes)
        return sig

    def flush(self, sem, resources):
        for r in resources:
            self.w[r] = (sem, self.cnt[sem])

    def wait_all(self, eng, resources):
        waits = self._deps(eng, resources, ())
        self.q[eng].append((waits, None, None, 0))

    def emit(self):
        nc = self.nc
        names = {'pe': 'tensor', 'act': 'scalar', 'dve': 'vector', 'pool': 'gpsimd', 'sp': 'sync'}
        with nc.Block() as block:
            for k in ENGS:
                lst = self.q[k]
                if not lst:
                    continue

                def body(e, lst=lst):
                    for waits, fn, sname, inc in lst:
                        for s, v in waits:
                            e.wait_ge(self.sems[s], v)
                        if fn is not None:
                            fn(e).then_inc(self.sems[sname], inc)
                getattr(block, names[k])(body)


class Ring:
    def __init__(self, items):
        self.items = items
        self.i = 0

    def next(self):
        it = self.items[self.i % len(self.items)]
        self.i += 1
        return it


def alibi_slopes(n):
    return [2.0 ** (-8.0 * (i + 1) / n) for i in range(n)]


def bview(ap, pattern):
    p = ap.ap[0]
    return AP(ap.tensor, ap.offset, [[p[0], p[1]]] + [list(x) for x in pattern])


def build(nseq=2, layers=(0, 1, 2, 3), dbg=False, phase=(0, 0, 0, 0)):
    nc = bass.Bass("TRN2", target_bir_lowering=False)
    dt_in = lambda name, shape: nc.dram_tensor(name, list(shape), F32, kind="ExternalInput").ap()
    xin = dt_in("xin", [nseq, S, D])
    norm_gain = dt_in("norm_gain", [4, D])
    w_in_ab = dt_in("w_in_ab", [2, D, 4104])
    b_forget = dt_in("b_forget", [2, 8])
    w_out_ab = dt_in("w_out_ab", [2, D, D])
    w_in_cd = dt_in("w_in_cd", [2, D, 3328])
    w_out_cd = dt_in("w_out_cd", [2, D, D])
    hg = {n: dt_in(n, [2, 64]) for n in ["moba_q_gain", "moba_k_gain", "fox_q_gain", "fox_k_gain",
                                        "diff_q_gain", "diff_k_gain", "swa_q_gain", "swa_k_gain"]}
    diff_lambda = dt_in("diff_lambda", [2, 4, 64])
    diff_subln_gain = dt_in("diff_subln_gain", [2, 128])
    swa_sinks = dt_in("swa_sinks", [2, 8])
    c_ident = dt_in("c_ident", [128, 128])
    c_bd = dt_in("c_bd", [128, 128])
    c_a128 = dt_in("c_a128", [128, 128])
    c_tric = dt_in("c_tric", [128, 128])
    c_trip = dt_in("c_trip", [128, 128])
    c_triu = dt_in("c_triu", [128, 128])
    c_kaug = dt_in("c_kaug", [12, S])
    c_qalibi = dt_in("c_qalibi", [9, 4, S])
    c_cmask = dt_in("c_cmask", [128, 128])
    yout = nc.dram_tensor("yout", [nseq, S, D], F32, kind="ExternalOutput").ap()

    with ExitStack() as st:
        P = Prog(nc, st)
        x_sb = P.sb("x_sb", [128, NT, D], F32)
        hT = P.sb("hT", [128, 8, S], BF16)
        yT = P.sb("yT", [128, 8, S], BF16)
        NW = 3
        wbuf = [P.sb("wb%d" % i, [128, 8, 256], BF16) for i in range(NW)]
        QA = [P.sb("QA%d" % i, [128, S], BF16) for i in range(2)]
        KA = [P.sb("KA%d" % i, [128, S], BF16) for i in range(2)]
        VA = P.sb("VA", [128, NT, 512], BF16)
        NPR = 3
        Pt = [P.sb("Pt%d" % i, [128, 512], BF16) for i in range(NPR)]
        sqr = [P.sb("sq%d" % i, [128, 512], BF16) for i in range(1)]
        lnr = [P.sb("ln%d" % i, [128, 512], F32) for i in range(2)]
        rdr = [P.sb("rd%d" % i, [128, 512], F32) for i in range(1)]
        rgr = [P.sb("rg%d" % i, [128, 512], F32) for i in range(1)]
        n1t = P.sb("n1t", [128, 512], F32)
        n2t = P.sb("n2t", [128, 512], F32)
        gnb = P.sb("gnb", [128, D], F32)
        hrow = [P.sb("hrow%d" % i, [128, D], BF16) for i in range(1)]
        junk = hT[:, 0, 0:D]
        ssx = P.sb("ssx", [128, NT], F32)
        lnx = P.sb("lnx", [128, NT], F32)
        rsx = P.sb("rsx", [128, NT], F32)
        ident = P.sb("ident", [128, 128], BF16)
        bd = P.sb("bd", [128, 128], BF16)
        a128 = P.sb("a128", [128, 128], BF16)
        tric = P.sb("tric", [128, 128], BF16)
        trip = P.sb("trip", [128, 128], BF16)
        triu_f = P.sb("triu_f", [128, 128], F32)
        ones_f = P.sb("ones_f", [128, 128], F32)
        cmask = P.sb("cmask", [128, 128], F32)
        gcols = P.sb("gcols", [128, 20], F32)
        bfb = P.sb("bfb", [128, 2, 8], F32)
        lamb = n1t[:, :].rearrange("p (a b c) -> p a b c", a=2, b=4)
        lamw = n2t[:, 0:256].rearrange("p (a b c) -> p a b c", a=2, b=2)
        lams = P.sb("lams", [128, 2, 2], F32)
        lame = P.sb("lame", [128, 2, 2], F32)
        nlam = P.sb("nlam", [128, 2], F32)
        sinkb = P.sb("sinkb", [128, 2, 8], F32)
        esink = P.sb("esink", [128, 2, 8], F32)
        KM = P.sb("KM", [128, 16], BF16)
        kmf = P.sb("kmf", [128, 8], F32)
        kmf2 = P.sb("kmf2", [128, 8], F32)
        Gsb = P.sb("Gsb", [128, 256], F32)
        Gm = P.sb("Gm", [128, 128], F32)
        cmpt = gnb
        rank = P.sb("rank", [128, 128], F32)
        MT = P.sb("MT", [128, 128], BF16)
        zf = P.sb("zf", [128, 128], F32)
        Lf = P.sb("Lf", [128, 128], F32)
        cwt = P.sb("cwt", [128, 256], F32)
        cpre = P.sb("cpre", [128, 128], F32)
        cneg = P.sb("cneg", [128, 128], F32)
        cend = P.sb("cend", [128, 4, 8], F32)
        FB = P.sb("FB", [128, 8, NT, 4], F32)
        Sps = [P.ps("S%d" % i, [128, 512]) for i in range(2)]
        Ops = [P.ps("O%d" % i, [128, 512]) for i in range(3)]
        Mps = [P.ps("M%d" % i, [128, 512]) for i in range(2)]
        Tps = P.ps("Tps", [128, 1024], BF16)
        Sring = Ring([('S%d' % i, Sps[i]) for i in range(2)])
        Oring = Ring([('O%d' % i, Ops[i]) for i in range(3)])
        Mring = Ring([('M%d' % i, Mps[i]) for i in range(2)])
        Pring = Ring([('Pt%d' % i, Pt[i]) for i in range(NPR)])
        sqring = Ring([('sq%d' % i, sqr[i]) for i in range(1)])
        lnring = Ring([('ln%d' % i, lnr[i]) for i in range(2)])
        rdring = Ring([('rd%d' % i, rdr[i]) for i in range(1)])
        rgring = Ring([('rg%d' % i, rgr[i]) for i in range(1)])
        hring = Ring([('hrow%d' % i, hrow[i]) for i in range(1)])
        Oring.i, Pring.i, Sring.i, Mring.i = phase

        P.dma('pool', ident[:], c_ident, 'cst', writes=['ident'])
        P.dma('pool', bd[:], c_bd, 'cst', writes=['bd'])
        P.dma('pool', a128[:], c_a128, 'cst', writes=['a128'])
        P.dma('pool', tric[:], c_tric, 'cst', writes=['tric'])
        P.dma('pool', trip[:], c_trip, 'cst', writes=['trip'])
        P.dma('sp', triu_f[:], c_triu, 'cst2', writes=['triu_f'])
        P.dma('sp', cmask[:], c_cmask, 'cst2', writes=['cmask'])
        P.op('dve', lambda e: e.memset(ones_f[:], 1.0), writes=['ones_f'])
        for i in range(2):
            P.op('dve', lambda e, i=i: e.memset(QA[i][:], 0.0), writes=['QA%d' % i, 'QAm%d' % i, 'QAa%d' % i])
            P.op('dve', lambda e, i=i: e.memset(KA[i][:], 0.0), writes=['KA%d' % i, 'KAaug%d' % i])
            P.dma('pool', KA[i][64:76, :], c_kaug, 'cst', reads=[], writes=['KAaug%d' % i])
        gnames = ["moba_q_gain", "moba_k_gain", "fox_q_gain", "fox_k_gain", "diff_q_gain", "diff_k_gain", "swa_q_gain", "swa_k_gain"]
        GC = {}
        col = 0
        P.op('dve', lambda e: e.memset(gcols[:], 0.0), writes=['gcols'])
        for j in range(2):
            for n in gnames:
                GC[(n, j)] = col
                src = hg[n][j].rearrange("(p o) -> p o", o=1)
                P.dma('sp', gcols[0:64, col:col + 1], src, 'cstg', writes=['gcols'])
                P.dma('sp', gcols[64:128, col:col + 1], src, 'cstg', writes=['gcols'])
                col += 1
        for j in range(2):
            GC[('subln', j)] = col
            P.dma('sp', gcols[:, col:col + 1], diff_subln_gain[j].rearrange("(p o) -> p o", o=1), 'cstg', writes=['gcols'])
            col += 1
        P.flush('cstg', ['gcols'])
        for j in range(2):
            for n in gnames:
                if n.endswith("q_gain"):
                    c = GC[(n, j)]
                    P.op('dve', lambda e, c=c: e.tensor_scalar(gcols[:, c:c + 1], gcols[:, c:c + 1], 0.125, None, ALU.mult), reads=['gcols'], writes=['gcols'])
            layer = 2 * j + 1
            lam_init = 0.8 - 0.6 * math.exp(-0.3 * layer)
            c = GC[('subln', j)]
            P.op('dve', lambda e, c=c, v=1.0 - lam_init: e.tensor_scalar(gcols[:, c:c + 1], gcols[:, c:c + 1], float(v), None, ALU.mult), reads=['gcols'], writes=['gcols'])
        P.dma('sp', bfb[:].rearrange("p a b -> p (a b)"), b_forget.rearrange("(o a) b -> o (a b)", o=1).partition_broadcast(128), 'cst2', writes=['bfb'])
        P.dma('sp', sinkb[:].rearrange("p a b -> p (a b)"), swa_sinks.rearrange("(o a) b -> o (a b)", o=1).partition_broadcast(128), 'cst4', writes=['sinkb'])
        P.dma('sp', n1t[:, :], diff_lambda.rearrange("(o a) b c -> o (a b c)", o=1).partition_broadcast(128), 'cst3', writes=['n1t'])
        P.op('act', lambda e: e.activation(esink[:], sinkb[:], AF.Exp), reads=['sinkb'], writes=['esink'])
        P.op('dve', lambda e: e.tensor_tensor(lamw, bview(lamb[:, 0, 0, :], [[256, 2], [128, 2], [1, 64]]),
                                              bview(lamb[:, 0, 1, :], [[256, 2], [128, 2], [1, 64]]), ALU.mult), reads=['n1t'], writes=['n2t'])
        P.op('dve', lambda e: e.tensor_reduce(lams[:], lamw, AX.X, ALU.add), reads=['n2t'], writes=['lams'])
        P.op('act', lambda e: e.activation(lame[:], lams[:], AF.Exp), reads=['lams'], writes=['lame'])
        for j in range(2):
            lam_init = 0.8 - 0.6 * math.exp(-0.3 * (2 * j + 1))
            P.op('dve', lambda e, j=j, li=lam_init: e.scalar_tensor_tensor(nlam[:, j:j + 1], lame[:, j, 1:2], float(-li), lame[:, j, 0:1], ALU.add, ALU.subtract),
                 reads=['lame'], writes=['nlam'])

        P.flush('cst', ['ident', 'bd', 'a128', 'tric', 'trip', 'KAaug0', 'KAaug1'])
        P.flush('cst2', ['triu_f', 'cmask', 'bfb'])
        wstate = {'n': 0}

        def wload(parts):
            i = wstate['n'] % NW
            wstate['n'] += 1
            name = 'wb%d' % i
            for src, off in parts:
                wdt = src.shape[1]
                P.dma('pool', wbuf[i][:, :, off:off + wdt], src.rearrange("(k p) c -> p k c", p=128), 'w%d' % i, writes=[name])
            return name, wbuf[i]

        def rms_to_hT(l):
            P.dma('sp', gnb[:], norm_gain[l:l + 1, :].partition_broadcast(128), 'gnb', writes=['gnb'])
            for i in range(NT):
                P.op('act', lambda e, i=i: e.activation(junk, x_sb[:, i, :], AF.Square, accum_out=ssx[:, i:i + 1]), reads=['x%d' % i], writes=['hT', 'ssx'])
            P.op('act', lambda e: e.activation(lnx[:], ssx[:], AF.Ln, bias=EPS, scale=1.0 / D), reads=['ssx'], writes=['lnx'])
            P.op('act', lambda e: e.activation(rsx[:], lnx[:], AF.Exp, scale=-0.5), reads=['lnx'], writes=['rsx'])
            for i in range(NT):
                hn, ht = hring.next()
                P.op('dve', lambda e, i=i, ht=ht: e.scalar_tensor_tensor(ht[:], x_sb[:, i, :], rsx[:, i:i + 1], gnb[:], ALU.mult, ALU.mult),
                     reads=['x%d' % i, 'rsx', 'gnb'], writes=[hn])
                for k in range(8):
                    P.op('pe', lambda e, k=k, ht=ht: e.transpose(Tps[:, k * 128:(k + 1) * 128], ht[:, k * 128:(k + 1) * 128], ident[:]),
                         reads=[hn, 'ident'], writes=['Tps'])
                eng = 'dve' if i % 2 == 0 else 'act'
                src = Tps[:, :].rearrange("p (k t) -> p k t", k=8)
                dst = hT[:, :, i * 128:(i + 1) * 128]
                if eng == 'dve':
                    P.op('dve', lambda e, src=src, dst=dst: e.tensor_copy(dst, src), reads=['Tps'], writes=['hT'])
                else:
                    P.op('act', lambda e, src=src, dst=dst: e.copy(dst, src), reads=['Tps'], writes=['hT'])

        def inproj_fm(wname, wt, woff, tc, Mn, Mt, M=128):
            for k in range(8):
                P.op('pe', lambda e, k=k: e.matmul(Mt[0:M, :], wt[:, k, woff:woff + M], hT[:, k, tc * 512:(tc + 1) * 512], start=(k == 0), stop=(k == 7)),
                     reads=[wname, 'hT'], writes=[Mn])

        def gates(W, gcols_list):
            for blk in range(4):
                wname, wt = wload([(W[:, gcols_list[2 * blk]:gcols_list[2 * blk] + 128], 0), (W[:, gcols_list[2 * blk + 1]:gcols_list[2 * blk + 1] + 128], 128)])
                for c in range(2):
                    p = 2 * blk + c
                    for tc in range(4):
                        Mn, Mt = Mring.next()
                        inproj_fm(wname, wt, c * 128, tc, Mn, Mt)
                        P.op('act', lambda e, Mt=Mt, p=p, tc=tc: e.activation(yT[:, p, tc * 512:(tc + 1) * 512], Mt[:], AF.Silu), reads=[Mn], writes=['yT%d' % p])

        def qk_pair(wname, wt, woff, gcol, dst, dstnames):
            for tc in range(4):
                Mn, Mt = Mring.next()
                inproj_fm(wname, wt, woff, tc, Mn, Mt)
                sn, sq = sqring.next()
                P.op('act', lambda e, Mt=Mt, sq=sq: e.activation(sq[:], Mt[:], AF.Square), reads=[Mn], writes=[sn])
                Sn, St = Sring.next()
                P.op('pe', lambda e, St=St, sq=sq: e.matmul(St[:], bd[:], sq[:], start=True, stop=True), reads=[sn, 'bd'], writes=[Sn])
                ln_n, ln_t = lnring.next()
                P.op('act', lambda e, St=St, ln_t=ln_t: e.activation(ln_t[:], St[:], AF.Ln, bias=EPS), reads=[Sn], writes=[ln_n])
                rn, rt = ln_n, ln_t
                P.op('act', lambda e, ln_t=ln_t: e.activation(ln_t[:], ln_t[:], AF.Exp, scale=-0.5), reads=[ln_n], writes=[ln_n])
                cs = slice(tc * 512, (tc + 1) * 512)
                P.op('dve', lambda e, Mt=Mt, rt=rt, cs=cs: e.scalar_tensor_tensor(dst[0][0:64, cs], Mt[0:64, :], gcols[0:64, gcol:gcol + 1], rt[0:64, :], ALU.mult, ALU.mult),
                     reads=[Mn, rn, 'gcols'], writes=[dstnames[0]])
                P.op('dve', lambda e, Mt=Mt, rt=rt, cs=cs: e.scalar_tensor_tensor(dst[1][0:64, cs], Mt[64:128, :], gcols[64:128, gcol:gcol + 1], rt[64:128, :], ALU.mult, ALU.mult),
                     reads=[Mn, rn, 'gcols'], writes=[dstnames[1]])

        def load_alibi(hslot, sidx):
            P.dma('pool', QA[hslot][72:76, :], c_qalibi[sidx], 'qal%d' % hslot, writes=['QAa%d' % hslot])

        def dense_plan():
            plan = []
            for g in range(4):
                lst = []
                for j in range(4 * g + 4):
                    r = j - 4 * g
                    c0 = max(r, 0) * 128
                    masks = [(tric, 'tric', c0)] if r >= 0 else []
                    lst.append((j, c0, 512, masks))
                plan.append(lst)
            return plan

        def swa_plan():
            plan = []
            for g in range(4):
                lst = []
                for j in range(max(0, 4 * g - 1), 4 * g + 4):
                    masks = []
                    cols = []
                    for i, tab, tn in ((j, tric, 'tric'), (j + 1, trip, 'trip')):
                        if 4 * g <= i <= 4 * g + 3:
                            c = (i - 4 * g) * 128
                            masks.append((tab, tn, c))
                            cols.append(c)
                    lst.append((j, min(cols), max(cols) + 128, masks))
                plan.append(lst)
            return plan

        DENSE = dense_plan()
        SWA = swa_plan()

        def attn_group(qslot, kslot, g, plan_g, lhsT_fns, bias_fn, qreads, kreads, vreads):
            Os = [Oring.next() for _ in lhsT_fns]
            first = [True] * len(lhsT_fns)
            for (j, c0, c1, masks) in plan_g:
                Sn, St = Sring.next()
                fm = True
                for tab, tn, c in masks:
                    P.op('pe', lambda e, St=St, tab=tab, c=c, fm=fm: e.matmul(St[:, c:c + 128], ident[:], tab[:], start=fm, stop=False, skip_group_check=True),
                         reads=['ident', tn], writes=[Sn])
                    fm = False
                P.op('pe', lambda e, St=St, j=j, c0=c0, c1=c1, fm=fm: e.matmul(St[:, c0:c1], KA[kslot][0:KR, j * 128:(j + 1) * 128],
                                                                              QA[qslot][0:KR, g * 512 + c0:g * 512 + c1], start=fm, stop=True, skip_group_check=True),
                     reads=qreads + kreads, writes=[Sn])
                Pn, Ptile = Pring.next()
                b = bias_fn(j, g) if bias_fn is not None else None
                if b is None:
                    P.op('act', lambda e, St=St, Ptile=Ptile, c0=c0, c1=c1: e.activation(Ptile[:, c0:c1], St[:, c0:c1], AF.Exp), reads=[Sn], writes=[Pn])
                else:
                    bap, bname = b
                    P.op('act', lambda e, St=St, Ptile=Ptile, c0=c0, c1=c1, bap=bap: e.activation(Ptile[:, c0:c1], St[:, c0:c1], AF.Exp, bias=bap), reads=[Sn, bname], writes=[Pn])
                for v, lf in enumerate(lhsT_fns):
                    On, Ot = Os[v]
                    P.op('pe', lambda e, Ot=Ot, lf=lf, j=j, c0=c0, c1=c1, Ptile=Ptile, f=first[v]: e.matmul(Ot[:, c0:c1], lf(j), Ptile[:, c0:c1], start=f, stop=False, skip_group_check=True),
                         reads=[Pn] + vreads, writes=[On])
                    first[v] = False
            return Os

        def finalize_std(O, p, hf, g, extra_den=None):
            On, Ot = O
            b0 = 64 * hf
            cs = slice(g * 512, (g + 1) * 512)
            rn, rt = rdring.next()
            if extra_den is not None:
                P.op('dve', lambda e: e.tensor_scalar(rt[b0:b0 + 64, :], Ot[64:128, :], extra_den, None, ALU.add), reads=[On, 'esink'], writes=[rn])
                P.op('dve', lambda e: e.reciprocal(rt[b0:b0 + 64, :], rt[b0:b0 + 64, :]), reads=[rn], writes=[rn])
            else:
                P.op('dve', lambda e: e.reciprocal(rt[b0:b0 + 64, :], Ot[64:128, :]), reads=[On], writes=[rn])
            gn, gt = rgring.next()
            P.op('pool', lambda e: e.tensor_tensor(gt[b0:b0 + 64, :], rt[b0:b0 + 64, :], yT[b0:b0 + 64, p, cs], ALU.mult), reads=[rn, 'yT%d' % p], writes=[gn])
            P.op('dve', lambda e: e.tensor_tensor(yT[b0:b0 + 64, p, cs], Ot[0:64, :], gt[b0:b0 + 64, :], ALU.mult), reads=[On, gn], writes=['yT%d' % p])

        def v_block(W, vbase, ncols, layout):
            wname, wt = wload([(W[:, vbase:vbase + ncols], 0)])
            for i in range(NT):
                Mn, Mt = Mring.next()
                for k in range(8):
                    P.op('pe', lambda e, k=k, i=i, Mt=Mt: e.matmul(Mt[:, 0:ncols], hT[:, k, i * 128:(i + 1) * 128], wt[:, k, 0:ncols], start=(k == 0), stop=(k == 7)),
                         reads=[wname, 'hT'], writes=[Mn])
                nh, w0, stride_dst, dst0 = layout
                src = Mt[:, 0:ncols].rearrange("p (h d) -> p h d", h=nh)
                dstv = bview(VA[:, i, dst0:dst0 + 1], [[stride_dst, nh], [1, w0]])
                if i % 2 == 0:
                    P.op('dve', lambda e, src=src, dstv=dstv: e.tensor_copy(dstv, src), reads=[Mn], writes=['VA'])
                else:
                    P.op('act', lambda e, src=src, dstv=dstv: e.copy(dstv, src), reads=[Mn], writes=['VA'])

        def set_va_ones(regions):
            for (c0, strd, n, wdt) in regions:
                v = bview(VA[:, 0, c0:c0 + 1], [[512, NT], [strd, n], [1, wdt]])
                P.op('pool', lambda e, v=v: e.memset(v, 1.0), writes=['VA'])

        def moba_masks(hs):
            qn, kn = 'QA%d' % hs, 'KA%d' % hs
            P.op('dve', lambda e: e.tensor_reduce(kmf[0:64, :], KA[hs][0:64, :].rearrange("p (n l) -> p n l", n=8), AX.X, ALU.add), reads=[kn], writes=['kmf'])
            P.op('dve', lambda e: e.tensor_copy(KM[0:64, 0:8], kmf[0:64, :]), reads=['kmf'], writes=['KM'])
            P.op('dve', lambda e: e.tensor_tensor(kmf2[0:64, :], kmf[0:64, :], KM[0:64, 0:8], ALU.subtract), reads=['kmf', 'KM'], writes=['kmf2'])
            P.op('dve', lambda e: e.tensor_copy(KM[0:64, 8:16], kmf2[0:64, :]), reads=['kmf2'], writes=['KM'])
            Mn, Mt = Mring.next()
            for i in range(NT):
                P.op('pe', lambda e, i=i, Mt=Mt: e.matmul(Mt[:, i * 16:(i + 1) * 16], QA[hs][0:64, i * 128:(i + 1) * 128], KM[0:64, :], start=(i == 0), stop=(i == NT - 1), skip_group_check=True),
                     reads=[qn, 'KM'], writes=[Mn])
            P.op('act', lambda e, Mt=Mt: e.copy(Gsb[:], Mt[:, 0:256]), reads=[Mn], writes=['Gsb'])
            gv = Gsb[:].rearrange("p (i c) -> p i c", c=16)
            P.op('dve', lambda e: e.tensor_tensor(Gm[:].rearrange("p (i n) -> p i n", n=8), gv[:, :, 0:8], gv[:, :, 8:16], ALU.add), reads=['Gsb'], writes=['Gm'])
            P.op('dve', lambda e: e.tensor_tensor(Gm[:], Gm[:], cmask[:], ALU.add), reads=['Gm', 'cmask'], writes=['Gm'])
            in0 = bview(Gm[:, 0:1], [[8, NT], [0, 8], [1, 8]])
            in1 = bview(Gm[:, 0:1], [[8, NT], [1, 8], [0, 8]])
            P.op('dve', lambda e: e.tensor_tensor(cmpt[:].rearrange("p (i n m) -> p i n m", n=8, m=8), in0, in1, ALU.is_gt), reads=['Gm'], writes=['gnb'])
            P.op('dve', lambda e: e.tensor_reduce(rank[:].rearrange("p (i n) -> p i n", n=8), cmpt[:].rearrange("p (i n m) -> p i n m", n=8, m=8), AX.X, ALU.add), reads=['gnb'], writes=['rank'])
            P.op('dve', lambda e: e.tensor_scalar(MT[:], rank[:], 3.5, NEG, ALU.is_gt, ALU.mult), reads=['rank'], writes=['MT'])
            for half in range(2):
                for ii in range(8):
                    i = half * 8 + ii
                    P.op('pe', lambda e, i=i, ii=ii: e.transpose(Tps[0:8, ii * 128:(ii + 1) * 128], MT[:, i * 8:(i + 1) * 8], ident[:]), reads=['MT', 'ident'], writes=['Tps'])
                P.op('act', lambda e, half=half: e.copy(QA[hs][64:72, half * 1024:(half + 1) * 1024], Tps[0:8, :]), reads=['Tps'], writes=['QAm%d' % hs])

        def fox_prep(W, j):
            wname, wt = wload([(W[:, 4096:4104], 0)])
            Mn, Mt = Mring.next()
            for i in range(NT):
                for k in range(8):
                    P.op('pe', lambda e, i=i, k=k, Mt=Mt: e.matmul(Mt[:, i * 8:(i + 1) * 8], hT[:, k, i * 128:(i + 1) * 128], wt[:, k, 0:8],
                                                                start=(i == 0 and k == 0), stop=(i == NT - 1 and k == 7), skip_group_check=True),
                         reads=[wname, 'hT'], writes=[Mn])
            bb = bview(bfb[:, j, 0:1], [[0, NT], [1, 8]])
            P.op('dve', lambda e, Mt=Mt: e.tensor_tensor(zf[:].rearrange("p (i h) -> p i h", h=8), Mt[:, 0:128].rearrange("p (i h) -> p i h", h=8), bb, ALU.add), reads=[Mn, 'bfb'], writes=['zf'])
            P.op('act', lambda e: e.activation(zf[:], zf[:], AF.Exp, scale=-1.0), reads=['zf'], writes=['zf'])
            P.op('act', lambda e: e.activation(Lf[:], zf[:], AF.Ln, bias=1.0), reads=['zf'], writes=['Lf'])
            Mn2, Mt2 = Mring.next()
            P.op('pe', lambda e: e.matmul(Mt2[:, 0:128], triu_f[:], Lf[:], start=True, stop=True), reads=['triu_f', 'Lf'], writes=[Mn2])
            P.op('pe', lambda e: e.matmul(Mt2[:, 128:256], ones_f[:], Lf[:], start=False, stop=True, skip_group_check=True), reads=['ones_f', 'Lf'], writes=[Mn2])
            P.op('act', lambda e: e.copy(cwt[:], Mt2[:, 0:256]), reads=[Mn2], writes=['cwt'])
            P.op('dve', lambda e: e.memset(cpre[:, 0:8], 0.0), writes=['cpre'])
            for i in range(1, NT):
                P.op('dve', lambda e, i=i: e.tensor_tensor(cpre[:, i * 8:(i + 1) * 8], cpre[:, (i - 1) * 8:i * 8], cwt[:, 128 + (i - 1) * 8:128 + i * 8], ALU.add),
                     reads=['cpre', 'cwt'], writes=['cpre'])
            P.op('dve', lambda e: e.tensor_tensor(cneg[:], cwt[:, 0:128], cpre[:], ALU.add), reads=['cwt', 'cpre'], writes=['cneg'])
            for g in range(4):
                i = 4 * g + 3
                P.op('dve', lambda e, g=g, i=i: e.tensor_tensor(cend[:, g, :], cpre[:, i * 8:(i + 1) * 8], cwt[:, 128 + i * 8:128 + (i + 1) * 8], ALU.add), reads=['cpre', 'cwt'], writes=['cend'])
            for g in range(4):
                outv = bview(FB[:, 0, 0, g:g + 1], [[NT * 4, 8], [4, NT]])
                in0 = bview(cneg[:, 0:1], [[1, 8], [8, NT]])
                in1 = bview(cend[:, g, 0:1], [[1, 8], [0, NT]])
                P.op('dve', lambda e, outv=outv, in0=in0, in1=in1: e.tensor_tensor(outv, in0, in1, ALU.subtract), reads=['cneg', 'cend'], writes=['FB'])

        def layer_ab(l):
            j = l // 2
            W = w_in_ab[j]
            rms_to_hT(l)
            gates(W, [1536 + 128 * p for p in range(4)] + [3584 + 128 * p for p in range(4)])
            set_va_ones([(64, 128, 4, 64)])
            for mixer in range(2):
                qb, kb, vb = (0, 512, 1024) if mixer == 0 else (2048, 2560, 3072)
                gq = GC[("moba_q_gain" if mixer == 0 else "fox_q_gain", j)]
                gk = GC[("moba_k_gain" if mixer == 0 else "fox_k_gain", j)]
                if mixer == 1:
                    fox_prep(W, j)
                for sbt in range(2):
                    v_block(W, vb + 256 * sbt, 256, (4, 64, 128, 0))
                    for pp in range(2):
                        pair = 2 * sbt + pp
                        wname, wt = wload([(W[:, qb + 128 * pair:qb + 128 * pair + 128], 0), (W[:, kb + 128 * pair:kb + 128 * pair + 128], 128)])
                        qk_pair(wname, wt, 0, gq, QA, ['QA0', 'QA1'])
                        qk_pair(wname, wt, 128, gk, KA, ['KA0', 'KA1'])
                        for hs in range(2):
                            h = 2 * pair + hs
                            if mixer == 0:
                                load_alibi(hs, h + 1)
                                moba_masks(hs)
                                bias_fn = None
                            else:
                                load_alibi(hs, 0)
                                if pair == 0:
                                    P.op('dve', lambda e, hs=hs: e.memset(QA[hs][64:72, :], 0.0), writes=['QAm%d' % hs])
                                bias_fn = (lambda jj, g, h=h: (FB[:, h, jj, g:g + 1], 'FB'))
                            hv = 2 * pp + hs
                            lf = lambda jj, hv=hv: VA[:, jj, hv * 128:(hv + 1) * 128]
                            for g in range(4):
                                Os = attn_group(hs, hs, g, DENSE[g], [lf], bias_fn, ['QA%d' % hs, 'QAm%d' % hs, 'QAa%d' % hs], ['KA%d' % hs, 'KAaug%d' % hs], ['VA'])
                                finalize_std(Os[0], 4 * mixer + pair, hs, g)
            out_proj(w_out_ab[j])

        def layer_cd(l):
            j = l // 2
            W = w_in_cd[j]
            rms_to_hT(l)
            gates(W, [1536 + 128 * p for p in range(4)] + [2816 + 128 * p for p in range(4)])
            set_va_ones([(64, 256, 2, 128)])
            gq, gk = GC[("diff_q_gain", j)], GC[("diff_k_gain", j)]
            for hs in range(2):
                P.op('dve', lambda e, hs=hs: e.memset(QA[hs][64:72, :], 0.0), writes=['QAm%d' % hs])
            sub_c = GC[('subln', j)]
            for sbt in range(2):
                wname, wt = wload([(W[:, 1024 + 256 * sbt:1024 + 256 * sbt + 256], 0)])
                for i in range(NT):
                    Mn, Mt = Mring.next()
                    for k in range(8):
                        P.op('pe', lambda e, k=k, i=i, Mt=Mt, wt=wt: e.matmul(Mt[:, 0:256], hT[:, k, i * 128:(i + 1) * 128], wt[:, k, 0:256], start=(k == 0), stop=(k == 7)),
                             reads=[wname, 'hT'], writes=[Mn])
                    src = Mt[:, 0:256].rearrange("p (h c d) -> p h c d", h=2, c=2)
                    dstv = bview(VA[:, i, 0:1], [[256, 2], [192, 2], [1, 64]])
                    if i % 2 == 0:
                        P.op('dve', lambda e, src=src, dstv=dstv: e.tensor_copy(dstv, src), reads=[Mn], writes=['VA'])
                    else:
                        P.op('act', lambda e, src=src, dstv=dstv: e.copy(dstv, src), reads=[Mn], writes=['VA'])
                for pp in range(2):
                    h = 2 * sbt + pp
                    wname, wt = wload([(W[:, 128 * h:128 * h + 128], 0), (W[:, 512 + 128 * h:512 + 128 * h + 128], 128)])
                    qk_pair(wname, wt, 0, gq, QA, ['QA0', 'QA1'])
                    qk_pair(wname, wt, 128, gk, KA, ['KA0', 'KA1'])
                    for c in range(2):
                        load_alibi(c, 2 * h + 2)
                    lfs = [lambda jj, pp=pp: VA[:, jj, pp * 256:pp * 256 + 128], lambda jj, pp=pp: VA[:, jj, pp * 256 + 128:pp * 256 + 256]]
                    for g in range(4):
                        cs = slice(g * 512, (g + 1) * 512)
                        for c in range(2):
                            Os = attn_group(c, c, g, DENSE[g], lfs, None, ['QA%d' % c, 'QAm%d' % c, 'QAa%d' % c], ['KA%d' % c, 'KAaug%d' % c], ['VA'])
                            (On0, Ot0), (On1, Ot1) = Os
                            rn, rt = rdring.next()
                            P.op('dve', lambda e, rt=rt, Ot0=Ot0: e.reciprocal(rt[0:64, :], Ot0[64:128, :]), reads=[On0], writes=[rn])
                            P.op('dve', lambda e, rt=rt, Ot1=Ot1: e.reciprocal(rt[64:128, :], Ot1[0:64, :]), reads=[On1], writes=[rn])
                            dstt = n1t if c == 0 else n2t
                            dn = 'n1t' if c == 0 else 'n2t'
                            P.op('dve', lambda e, rt=rt, Ot0=Ot0, dstt=dstt: e.tensor_tensor(dstt[0:64, :], Ot0[0:64, :], rt[0:64, :], ALU.mult), reads=[On0, rn], writes=[dn])
                            P.op('dve', lambda e, rt=rt, Ot1=Ot1, dstt=dstt: e.tensor_tensor(dstt[64:128, :], Ot1[64:128, :], rt[64:128, :], ALU.mult), reads=[On1, rn], writes=[dn])
                        P.op('dve', lambda e: e.scalar_tensor_tensor(n1t[:], n2t[:], nlam[:, j:j + 1], n1t[:], ALU.mult, ALU.add), reads=['n1t', 'n2t', 'nlam'], writes=['n1t'])
                        sn, sq = sqring.next()
                        P.op('act', lambda e, sq=sq: e.activation(sq[:], n1t[:], AF.Square), reads=['n1t'], writes=[sn])
                        Sn, St = Sring.next()
                        P.op('pe', lambda e, St=St, sq=sq: e.matmul(St[:], a128[:], sq[:], start=True, stop=True), reads=[sn, 'a128'], writes=[Sn])
                        ln_n, ln_t = lnring.next()
                        P.op('act', lambda e, St=St, ln_t=ln_t: e.activation(ln_t[:], St[:], AF.Ln, bias=EPS), reads=[Sn], writes=[ln_n])
                        rn2, rt2 = ln_n, ln_t
                        P.op('act', lambda e, ln_t=ln_t: e.activation(ln_t[:], ln_t[:], AF.Exp, scale=-0.5), reads=[ln_n], writes=[ln_n])
                        gn, gt = rgring.next()
                        P.op('pool', lambda e, gt=gt, rt2=rt2, cs=cs, h=h: e.tensor_tensor(gt[:], rt2[:], yT[:, h, cs], ALU.mult), reads=[rn2, 'yT%d' % h], writes=[gn])
                        P.op('dve', lambda e, gt=gt, cs=cs, h=h: e.scalar_tensor_tensor(yT[:, h, cs], n1t[:], gcols[:, sub_c:sub_c + 1], gt[:], ALU.mult, ALU.mult),
                             reads=['n1t', gn, 'gcols'], writes=['yT%d' % h])
            set_va_ones([(64, 128, 2, 64)])
            gq, gk = GC[("swa_q_gain", j)], GC[("swa_k_gain", j)]
            v_block(W, 2688, 128, (2, 64, 128, 0))
            wname, wt = wload([(W[:, 2560:2688], 0)])
            qk_pair(wname, wt, 0, gk, KA, ['KA0', 'KA1'])
            for blk in range(2):
                wname, wt = wload([(W[:, 2048 + 256 * blk:2048 + 256 * blk + 256], 0)])
                for pp in range(2):
                    pair = 2 * blk + pp
                    qk_pair(wname, wt, 128 * pp, gq, QA, ['QA0', 'QA1'])
                    kv = pair // 2
                    for hs in range(2):
                        hq = 2 * pair + hs
                        load_alibi(hs, hq + 1)
                        lf = lambda jj, kv=kv: VA[:, jj, kv * 128:(kv + 1) * 128]
                        for g in range(4):
                            Os = attn_group(hs, kv, g, SWA[g], [lf], None, ['QA%d' % hs, 'QAm%d' % hs, 'QAa%d' % hs], ['KA%d' % kv, 'KAaug%d' % kv], ['VA'])
                            finalize_std(Os[0], 4 + pair, hs, g, extra_den=esink[64:128, j, hq:hq + 1])
            out_proj(w_out_cd[j])

        def out_proj(Wo):
            for blk in range(4):
                wname, wt = wload([(Wo[:, 256 * blk:256 * blk + 256], 0)])
                for i in range(NT):
                    Mn, Mt = Mring.next()
                    for k in range(8):
                        P.op('pe', lambda e, k=k, i=i, Mt=Mt, wt=wt: e.matmul(Mt[:, 0:256], yT[:, k, i * 128:(i + 1) * 128], wt[:, k, :], start=(k == 0), stop=(k == 7)),
                             reads=[wname] + ['yT%d' % k for k in range(8)] if k == 0 else [wname], writes=[Mn])
                    xs = x_sb[:, i, 256 * blk:256 * blk + 256]
                    P.op('dve', lambda e, xs=xs, Mt=Mt: e.tensor_tensor(xs, xs, Mt[:, 0:256], ALU.add), reads=[Mn, 'x%d' % i], writes=['x%d' % i])

        for s in range(nseq):
            for i in range(NT):
                P.dma('sp', x_sb[:, i, :], xin[s, i * 128:(i + 1) * 128, :], 'xld%d' % i, reads=[], writes=['x%d' % i])
            for l in layers:
                if l % 2 == 0:
                    layer_ab(l)
                else:
                    layer_cd(l)
            for i in range(NT):
                P.dma('sp', yout[s, i * 128:(i + 1) * 128, :], x_sb[:, i, :], 'xst%d' % i, reads=['x%d' % i], writes=['out%d' % i])
        P.wait_all('sp', ['out%d' % i for i in range(NT)])
        P.emit()
    return nc


def make_consts():
    c = {}
    c["c_ident"] = np.eye(128, dtype=np.float32)
    bdm = np.zeros((128, 128), np.float32)
    bdm[0:64, 0:64] = 1.0 / 64
    bdm[64:128, 64:128] = 1.0 / 64
    c["c_bd"] = bdm
    c["c_a128"] = np.full((128, 128), 1.0 / 128, np.float32)
    s = np.arange(128)[:, None]
    t = np.arange(128)[None, :]
    c["c_tric"] = np.where(s <= t, 0.0, NEG).astype(np.float32)
    c["c_trip"] = np.where(s > t, 0.0, NEG).astype(np.float32)
    c["c_triu"] = (s <= t).astype(np.float32)
    pos = np.arange(S)
    kaug = np.zeros((12, S), np.float32)
    for n in range(8):
        kaug[n] = (pos // 256 == n)
    kaug[8] = 128 * (pos // 128)
    kaug[9] = pos % 128
    kaug[10] = 1.0
    kaug[11] = 1.0
    c["c_kaug"] = kaug
    qal = np.zeros((9, 4, S), np.float32)
    for i in range(1, 9):
        sl = 2.0 ** (-i)
        qal[i, 0] = sl
        qal[i, 1] = sl
        qal[i, 2] = -sl * 128 * (pos // 128)
        qal[i, 3] = -sl * (pos % 128)
    c["c_qalibi"] = qal
    cm = np.zeros((128, 16, 8), np.float32)
    for i in range(16):
        qb = i // 2
        for n in range(8):
            cm[:, i, n] = 0.0 if n < qb else (1e30 if n == qb else -1e30)
    c["c_cmask"] = cm.reshape(128, 128)
    return c


PARAMS = ["norm_gain", "w_in_ab", "b_forget", "moba_q_gain", "moba_k_gain", "fox_q_gain", "fox_k_gain", "w_out_ab", "w_in_cd",
          "diff_q_gain", "diff_k_gain", "diff_lambda", "diff_subln_gain", "swa_q_gain", "swa_k_gain", "swa_sinks", "w_out_cd"]


LAUNCH_GROUPS = [(0,), (1,), (2,), (3,)]


def kernel(**inputs):
    x = np.ascontiguousarray(np.asarray(inputs["x"], dtype=np.float32))
    n = 8
    per = x.shape[0] // n
    consts = make_consts()
    base = {k: np.ascontiguousarray(np.asarray(inputs[k], dtype=np.float32)) for k in PARAMS}
    base.update(consts)
    cur = x
    for grp in LAUNCH_GROUPS:
        nc = build(nseq=per, layers=grp)
        in_maps = []
        for c in range(n):
            m = dict(base)
            m["xin"] = np.ascontiguousarray(cur[c * per:(c + 1) * per])
            in_maps.append(m)
        res = run_bass_kernel_spmd(nc, in_maps, core_ids=list(range(n)))
        cur = np.concatenate([r["yout"] for r in res.results], axis=0)
    return cur
```

```python
import math
import numpy as np
from contextlib import ExitStack
import concourse.bass as bass
import concourse.mybir as mybir
from concourse.ap import AP
from concourse.bass_utils import run_bass_kernel_spmd

F32 = mybir.dt.float32
BF16 = mybir.dt.bfloat16
ALU = mybir.AluOpType
AF = mybir.ActivationFunctionType
AX = mybir.AxisListType

ENGS = ['pe', 'act', 'dve', 'pool', 'sp']
S = 2048
D = 1024
NT = 16
KR = 76
EPS = 1e-6
NEG = -30000.0


class Prog:
    def __init__(self, nc, stack):
        self.nc = nc
        self.stack = stack
        self.q = {k: [] for k in ENGS}
        self.cnt = {}
        self.sems = {}
        self.seen = {k: {} for k in ENGS}
        self.w = {}
        self.r = {}
        for k in ENGS:
            self._sem('c_' + k)

    def _sem(self, name):
        if name not in self.sems:
            self.sems[name] = self.stack.enter_context(self.nc.semaphore(name))
            self.cnt[name] = 0
        return self.sems[name]

    def sb(self, name, shape, dt):
        return self.stack.enter_context(self.nc.sbuf_tensor(name, shape, dt))

    def ps(self, name, shape, dt=F32):
        return self.stack.enter_context(self.nc.psum_tensor(name, shape, dt))

    def _deps(self, eng, reads, writes):
        deps = {}

        def add(sig):
            if sig is None:
                return
            s, v = sig
            if deps.get(s, 0) < v:
                deps[s] = v
        for r in reads:
            add(self.w.get(r))
        for w_ in writes:
            add(self.w.get(w_))
            for sig in self.r.get(w_, ()):
                add(sig)
        out = []
        own = 'c_' + eng
        for s, v in deps.items():
            if eng == 'pe' and s == own:
                continue
            if self.seen[eng].get(s, 0) < v:
                self.seen[eng][s] = v
                out.append((s, v))
        return out

    def _record(self, sig, reads, writes):
        for r in reads:
            lst = self.r.setdefault(r, [])
            lst[:] = [x for x in lst if x[0] != sig[0]] + [sig]
        for w_ in writes:
            self.w[w_] = sig
            self.r[w_] = []

    def op(self, eng, fn, reads=(), writes=()):
        waits = self._deps(eng, reads, writes)
        name = 'c_' + eng
        self.cnt[name] += 1
        sig = (name, self.cnt[name])
        self.q[eng].append((waits, fn, name, 1))
        self._record(sig, reads, writes)
        return sig

    def dma(self, eng, out, in_, sem, reads=(), writes=()):
        waits = self._deps(eng, reads, writes)
        self._sem(sem)
        self.cnt[sem] += 16
        sig = (sem, self.cnt[sem])
        self.q[eng].append((waits, lambda e: e.dma_start(out=out, in_=in_), sem, 16))
        self._record(sig, reads, writes)
        return sig

    def flush(self, sem, resources):
        for r in resources:
            self.w[r] = (sem, self.cnt[sem])

    def wait_all(self, eng, resources):
        waits = self._deps(eng, resources, ())
        self.q[eng].append((waits, None, None, 0))

    def emit(self):
        nc = self.nc
        names = {'pe': 'tensor', 'act': 'scalar', 'dve': 'vector', 'pool': 'gpsimd', 'sp': 'sync'}
        with nc.Block() as block:
            for k in ENGS:
                lst = self.q[k]
                if not lst:
                    continue

                def body(e, lst=lst):
                    for waits, fn, sname, inc in lst:
                        for s, v in waits:
                            e.wait_ge(self.sems[s], v)
                        if fn is not None:
                            fn(e).then_inc(self.sems[sname], inc)
                getattr(block, names[k])(body)


class Ring:
    def __init__(self, items):
        self.items = items
        self.i = 0

    def next(self):
        it = self.items[self.i % len(self.items)]
        self.i += 1
        return it


def alibi_slopes(n):
    return [2.0 ** (-8.0 * (i + 1) / n) for i in range(n)]


def bview(ap, pattern):
    p = ap.ap[0]
    return AP(ap.tensor, ap.offset, [[p[0], p[1]]] + [list(x) for x in pattern])


SKIP = set()


def build(nseq=2, layers=(0, 1, 2, 3), dbg=False, phase=(0, 0, 0, 0)):
    nc = bass.Bass("TRN2", target_bir_lowering=False)
    dt_in = lambda name, shape: nc.dram_tensor(name, list(shape), F32, kind="ExternalInput").ap()
    xin = dt_in("xin", [nseq, S, D])
    norm_gain = dt_in("norm_gain", [4, D])
    w_in_ab = dt_in("w_in_ab", [2, D, 4104])
    b_forget = dt_in("b_forget", [2, 8])
    w_out_ab = dt_in("w_out_ab", [2, D, D])
    w_in_cd = dt_in("w_in_cd", [2, D, 3328])
    w_out_cd = dt_in("w_out_cd", [2, D, D])
    hg = {n: dt_in(n, [2, 64]) for n in ["moba_q_gain", "moba_k_gain", "fox_q_gain", "fox_k_gain",
                                        "diff_q_gain", "diff_k_gain", "swa_q_gain", "swa_k_gain"]}
    diff_lambda = dt_in("diff_lambda", [2, 4, 64])
    diff_subln_gain = dt_in("diff_subln_gain", [2, 128])
    swa_sinks = dt_in("swa_sinks", [2, 8])
    c_ident = dt_in("c_ident", [128, 128])
    c_bd = dt_in("c_bd", [128, 128])
    c_a128 = dt_in("c_a128", [128, 128])
    c_tric = dt_in("c_tric", [128, 128])
    c_trip = dt_in("c_trip", [128, 128])
    c_triu = dt_in("c_triu", [128, 128])
    c_kaug = dt_in("c_kaug", [12, S])
    c_qalibi = dt_in("c_qalibi", [9, 4, S])
    c_cmask = dt_in("c_cmask", [128, 128])
    yout = nc.dram_tensor("yout", [nseq, S, D], F32, kind="ExternalOutput").ap()

    with ExitStack() as st:
        P = Prog(nc, st)
        x_sb = P.sb("x_sb", [128, NT, D], F32)
        hT = P.sb("hT", [128, 8, S], BF16)
        yT = P.sb("yT", [128, 8, S], BF16)
        NW = 3
        wbuf = [P.sb("wb%d" % i, [128, 8, 256], BF16) for i in range(NW)]
        QA = [P.sb("QA%d" % i, [128, S], BF16) for i in range(2)]
        KA = [P.sb("KA%d" % i, [128, S], BF16) for i in range(2)]
        VA = P.sb("VA", [128, NT, 512], BF16)
        NPR = 3
        Pt = [P.sb("Pt%d" % i, [128, 512], BF16) for i in range(NPR)]
        sqr = [P.sb("sq%d" % i, [128, 512], BF16) for i in range(1)]
        lnr = [P.sb("ln%d" % i, [128, 512], F32) for i in range(2)]
        rdr = [P.sb("rd%d" % i, [128, 512], F32) for i in range(1)]
        rgr = [P.sb("rg%d" % i, [128, 512], F32) for i in range(1)]
        n1t = P.sb("n1t", [128, 512], F32)
        n2t = P.sb("n2t", [128, 512], F32)
        gnb = P.sb("gnb", [128, D], F32)
        hrow = [P.sb("hrow%d" % i, [128, D], BF16) for i in range(1)]
        junk = hT[:, 0, 0:D]
        ssx = P.sb("ssx", [128, NT], F32)
        lnx = P.sb("lnx", [128, NT], F32)
        rsx = P.sb("rsx", [128, NT], F32)
        ident = P.sb("ident", [128, 128], BF16)
        bd = P.sb("bd", [128, 128], BF16)
        a128 = P.sb("a128", [128, 128], BF16)
        tric = P.sb("tric", [128, 128], BF16)
        trip = P.sb("trip", [128, 128], BF16)
        triu_f = P.sb("triu_b", [128, 128], BF16)
        ones_f = P.sb("ones_b", [128, 128], BF16)
        Lb = P.sb("Lb", [128, 3, 128], BF16)
        cmask = P.sb("cmask", [128, 128], F32)
        gcols = P.sb("gcols", [128, 20], F32)
        bfb = P.sb("bfb", [128, 2, 8], F32)
        lamb = n1t[:, :].rearrange("p (a b c) -> p a b c", a=2, b=4)
        lamw = n2t[:, 0:256].rearrange("p (a b c) -> p a b c", a=2, b=2)
        lams = P.sb("lams", [128, 2, 2], F32)
        lame = P.sb("lame", [128, 2, 2], F32)
        nlam = P.sb("nlam", [128, 2], F32)
        sinkb = P.sb("sinkb", [128, 2, 8], F32)
        esink = P.sb("esink", [128, 2, 8], F32)
        KM = P.sb("KM", [128, 16], BF16)
        kmf = P.sb("kmf", [128, 8], F32)
        kmf2 = P.sb("kmf2", [128, 8], F32)
        Gsb = P.sb("Gsb", [128, 256], F32)
        Gm = P.sb("Gm", [128, 128], F32)
        cmpt = gnb
        rank = P.sb("rank", [128, 128], F32)
        MT = P.sb("MT", [128, 128], BF16)
        zf = P.sb("zf", [128, 128], F32)
        Lf = P.sb("Lf", [128, 128], F32)
        cwt = P.sb("cwt", [128, 256], F32)
        cpre = P.sb("cpre", [128, 128], F32)
        cneg = P.sb("cneg", [128, 128], F32)
        cend = P.sb("cend", [128, 4, 8], F32)
        FB = P.sb("FB", [128, 8, NT, 4], F32)
        Sps = [P.ps("S%d" % i, [128, 512]) for i in range(2)]
        Ops = [P.ps("O%d" % i, [128, 512]) for i in range(3)]
        Mps = [P.ps("M%d" % i, [128, 512]) for i in range(2)]
        Tps = P.ps("Tps", [128, 1024], BF16)
        Sring = Ring([('S%d' % i, Sps[i]) for i in range(2)])
        Oring = Ring([('O%d' % i, Ops[i]) for i in range(3)])
        Mring = Ring([('M%d' % i, Mps[i]) for i in range(2)])
        Pring = Ring([('Pt%d' % i, Pt[i]) for i in range(NPR)])
        sqring = Ring([('sq%d' % i, sqr[i]) for i in range(1)])
        lnring = Ring([('ln%d' % i, lnr[i]) for i in range(2)])
        rdring = Ring([('rd%d' % i, rdr[i]) for i in range(1)])
        rgring = Ring([('rg%d' % i, rgr[i]) for i in range(1)])
        hring = Ring([('hrow%d' % i, hrow[i]) for i in range(1)])
        Oring.i, Pring.i, Sring.i, Mring.i = phase

        P.dma('pool', ident[:], c_ident, 'cst', writes=['ident'])
        P.dma('pool', bd[:], c_bd, 'cst', writes=['bd'])
        P.dma('pool', a128[:], c_a128, 'cst', writes=['a128'])
        P.dma('pool', tric[:], c_tric, 'cst', writes=['tric'])
        P.dma('pool', trip[:], c_trip, 'cst', writes=['trip'])
        P.dma('pool', triu_f[:], c_triu, 'cst', writes=['triu_f'])
        P.dma('sp', cmask[:], c_cmask, 'cst2', writes=['cmask'])
        P.op('dve', lambda e: e.memset(ones_f[:], 1.0), writes=['ones_f'])
        for i in range(2):
            P.op('dve', lambda e, i=i: e.memset(QA[i][:], 0.0), writes=['QA%d' % i, 'QAm%d' % i, 'QAa%d' % i])
            P.op('dve', lambda e, i=i: e.memset(KA[i][:], 0.0), writes=['KA%d' % i, 'KAaug%d' % i])
            P.dma('pool', KA[i][64:76, :], c_kaug, 'cst', reads=[], writes=['KAaug%d' % i])
        gnames = ["moba_q_gain", "moba_k_gain", "fox_q_gain", "fox_k_gain", "diff_q_gain", "diff_k_gain", "swa_q_gain", "swa_k_gain"]
        GC = {}
        col = 0
        P.op('dve', lambda e: e.memset(gcols[:], 0.0), writes=['gcols'])
        for j in range(2):
            for n in gnames:
                GC[(n, j)] = col
                src = hg[n][j].rearrange("(p o) -> p o", o=1)
                P.dma('sp', gcols[0:64, col:col + 1], src, 'cstg', writes=['gcols'])
                P.dma('sp', gcols[64:128, col:col + 1], src, 'cstg', writes=['gcols'])
                col += 1
        for j in range(2):
            GC[('subln', j)] = col
            P.dma('sp', gcols[:, col:col + 1], diff_subln_gain[j].rearrange("(p o) -> p o", o=1), 'cstg', writes=['gcols'])
            col += 1
        P.flush('cstg', ['gcols'])
        for j in range(2):
            for n in gnames:
                if n.endswith("q_gain"):
                    c = GC[(n, j)]
                    P.op('dve', lambda e, c=c: e.tensor_scalar(gcols[:, c:c + 1], gcols[:, c:c + 1], 0.125, None, ALU.mult), reads=['gcols'], writes=['gcols'])
            layer = 2 * j + 1
            lam_init = 0.8 - 0.6 * math.exp(-0.3 * layer)
            c = GC[('subln', j)]
            P.op('dve', lambda e, c=c, v=1.0 - lam_init: e.tensor_scalar(gcols[:, c:c + 1], gcols[:, c:c + 1], float(v), None, ALU.mult), reads=['gcols'], writes=['gcols'])
        P.dma('sp', bfb[:].rearrange("p a b -> p (a b)"), b_forget.rearrange("(o a) b -> o (a b)", o=1).partition_broadcast(128), 'cst2', writes=['bfb'])
        P.dma('sp', sinkb[:].rearrange("p a b -> p (a b)"), swa_sinks.rearrange("(o a) b -> o (a b)", o=1).partition_broadcast(128), 'cst4', writes=['sinkb'])
        P.dma('sp', n1t[:, :], diff_lambda.rearrange("(o a) b c -> o (a b c)", o=1).partition_broadcast(128), 'cst3', writes=['n1t'])
        P.op('act', lambda e: e.activation(esink[:], sinkb[:], AF.Exp), reads=['sinkb'], writes=['esink'])
        P.op('dve', lambda e: e.tensor_tensor(lamw, bview(lamb[:, 0, 0, :], [[256, 2], [128, 2], [1, 64]]),
                                              bview(lamb[:, 0, 1, :], [[256, 2], [128, 2], [1, 64]]), ALU.mult), reads=['n1t'], writes=['n2t'])
        P.op('dve', lambda e: e.tensor_reduce(lams[:], lamw, AX.X, ALU.add), reads=['n2t'], writes=['lams'])
        P.op('act', lambda e: e.activation(lame[:], lams[:], AF.Exp), reads=['lams'], writes=['lame'])
        for j in range(2):
            lam_init = 0.8 - 0.6 * math.exp(-0.3 * (2 * j + 1))
            P.op('dve', lambda e, j=j, li=lam_init: e.scalar_tensor_tensor(nlam[:, j:j + 1], lame[:, j, 1:2], float(-li), lame[:, j, 0:1], ALU.add, ALU.subtract),
                 reads=['lame'], writes=['nlam'])

        P.flush('cst', ['ident', 'bd', 'a128', 'tric', 'trip', 'KAaug0', 'KAaug1', 'triu_f'])
        P.flush('cst2', ['cmask', 'bfb'])
        wstate = {'n': 0}

        def wload(parts):
            i = wstate['n'] % NW
            wstate['n'] += 1
            name = 'wb%d' % i
            for src, off in parts:
                wdt = src.shape[1]
                P.dma('pool', wbuf[i][:, :, off:off + wdt], src.rearrange("(k p) c -> p k c", p=128), 'w%d' % i, writes=[name])
            return name, wbuf[i]

        def rms_to_hT(l):
            P.dma('sp', gnb[:], norm_gain[l:l + 1, :].partition_broadcast(128), 'gnb', writes=['gnb'])
            for i in range(NT):
                P.op('act', lambda e, i=i: e.activation(junk, x_sb[:, i, :], AF.Square, accum_out=ssx[:, i:i + 1]), reads=['x%d' % i], writes=['hT', 'ssx'])
            P.op('act', lambda e: e.activation(lnx[:], ssx[:], AF.Ln, bias=EPS, scale=1.0 / D), reads=['ssx'], writes=['lnx'])
            P.op('act', lambda e: e.activation(rsx[:], lnx[:], AF.Exp, scale=-0.5), reads=['lnx'], writes=['rsx'])
            for i in range(NT):
                hn, ht = hring.next()
                P.op('dve', lambda e, i=i, ht=ht: e.scalar_tensor_tensor(ht[:], x_sb[:, i, :], rsx[:, i:i + 1], gnb[:], ALU.mult, ALU.mult),
                     reads=['x%d' % i, 'rsx', 'gnb'], writes=[hn])
                for k in range(8):
                    P.op('pe', lambda e, k=k, ht=ht: e.transpose(Tps[:, k * 128:(k + 1) * 128], ht[:, k * 128:(k + 1) * 128], ident[:]),
                         reads=[hn, 'ident'], writes=['Tps'])
                eng = 'dve' if i % 2 == 0 else 'act'
                src = Tps[:, :].rearrange("p (k t) -> p k t", k=8)
                dst = hT[:, :, i * 128:(i + 1) * 128]
                if eng == 'dve':
                    P.op('dve', lambda e, src=src, dst=dst: e.tensor_copy(dst, src), reads=['Tps'], writes=['hT'])
                else:
                    P.op('act', lambda e, src=src, dst=dst: e.copy(dst, src), reads=['Tps'], writes=['hT'])

        def inproj_fm(wname, wt, woff, tc, Mn, Mt, M=128):
            for k in range(8):
                P.op('pe', lambda e, k=k: e.matmul(Mt[0:M, :], wt[:, k, woff:woff + M], hT[:, k, tc * 512:(tc + 1) * 512], start=(k == 0), stop=(k == 7)),
                     reads=[wname, 'hT'], writes=[Mn])

        def gates(W, gcols_list):
            for blk in range(4):
                wname, wt = wload([(W[:, gcols_list[2 * blk]:gcols_list[2 * blk] + 128], 0), (W[:, gcols_list[2 * blk + 1]:gcols_list[2 * blk + 1] + 128], 128)])
                for c in range(2):
                    p = 2 * blk + c
                    for tc in range(4):
                        Mn, Mt = Mring.next()
                        inproj_fm(wname, wt, c * 128, tc, Mn, Mt)
                        P.op('act', lambda e, Mt=Mt, p=p, tc=tc: e.activation(yT[:, p, tc * 512:(tc + 1) * 512], Mt[:], AF.Silu), reads=[Mn], writes=['yT%d' % p])

        def qk_pair(wname, wt, woff, gcol, dst, dstnames):
            for tc in range(4):
                Mn, Mt = Mring.next()
                inproj_fm(wname, wt, woff, tc, Mn, Mt)
                sn, sq = sqring.next()
                P.op('act', lambda e, Mt=Mt, sq=sq: e.activation(sq[:], Mt[:], AF.Square), reads=[Mn], writes=[sn])
                Sn, St = Sring.next()
                P.op('pe', lambda e, St=St, sq=sq: e.matmul(St[:], bd[:], sq[:], start=True, stop=True), reads=[sn, 'bd'], writes=[Sn])
                ln_n, ln_t = lnring.next()
                P.op('act', lambda e, St=St, ln_t=ln_t: e.activation(ln_t[:], St[:], AF.Ln, bias=EPS), reads=[Sn], writes=[ln_n])
                rn, rt = ln_n, ln_t
                P.op('act', lambda e, ln_t=ln_t: e.activation(ln_t[:], ln_t[:], AF.Exp, scale=-0.5), reads=[ln_n], writes=[ln_n])
                cs = slice(tc * 512, (tc + 1) * 512)
                P.op('dve', lambda e, Mt=Mt, rt=rt, cs=cs: e.scalar_tensor_tensor(dst[0][0:64, cs], Mt[0:64, :], gcols[0:64, gcol:gcol + 1], rt[0:64, :], ALU.mult, ALU.mult),
                     reads=[Mn, rn, 'gcols'], writes=[dstnames[0]])
                P.op('dve', lambda e, Mt=Mt, rt=rt, cs=cs: e.scalar_tensor_tensor(dst[1][0:64, cs], Mt[64:128, :], gcols[64:128, gcol:gcol + 1], rt[64:128, :], ALU.mult, ALU.mult),
                     reads=[Mn, rn, 'gcols'], writes=[dstnames[1]])

        def load_alibi(hslot, sidx):
            P.dma('pool', QA[hslot][72:76, :], c_qalibi[sidx], 'qal%d' % hslot, writes=['QAa%d' % hslot])

        def dense_plan():
            plan = []
            for g in range(4):
                lst = []
                for j in range(4 * g + 4):
                    r = j - 4 * g
                    c0 = max(r, 0) * 128
                    masks = [(tric, 'tric', c0)] if r >= 0 else []
                    lst.append((j, c0, 512, masks))
                plan.append(lst)
            return plan

        def swa_plan():
            plan = []
            for g in range(4):
                lst = []
                for j in range(max(0, 4 * g - 1), 4 * g + 4):
                    masks = []
                    cols = []
                    for i, tab, tn in ((j, tric, 'tric'), (j + 1, trip, 'trip')):
                        if 4 * g <= i <= 4 * g + 3:
                            c = (i - 4 * g) * 128
                            masks.append((tab, tn, c))
                            cols.append(c)
                    lst.append((j, min(cols), max(cols) + 128, masks))
                plan.append(lst)
            return plan

        DENSE = dense_plan()
        SWA = swa_plan()

        def attn_group(qslot, kslot, g, plan_g, lhsT_fns, bias_fn, qreads, kreads, vreads):
            Os = [Oring.next() for _ in lhsT_fns]
            first = [True] * len(lhsT_fns)
            for (j, c0, c1, masks) in plan_g:
                Sn, St = Sring.next()
                fm = True
                for tab, tn, c in masks:
                    P.op('pe', lambda e, St=St, tab=tab, c=c, fm=fm: e.matmul(St[:, c:c + 128], ident[:], tab[:], start=fm, stop=False, skip_group_check=True),
                         reads=['ident', tn], writes=[Sn])
                    fm = False
                P.op('pe', lambda e, St=St, j=j, c0=c0, c1=c1, fm=fm: e.matmul(St[:, c0:c1], KA[kslot][0:KR, j * 128:(j + 1) * 128],
                                                                              QA[qslot][0:KR, g * 512 + c0:g * 512 + c1], start=fm, stop=True, skip_group_check=True),
                     reads=qreads + kreads, writes=[Sn])
                Pn, Ptile = Pring.next()
                b = bias_fn(j, g) if bias_fn is not None else None
                if b is None:
                    P.op('act', lambda e, St=St, Ptile=Ptile, c0=c0, c1=c1: e.activation(Ptile[:, c0:c1], St[:, c0:c1], AF.Exp), reads=[Sn], writes=[Pn])
                else:
                    bap, bname = b
                    P.op('act', lambda e, St=St, Ptile=Ptile, c0=c0, c1=c1, bap=bap: e.activation(Ptile[:, c0:c1], St[:, c0:c1], AF.Exp, bias=bap), reads=[Sn, bname], writes=[Pn])
                for v, lf in enumerate(lhsT_fns):
                    On, Ot = Os[v]
                    P.op('pe', lambda e, Ot=Ot, lf=lf, j=j, c0=c0, c1=c1, Ptile=Ptile, f=first[v]: e.matmul(Ot[:, c0:c1], lf(j), Ptile[:, c0:c1], start=f, stop=False, skip_group_check=True),
                         reads=[Pn] + vreads, writes=[On])
                    first[v] = False
            return Os

        def finalize_std(O, p, hf, g, extra_den=None):
            On, Ot = O
            b0 = 64 * hf
            cs = slice(g * 512, (g + 1) * 512)
            rn, rt = rdring.next()
            if extra_den is not None:
                P.op('dve', lambda e: e.tensor_scalar(rt[b0:b0 + 64, :], Ot[64:128, :], extra_den, None, ALU.add), reads=[On, 'esink'], writes=[rn])
                P.op('dve', lambda e: e.reciprocal(rt[b0:b0 + 64, :], rt[b0:b0 + 64, :]), reads=[rn], writes=[rn])
            else:
                P.op('dve', lambda e: e.reciprocal(rt[b0:b0 + 64, :], Ot[64:128, :]), reads=[On], writes=[rn])
            gn, gt = rgring.next()
            P.op('pool', lambda e: e.tensor_tensor(gt[b0:b0 + 64, :], rt[b0:b0 + 64, :], yT[b0:b0 + 64, p, cs], ALU.mult), reads=[rn, 'yT%d' % p], writes=[gn])
            P.op('dve', lambda e: e.tensor_tensor(yT[b0:b0 + 64, p, cs], Ot[0:64, :], gt[b0:b0 + 64, :], ALU.mult), reads=[On, gn], writes=['yT%d' % p])

        def v_block(W, vbase, ncols, layout):
            wname, wt = wload([(W[:, vbase:vbase + ncols], 0)])
            for i in range(NT):
                Mn, Mt = Mring.next()
                for k in range(8):
                    P.op('pe', lambda e, k=k, i=i, Mt=Mt: e.matmul(Mt[:, 0:ncols], hT[:, k, i * 128:(i + 1) * 128], wt[:, k, 0:ncols], start=(k == 0), stop=(k == 7)),
                         reads=[wname, 'hT'], writes=[Mn])
                nh, w0, stride_dst, dst0 = layout
                src = Mt[:, 0:ncols].rearrange("p (h d) -> p h d", h=nh)
                dstv = bview(VA[:, i, dst0:dst0 + 1], [[stride_dst, nh], [1, w0]])
                if i % 2 == 0:
                    P.op('dve', lambda e, src=src, dstv=dstv: e.tensor_copy(dstv, src), reads=[Mn], writes=['VA'])
                else:
                    P.op('act', lambda e, src=src, dstv=dstv: e.copy(dstv, src), reads=[Mn], writes=['VA'])

        def set_va_ones(regions):
            for (c0, strd, n, wdt) in regions:
                v = bview(VA[:, 0, c0:c0 + 1], [[512, NT], [strd, n], [1, wdt]])
                P.op('pool', lambda e, v=v: e.memset(v, 1.0), writes=['VA'])

        def moba_masks(hs):
            qn, kn = 'QA%d' % hs, 'KA%d' % hs
            P.op('dve', lambda e: e.tensor_reduce(kmf[0:64, :], KA[hs][0:64, :].rearrange("p (n l) -> p n l", n=8), AX.X, ALU.add), reads=[kn], writes=['kmf'])
            P.op('dve', lambda e: e.tensor_copy(KM[0:64, 0:8], kmf[0:64, :]), reads=['kmf'], writes=['KM'])
            P.op('dve', lambda e: e.tensor_tensor(kmf2[0:64, :], kmf[0:64, :], KM[0:64, 0:8], ALU.subtract), reads=['kmf', 'KM'], writes=['kmf2'])
            P.op('dve', lambda e: e.tensor_copy(KM[0:64, 8:16], kmf2[0:64, :]), reads=['kmf2'], writes=['KM'])
            Mn, Mt = Mring.next()
            for i in range(NT):
                P.op('pe', lambda e, i=i, Mt=Mt: e.matmul(Mt[:, i * 16:(i + 1) * 16], QA[hs][0:64, i * 128:(i + 1) * 128], KM[0:64, :], start=(i == 0), stop=(i == NT - 1), skip_group_check=True),
                     reads=[qn, 'KM'], writes=[Mn])
            P.op('act', lambda e, Mt=Mt: e.copy(Gsb[:], Mt[:, 0:256]), reads=[Mn], writes=['Gsb'])
            gv = Gsb[:].rearrange("p (i c) -> p i c", c=16)
            P.op('dve', lambda e: e.tensor_tensor(Gm[:].rearrange("p (i n) -> p i n", n=8), gv[:, :, 0:8], gv[:, :, 8:16], ALU.add), reads=['Gsb'], writes=['Gm'])
            P.op('dve', lambda e: e.tensor_tensor(Gm[:], Gm[:], cmask[:], ALU.add), reads=['Gm', 'cmask'], writes=['Gm'])
            in0 = bview(Gm[:, 0:1], [[8, NT], [0, 8], [1, 8]])
            in1 = bview(Gm[:, 0:1], [[8, NT], [1, 8], [0, 8]])
            P.op('dve', lambda e: e.tensor_tensor(cmpt[:].rearrange("p (i n m) -> p i n m", n=8, m=8), in0, in1, ALU.is_gt), reads=['Gm'], writes=['gnb'])
            P.op('dve', lambda e: e.tensor_reduce(rank[:].rearrange("p (i n) -> p i n", n=8), cmpt[:].rearrange("p (i n m) -> p i n m", n=8, m=8), AX.X, ALU.add), reads=['gnb'], writes=['rank'])
            P.op('dve', lambda e: e.tensor_scalar(MT[:], rank[:], 3.5, NEG, ALU.is_gt, ALU.mult), reads=['rank'], writes=['MT'])
            for half in range(2):
                for ii in range(8):
                    i = half * 8 + ii
                    P.op('pe', lambda e, i=i, ii=ii: e.transpose(Tps[0:8, ii * 128:(ii + 1) * 128], MT[:, i * 8:(i + 1) * 8], ident[:]), reads=['MT', 'ident'], writes=['Tps'])
                P.op('act', lambda e, half=half: e.copy(QA[hs][64:72, half * 1024:(half + 1) * 1024], Tps[0:8, :]), reads=['Tps'], writes=['QAm%d' % hs])

        def fox_prep(W, j):
            wname, wt = wload([(W[:, 4096:4104], 0)])
            Mn, Mt = Mring.next()
            for i in range(NT):
                for k in range(8):
                    P.op('pe', lambda e, i=i, k=k, Mt=Mt: e.matmul(Mt[:, i * 8:(i + 1) * 8], hT[:, k, i * 128:(i + 1) * 128], wt[:, k, 0:8],
                                                                start=(i == 0 and k == 0), stop=(i == NT - 1 and k == 7), skip_group_check=True),
                         reads=[wname, 'hT'], writes=[Mn])
            bb = bview(bfb[:, j, 0:1], [[0, NT], [1, 8]])
            P.op('dve', lambda e, Mt=Mt: e.tensor_tensor(zf[:].rearrange("p (i h) -> p i h", h=8), Mt[:, 0:128].rearrange("p (i h) -> p i h", h=8), bb, ALU.add), reads=[Mn, 'bfb'], writes=['zf'])
            P.op('act', lambda e: e.activation(zf[:], zf[:], AF.Exp, scale=-1.0), reads=['zf'], writes=['zf'])
            P.op('act', lambda e: e.activation(Lf[:], zf[:], AF.Ln, bias=1.0), reads=['zf'], writes=['Lf'])
            Mn2, Mt2 = Mring.next()
            P.op('dve', lambda e: e.tensor_copy(Lb[:, 0, :], Lf[:]), reads=['Lf'], writes=['Lb'])
            P.op('dve', lambda e: e.tensor_tensor(zf[:], Lf[:], Lb[:, 0, :], ALU.subtract), reads=['Lf', 'Lb'], writes=['zf'])
            P.op('dve', lambda e: e.tensor_copy(Lb[:, 1, :], zf[:]), reads=['zf'], writes=['Lb'])
            P.op('dve', lambda e: e.tensor_tensor(Lf[:], zf[:], Lb[:, 1, :], ALU.subtract), reads=['zf', 'Lb'], writes=['Lf'])
            P.op('dve', lambda e: e.tensor_copy(Lb[:, 2, :], Lf[:]), reads=['Lf'], writes=['Lb'])
            for c in range(3):
                P.op('pe', lambda e, c=c: e.matmul(Mt2[:, 0:128], triu_f[:], Lb[:, c, :], start=(c == 0), stop=(c == 2), skip_group_check=True), reads=['triu_f', 'Lb'], writes=[Mn2])
            for c in range(3):
                P.op('pe', lambda e, c=c: e.matmul(Mt2[:, 128:256], ones_f[:], Lb[:, c, :], start=False, stop=(c == 2), skip_group_check=True), reads=['ones_f', 'Lb'], writes=[Mn2])
            P.op('act', lambda e: e.copy(cwt[:], Mt2[:, 0:256]), reads=[Mn2], writes=['cwt'])
            P.op('dve', lambda e: e.memset(cpre[:, 0:8], 0.0), writes=['cpre'])
            for i in range(1, NT):
                P.op('dve', lambda e, i=i: e.tensor_tensor(cpre[:, i * 8:(i + 1) * 8], cpre[:, (i - 1) * 8:i * 8], cwt[:, 128 + (i - 1) * 8:128 + i * 8], ALU.add),
                     reads=['cpre', 'cwt'], writes=['cpre'])
            P.op('dve', lambda e: e.tensor_tensor(cneg[:], cwt[:, 0:128], cpre[:], ALU.add), reads=['cwt', 'cpre'], writes=['cneg'])
            for g in range(4):
                i = 4 * g + 3
                P.op('dve', lambda e, g=g, i=i: e.tensor_tensor(cend[:, g, :], cpre[:, i * 8:(i + 1) * 8], cwt[:, 128 + i * 8:128 + (i + 1) * 8], ALU.add), reads=['cpre', 'cwt'], writes=['cend'])
            for g in range(4):
                outv = bview(FB[:, 0, 0, g:g + 1], [[NT * 4, 8], [4, NT]])
                in0 = bview(cneg[:, 0:1], [[1, 8], [8, NT]])
                in1 = bview(cend[:, g, 0:1], [[1, 8], [0, NT]])
                P.op('dve', lambda e, outv=outv, in0=in0, in1=in1: e.tensor_tensor(outv, in0, in1, ALU.subtract), reads=['cneg', 'cend'], writes=['FB'])

        def layer_ab(l):
            j = l // 2
            W = w_in_ab[j]
            rms_to_hT(l)
            gates(W, [1536 + 128 * p for p in range(4)] + [3584 + 128 * p for p in range(4)])
            set_va_ones([(64, 128, 4, 64)])
            for mixer in range(2):
                if l == 2 and ('m%d' % mixer) in SKIP:
                    continue
                qb, kb, vb = (0, 512, 1024) if mixer == 0 else (2048, 2560, 3072)
                gq = GC[("moba_q_gain" if mixer == 0 else "fox_q_gain", j)]
                gk = GC[("moba_k_gain" if mixer == 0 else "fox_k_gain", j)]
                if mixer == 1:
                    fox_prep(W, j)
                for sbt in range(2):
                    v_block(W, vb + 256 * sbt, 256, (4, 64, 128, 0))
                    for pp in range(2):
                        pair = 2 * sbt + pp
                        wname, wt = wload([(W[:, qb + 128 * pair:qb + 128 * pair + 128], 0), (W[:, kb + 128 * pair:kb + 128 * pair + 128], 128)])
                        qk_pair(wname, wt, 0, gq, QA, ['QA0', 'QA1'])
                        qk_pair(wname, wt, 128, gk, KA, ['KA0', 'KA1'])
                        for hs in range(2):
                            h = 2 * pair + hs
                            if mixer == 0:
                                load_alibi(hs, h + 1)
                                moba_masks(hs)
                                bias_fn = None
                            else:
                                load_alibi(hs, 0)
                                if pair == 0:
                                    P.op('dve', lambda e, hs=hs: e.memset(QA[hs][64:72, :], 0.0), writes=['QAm%d' % hs])
                                bias_fn = (lambda jj, g, h=h: (FB[:, h, jj, g:g + 1], 'FB'))
                            hv = 2 * pp + hs
                            lf = lambda jj, hv=hv: VA[:, jj, hv * 128:(hv + 1) * 128]
                            for g in range(4):
                                Os = attn_group(hs, hs, g, DENSE[g], [lf], bias_fn, ['QA%d' % hs, 'QAm%d' % hs, 'QAa%d' % hs], ['KA%d' % hs, 'KAaug%d' % hs], ['VA'])
                                finalize_std(Os[0], 4 * mixer + pair, hs, g)
            out_proj(w_out_ab[j])

        def layer_cd(l):
            j = l // 2
            W = w_in_cd[j]
            rms_to_hT(l)
            gates(W, [1536 + 128 * p for p in range(4)] + [2816 + 128 * p for p in range(4)])
            set_va_ones([(64, 256, 2, 128)])
            gq, gk = GC[("diff_q_gain", j)], GC[("diff_k_gain", j)]
            for hs in range(2):
                P.op('dve', lambda e, hs=hs: e.memset(QA[hs][64:72, :], 0.0), writes=['QAm%d' % hs])
            sub_c = GC[('subln', j)]
            for sbt in range(2):
                wname, wt = wload([(W[:, 1024 + 256 * sbt:1024 + 256 * sbt + 256], 0)])
                for i in range(NT):
                    Mn, Mt = Mring.next()
                    for k in range(8):
                        P.op('pe', lambda e, k=k, i=i, Mt=Mt, wt=wt: e.matmul(Mt[:, 0:256], hT[:, k, i * 128:(i + 1) * 128], wt[:, k, 0:256], start=(k == 0), stop=(k == 7)),
                             reads=[wname, 'hT'], writes=[Mn])
                    src = Mt[:, 0:256].rearrange("p (h c d) -> p h c d", h=2, c=2)
                    dstv = bview(VA[:, i, 0:1], [[256, 2], [192, 2], [1, 64]])
                    if i % 2 == 0:
                        P.op('dve', lambda e, src=src, dstv=dstv: e.tensor_copy(dstv, src), reads=[Mn], writes=['VA'])
                    else:
                        P.op('act', lambda e, src=src, dstv=dstv: e.copy(dstv, src), reads=[Mn], writes=['VA'])
                for pp in range(2):
                    h = 2 * sbt + pp
                    wname, wt = wload([(W[:, 128 * h:128 * h + 128], 0), (W[:, 512 + 128 * h:512 + 128 * h + 128], 128)])
                    qk_pair(wname, wt, 0, gq, QA, ['QA0', 'QA1'])
                    qk_pair(wname, wt, 128, gk, KA, ['KA0', 'KA1'])
                    for c in range(2):
                        load_alibi(c, 2 * h + 2)
                    lfs = [lambda jj, pp=pp: VA[:, jj, pp * 256:pp * 256 + 128], lambda jj, pp=pp: VA[:, jj, pp * 256 + 128:pp * 256 + 256]]
                    for g in range(4):
                        cs = slice(g * 512, (g + 1) * 512)
                        for c in range(2):
                            Os = attn_group(c, c, g, DENSE[g], lfs, None, ['QA%d' % c, 'QAm%d' % c, 'QAa%d' % c], ['KA%d' % c, 'KAaug%d' % c], ['VA'])
                            (On0, Ot0), (On1, Ot1) = Os
                            rn, rt = rdring.next()
                            P.op('dve', lambda e, rt=rt, Ot0=Ot0: e.reciprocal(rt[0:64, :], Ot0[64:128, :]), reads=[On0], writes=[rn])
                            P.op('dve', lambda e, rt=rt, Ot1=Ot1: e.reciprocal(rt[64:128, :], Ot1[0:64, :]), reads=[On1], writes=[rn])
                            dstt = n1t if c == 0 else n2t
                            dn = 'n1t' if c == 0 else 'n2t'
                            P.op('dve', lambda e, rt=rt, Ot0=Ot0, dstt=dstt: e.tensor_tensor(dstt[0:64, :], Ot0[0:64, :], rt[0:64, :], ALU.mult), reads=[On0, rn], writes=[dn])
                            P.op('dve', lambda e, rt=rt, Ot1=Ot1, dstt=dstt: e.tensor_tensor(dstt[64:128, :], Ot1[64:128, :], rt[64:128, :], ALU.mult), reads=[On1, rn], writes=[dn])
                        P.op('dve', lambda e: e.scalar_tensor_tensor(n1t[:], n2t[:], nlam[:, j:j + 1], n1t[:], ALU.mult, ALU.add), reads=['n1t', 'n2t', 'nlam'], writes=['n1t'])
                        sn, sq = sqring.next()
                        P.op('act', lambda e, sq=sq: e.activation(sq[:], n1t[:], AF.Square), reads=['n1t'], writes=[sn])
                        Sn, St = Sring.next()
                        P.op('pe', lambda e, St=St, sq=sq: e.matmul(St[:], a128[:], sq[:], start=True, stop=True), reads=[sn, 'a128'], writes=[Sn])
                        ln_n, ln_t = lnring.next()
                        P.op('act', lambda e, St=St, ln_t=ln_t: e.activation(ln_t[:], St[:], AF.Ln, bias=EPS), reads=[Sn], writes=[ln_n])
                        rn2, rt2 = ln_n, ln_t
                        P.op('act', lambda e, ln_t=ln_t: e.activation(ln_t[:], ln_t[:], AF.Exp, scale=-0.5), reads=[ln_n], writes=[ln_n])
                        gn, gt = rgring.next()
                        P.op('pool', lambda e, gt=gt, rt2=rt2, cs=cs, h=h: e.tensor_tensor(gt[:], rt2[:], yT[:, h, cs], ALU.mult), reads=[rn2, 'yT%d' % h], writes=[gn])
                        P.op('dve', lambda e, gt=gt, cs=cs, h=h: e.scalar_tensor_tensor(yT[:, h, cs], n1t[:], gcols[:, sub_c:sub_c + 1], gt[:], ALU.mult, ALU.mult),
                             reads=['n1t', gn, 'gcols'], writes=['yT%d' % h])
            set_va_ones([(64, 128, 2, 64)])
            gq, gk = GC[("swa_q_gain", j)], GC[("swa_k_gain", j)]
            v_block(W, 2688, 128, (2, 64, 128, 0))
            wname, wt = wload([(W[:, 2560:2688], 0)])
            qk_pair(wname, wt, 0, gk, KA, ['KA0', 'KA1'])
            for blk in range(2):
                wname, wt = wload([(W[:, 2048 + 256 * blk:2048 + 256 * blk + 256], 0)])
                for pp in range(2):
                    pair = 2 * blk + pp
                    qk_pair(wname, wt, 128 * pp, gq, QA, ['QA0', 'QA1'])
                    kv = pair // 2
                    for hs in range(2):
                        hq = 2 * pair + hs
                        load_alibi(hs, hq + 1)
                        lf = lambda jj, kv=kv: VA[:, jj, kv * 128:(kv + 1) * 128]
                        for g in range(4):
                            Os = attn_group(hs, kv, g, SWA[g], [lf], None, ['QA%d' % hs, 'QAm%d' % hs, 'QAa%d' % hs], ['KA%d' % kv, 'KAaug%d' % kv], ['VA'])
                            finalize_std(Os[0], 4 + pair, hs, g, extra_den=esink[64:128, j, hq:hq + 1])
            out_proj(w_out_cd[j])

        def out_proj(Wo):
            for blk in range(4):
                wname, wt = wload([(Wo[:, 256 * blk:256 * blk + 256], 0)])
                for i in range(NT):
                    Mn, Mt = Mring.next()
                    for k in range(8):
                        P.op('pe', lambda e, k=k, i=i, Mt=Mt, wt=wt: e.matmul(Mt[:, 0:256], yT[:, k, i * 128:(i + 1) * 128], wt[:, k, :], start=(k == 0), stop=(k == 7)),
                             reads=[wname] + ['yT%d' % k for k in range(8)] if k == 0 else [wname], writes=[Mn])
                    xs = x_sb[:, i, 256 * blk:256 * blk + 256]
                    P.op('dve', lambda e, xs=xs, Mt=Mt: e.tensor_tensor(xs, xs, Mt[:, 0:256], ALU.add), reads=[Mn, 'x%d' % i], writes=['x%d' % i])

        for s in range(nseq):
            for i in range(NT):
                P.dma('sp', x_sb[:, i, :], xin[s, i * 128:(i + 1) * 128, :], 'xld%d' % i, reads=[], writes=['x%d' % i])
            for l in layers:
                if l % 2 == 0:
                    layer_ab(l)
                else:
                    layer_cd(l)
            for i in range(NT):
                P.dma('sp', yout[s, i * 128:(i + 1) * 128, :], x_sb[:, i, :], 'xst%d' % i, reads=['x%d' % i], writes=['out%d' % i])
        P.wait_all('sp', ['out%d' % i for i in range(NT)])
        P.emit()
    return nc


def make_consts():
    c = {}
    c["c_ident"] = np.eye(128, dtype=np.float32)
    bdm = np.zeros((128, 128), np.float32)
    bdm[0:64, 0:64] = 1.0 / 64
    bdm[64:128, 64:128] = 1.0 / 64
    c["c_bd"] = bdm
    c["c_a128"] = np.full((128, 128), 1.0 / 128, np.float32)
    s = np.arange(128)[:, None]
    t = np.arange(128)[None, :]
    c["c_tric"] = np.where(s <= t, 0.0, NEG).astype(np.float32)
    c["c_trip"] = np.where(s > t, 0.0, NEG).astype(np.float32)
    c["c_triu"] = (s <= t).astype(np.float32)
    pos = np.arange(S)
    kaug = np.zeros((12, S), np.float32)
    for n in range(8):
        kaug[n] = (pos // 256 == n)
    kaug[8] = 128 * (pos // 128)
    kaug[9] = pos % 128
    kaug[10] = 1.0
    kaug[11] = 1.0
    c["c_kaug"] = kaug
    qal = np.zeros((9, 4, S), np.float32)
    for i in range(1, 9):
        sl = 2.0 ** (-i)
        qal[i, 0] = sl
        qal[i, 1] = sl
        qal[i, 2] = -sl * 128 * (pos // 128)
        qal[i, 3] = -sl * (pos % 128)
    c["c_qalibi"] = qal
    cm = np.zeros((128, 16, 8), np.float32)
    for i in range(16):
        qb = i // 2
        for n in range(8):
            cm[:, i, n] = 0.0 if n < qb else (1e30 if n == qb else -1e30)
    c["c_cmask"] = cm.reshape(128, 128)
    return c


PARAMS = ["norm_gain", "w_in_ab", "b_forget", "moba_q_gain", "moba_k_gain", "fox_q_gain", "fox_k_gain", "w_out_ab", "w_in_cd",
          "diff_q_gain", "diff_k_gain", "diff_lambda", "diff_subln_gain", "swa_q_gain", "swa_k_gain", "swa_sinks", "w_out_cd"]


LAUNCH_GROUPS = [(0, 1, 2, 3)]


def kernel(**inputs):
    x = np.ascontiguousarray(np.asarray(inputs["x"], dtype=np.float32))
    n = 8
    per = x.shape[0] // n
    consts = make_consts()
    base = {k: np.ascontiguousarray(np.asarray(inputs[k], dtype=np.float32)) for k in PARAMS}
    base.update(consts)
    cur = x
    for grp in LAUNCH_GROUPS:
        nc = build(nseq=per, layers=grp)
        in_maps = []
        for c in range(n):
            m = dict(base)
            m["xin"] = np.ascontiguousarray(cur[c * per:(c + 1) * per])
            in_maps.append(m)
        res = run_bass_kernel_spmd(nc, in_maps, core_ids=list(range(n)))
        cur = np.concatenate([r["yout"] for r in res.results], axis=0)
    return cur
```

```python
import math
import numpy as np
from contextlib import ExitStack
import concourse.bass as bass
import concourse.mybir as mybir
from concourse.ap import AP
from concourse.bass_utils import run_bass_kernel_spmd

F32 = mybir.dt.float32
BF16 = mybir.dt.bfloat16
ALU = mybir.AluOpType
AF = mybir.ActivationFunctionType
AX = mybir.AxisListType

ENGS = ['pe', 'act', 'dve', 'pool', 'sp']
S = 2048
D = 1024
NT = 16
KR = 76
EPS = 1e-6
NEG = -30000.0


class Prog:
    def __init__(self, nc, stack):
        self.nc = nc
        self.stack = stack
        self.q = {k: [] for k in ENGS}
        self.cnt = {}
        self.sems = {}
        self.seen = {k: {} for k in ENGS}
        self.w = {}
        self.r = {}
        for k in ENGS:
            self._sem('c_' + k)

    def _sem(self, name):
        if name not in self.sems:
            self.sems[name] = self.stack.enter_context(self.nc.semaphore(name))
            self.cnt[name] = 0
        return self.sems[name]

    def sb(self, name, shape, dt):
        return self.stack.enter_context(self.nc.sbuf_tensor(name, shape, dt))

    def ps(self, name, shape, dt=F32):
        return self.stack.enter_context(self.nc.psum_tensor(name, shape, dt))

    def _deps(self, eng, reads, writes):
        deps = {}

        def add(sig):
            if sig is None:
                return
            s, v = sig
            if deps.get(s, 0) < v:
                deps[s] = v
        for r in reads:
            add(self.w.get(r))
        for w_ in writes:
            add(self.w.get(w_))
            for sig in self.r.get(w_, ()):
                add(sig)
        out = []
        own = 'c_' + eng
        for s, v in deps.items():
            if eng == 'pe' and s == own:
                continue
            if self.seen[eng].get(s, 0) < v:
                self.seen[eng][s] = v
                out.append((s, v))
        return out

    def _record(self, sig, reads, writes):
        for r in reads:
            lst = self.r.setdefault(r, [])
            lst[:] = [x for x in lst if x[0] != sig[0]] + [sig]
        for w_ in writes:
            self.w[w_] = sig
            self.r[w_] = []

    def op(self, eng, fn, reads=(), writes=()):
        waits = self._deps(eng, reads, writes)
        name = 'c_' + eng
        self.cnt[name] += 1
        sig = (name, self.cnt[name])
        self.q[eng].append((waits, fn, name, 1))
        self._record(sig, reads, writes)
        return sig

    def dma(self, eng, out, in_, sem, reads=(), writes=()):
        waits = self._deps(eng, reads, writes)
        self._sem(sem)
        self.cnt[sem] += 16
        sig = (sem, self.cnt[sem])
        self.q[eng].append((waits, lambda e: e.dma_start(out=out, in_=in_), sem, 16))
        self._record(sig, reads, writes)
        return sig

    def flush(self, sem, resources):
        for r in resources:
            self.w[r] = (sem, self.cnt[sem])

    def wait_all(self, eng, resources):
        waits = self._deps(eng, resources, ())
        self.q[eng].append((waits, None, None, 0))

    def emit(self):
        nc = self.nc
        names = {'pe': 'tensor', 'act': 'scalar', 'dve': 'vector', 'pool': 'gpsimd', 'sp': 'sync'}
        with nc.Block() as block:
            for k in ENGS:
                lst = self.q[k]
                if not lst:
                    continue

                def body(e, lst=lst):
                    for waits, fn, sname, inc in lst:
                        for s, v in waits:
                            e.wait_ge(self.sems[s], v)
                        if fn is not None:
                            fn(e).then_inc(self.sems[sname], inc)
                getattr(block, names[k])(body)


class Ring:
    def __init__(self, items):
        self.items = items
        self.i = 0

    def next(self):
        it = self.items[self.i % len(self.items)]
        self.i += 1
        return it


def alibi_slopes(n):
    return [2.0 ** (-8.0 * (i + 1) / n) for i in range(n)]


def bview(ap, pattern):
    p = ap.ap[0]
    return AP(ap.tensor, ap.offset, [[p[0], p[1]]] + [list(x) for x in pattern])


SKIP = set()


def build(nseq=2, layers=(0, 1, 2, 3), dbg=False, phase=(0, 0, 0, 0)):
    nc = bass.Bass("TRN2", target_bir_lowering=False)
    dt_in = lambda name, shape: nc.dram_tensor(name, list(shape), F32, kind="ExternalInput").ap()
    xin = dt_in("xin", [nseq, S, D])
    norm_gain = dt_in("norm_gain", [4, D])
    w_in_ab = dt_in("w_in_ab", [2, D, 4104])
    b_forget = dt_in("b_forget", [2, 8])
    w_out_ab = dt_in("w_out_ab", [2, D, D])
    w_in_cd = dt_in("w_in_cd", [2, D, 3328])
    w_out_cd = dt_in("w_out_cd", [2, D, D])
    hg = {n: dt_in(n, [2, 64]) for n in ["moba_q_gain", "moba_k_gain", "fox_q_gain", "fox_k_gain",
                                        "diff_q_gain", "diff_k_gain", "swa_q_gain", "swa_k_gain"]}
    diff_lambda = dt_in("diff_lambda", [2, 4, 64])
    diff_subln_gain = dt_in("diff_subln_gain", [2, 128])
    swa_sinks = dt_in("swa_sinks", [2, 8])
    c_ident = dt_in("c_ident", [128, 128])
    c_bd = dt_in("c_bd", [128, 128])
    c_a128 = dt_in("c_a128", [128, 128])
    c_tric = dt_in("c_tric", [128, 128])
    c_trip = dt_in("c_trip", [128, 128])
    c_triu = dt_in("c_triu", [128, 128])
    c_kaug = dt_in("c_kaug", [12, S])
    c_qalibi = dt_in("c_qalibi", [9, 4, S])
    c_cmask = dt_in("c_cmask", [128, 128])
    yout = nc.dram_tensor("yout", [nseq, S, D], F32, kind="ExternalOutput").ap()

    with ExitStack() as st:
        P = Prog(nc, st)
        x_sb = P.sb("x_sb", [128, NT, D], F32)
        hT = P.sb("hT", [128, 8, S], BF16)
        yT = P.sb("yT", [128, 8, S], BF16)
        NW = 3
        wbuf = [P.sb("wb%d" % i, [128, 8, 256], BF16) for i in range(NW)]
        QA = [P.sb("QA%d" % i, [128, S], BF16) for i in range(2)]
        KA = [P.sb("KA%d" % i, [128, S], BF16) for i in range(2)]
        VA = P.sb("VA", [128, NT, 512], BF16)
        NPR = 3
        Pt = [P.sb("Pt%d" % i, [128, 512], BF16) for i in range(NPR)]
        sqr = [P.sb("sq%d" % i, [128, 512], BF16) for i in range(2)]
        lnr = [P.sb("ln%d" % i, [128, 512], F32) for i in range(2)]
        rdr = [P.sb("rd%d" % i, [128, 512], F32) for i in range(1)]
        rgr = [P.sb("rg%d" % i, [128, 512], F32) for i in range(1)]
        n1t = P.sb("n1t", [128, 512], F32)
        n2t = P.sb("n2t", [128, 512], F32)
        gnb = P.sb("gnb", [128, D], F32)
        hrow = [P.sb("hrow%d" % i, [128, D], BF16) for i in range(1)]
        junk = hT[:, 0, 0:D]
        ssx = P.sb("ssx", [128, NT], F32)
        lnx = P.sb("lnx", [128, NT], F32)
        rsx = P.sb("rsx", [128, NT], F32)
        ident = P.sb("ident", [128, 128], BF16)
        bd = P.sb("bd", [128, 128], BF16)
        a128 = P.sb("a128", [128, 128], BF16)
        tric = P.sb("tric", [128, 128], BF16)
        trip = P.sb("trip", [128, 128], BF16)
        triu_f = P.sb("triu_b", [128, 128], BF16)
        ones_f = P.sb("ones_b", [128, 128], BF16)
        Lb = P.sb("Lb", [128, 3, 128], BF16)
        cmask = P.sb("cmask", [128, 128], F32)
        gcols = P.sb("gcols", [128, 20], F32)
        bfb = P.sb("bfb", [128, 2, 8], F32)
        lamb = n1t[:, :].rearrange("p (a b c) -> p a b c", a=2, b=4)
        lamw = n2t[:, 0:256].rearrange("p (a b c) -> p a b c", a=2, b=2)
        lams = P.sb("lams", [128, 2, 2], F32)
        lame = P.sb("lame", [128, 2, 2], F32)
        nlam = P.sb("nlam", [128, 2], F32)
        sinkb = P.sb("sinkb", [128, 2, 8], F32)
        esink = P.sb("esink", [128, 2, 8], F32)
        KM = P.sb("KM", [128, 16], BF16)
        kmf = P.sb("kmf", [128, 8], F32)
        kmf2 = P.sb("kmf2", [128, 8], F32)
        Gsb = P.sb("Gsb", [128, 256], F32)
        Gm = P.sb("Gm", [128, 128], F32)
        cmpt = gnb
        rank = P.sb("rank", [128, 128], F32)
        MT = P.sb("MT", [128, 128], BF16)
        zf = P.sb("zf", [128, 128], F32)
        Lf = P.sb("Lf", [128, 128], F32)
        cwt = P.sb("cwt", [128, 256], F32)
        cpre = P.sb("cpre", [128, 128], F32)
        cneg = P.sb("cneg", [128, 128], F32)
        cend = P.sb("cend", [128, 4, 8], F32)
        FB = P.sb("FB", [128, 8, NT, 4], F32)
        Sps = [P.ps("S%d" % i, [128, 512]) for i in range(2)]
        Ops = [P.ps("O%d" % i, [128, 512]) for i in range(3)]
        Mps = [P.ps("M%d" % i, [128, 512]) for i in range(2)]
        Tps = P.ps("Tps", [128, 1024], BF16)
        Sring = Ring([('S%d' % i, Sps[i]) for i in range(2)])
        Oring = Ring([('O%d' % i, Ops[i]) for i in range(3)])
        Mring = Ring([('M%d' % i, Mps[i]) for i in range(2)])
        M5ring = Ring([('M%d' % i, Mps[i]) for i in range(2)] + [('O%d' % i, Ops[i]) for i in range(3)])
        Pring = Ring([('Pt%d' % i, Pt[i]) for i in range(NPR)])
        sqring = Ring([('sq%d' % i, sqr[i]) for i in range(2)])
        lnring = Ring([('ln%d' % i, lnr[i]) for i in range(2)])
        rdring = Ring([('rd%d' % i, rdr[i]) for i in range(1)])
        rgring = Ring([('rg%d' % i, rgr[i]) for i in range(1)])
        hring = Ring([('hrow%d' % i, hrow[i]) for i in range(1)])
        Oring.i, Pring.i, Sring.i, Mring.i = phase

        P.dma('pool', ident[:], c_ident, 'cst', writes=['ident'])
        P.dma('pool', bd[:], c_bd, 'cst', writes=['bd'])
        P.dma('pool', a128[:], c_a128, 'cst', writes=['a128'])
        P.dma('pool', tric[:], c_tric, 'cst', writes=['tric'])
        P.dma('pool', trip[:], c_trip, 'cst', writes=['trip'])
        P.dma('pool', triu_f[:], c_triu, 'cst', writes=['triu_f'])
        P.dma('sp', cmask[:], c_cmask, 'cst2', writes=['cmask'])
        P.op('dve', lambda e: e.memset(ones_f[:], 1.0), writes=['ones_f'])
        for i in range(2):
            P.op('dve', lambda e, i=i: e.memset(QA[i][:], 0.0), writes=['QA%d' % i, 'QAm%d' % i, 'QAa%d' % i])
            P.op('dve', lambda e, i=i: e.memset(KA[i][:], 0.0), writes=['KA%d' % i, 'KAaug%d' % i])
            P.dma('pool', KA[i][64:76, :], c_kaug, 'cst', reads=[], writes=['KAaug%d' % i])
        gnames = ["moba_q_gain", "moba_k_gain", "fox_q_gain", "fox_k_gain", "diff_q_gain", "diff_k_gain", "swa_q_gain", "swa_k_gain"]
        GC = {}
        col = 0
        P.op('dve', lambda e: e.memset(gcols[:], 0.0), writes=['gcols'])
        for j in range(2):
            for n in gnames:
                GC[(n, j)] = col
                src = hg[n][j].rearrange("(p o) -> p o", o=1)
                P.dma('sp', gcols[0:64, col:col + 1], src, 'cstg', writes=['gcols'])
                P.dma('sp', gcols[64:128, col:col + 1], src, 'cstg', writes=['gcols'])
                col += 1
        for j in range(2):
            GC[('subln', j)] = col
            P.dma('sp', gcols[:, col:col + 1], diff_subln_gain[j].rearrange("(p o) -> p o", o=1), 'cstg', writes=['gcols'])
            col += 1
        P.flush('cstg', ['gcols'])
        for j in range(2):
            for n in gnames:
                if n.endswith("q_gain"):
                    c = GC[(n, j)]
                    P.op('dve', lambda e, c=c: e.tensor_scalar(gcols[:, c:c + 1], gcols[:, c:c + 1], 0.125, None, ALU.mult), reads=['gcols'], writes=['gcols'])
            layer = 2 * j + 1
            lam_init = 0.8 - 0.6 * math.exp(-0.3 * layer)
            c = GC[('subln', j)]
            P.op('dve', lambda e, c=c, v=1.0 - lam_init: e.tensor_scalar(gcols[:, c:c + 1], gcols[:, c:c + 1], float(v), None, ALU.mult), reads=['gcols'], writes=['gcols'])
        P.dma('sp', bfb[:].rearrange("p a b -> p (a b)"), b_forget.rearrange("(o a) b -> o (a b)", o=1).partition_broadcast(128), 'cst2', writes=['bfb'])
        P.dma('sp', sinkb[:].rearrange("p a b -> p (a b)"), swa_sinks.rearrange("(o a) b -> o (a b)", o=1).partition_broadcast(128), 'cst4', writes=['sinkb'])
        P.dma('sp', n1t[:, :], diff_lambda.rearrange("(o a) b c -> o (a b c)", o=1).partition_broadcast(128), 'cst3', writes=['n1t'])
        P.op('act', lambda e: e.activation(esink[:], sinkb[:], AF.Exp), reads=['sinkb'], writes=['esink'])
        P.op('dve', lambda e: e.tensor_tensor(lamw, bview(lamb[:, 0, 0, :], [[256, 2], [128, 2], [1, 64]]),
                                              bview(lamb[:, 0, 1, :], [[256, 2], [128, 2], [1, 64]]), ALU.mult), reads=['n1t'], writes=['n2t'])
        P.op('dve', lambda e: e.tensor_reduce(lams[:], lamw, AX.X, ALU.add), reads=['n2t'], writes=['lams'])
        P.op('act', lambda e: e.activation(lame[:], lams[:], AF.Exp), reads=['lams'], writes=['lame'])
        for j in range(2):
            lam_init = 0.8 - 0.6 * math.exp(-0.3 * (2 * j + 1))
            P.op('dve', lambda e, j=j, li=lam_init: e.scalar_tensor_tensor(nlam[:, j:j + 1], lame[:, j, 1:2], float(-li), lame[:, j, 0:1], ALU.add, ALU.subtract),
                 reads=['lame'], writes=['nlam'])

        P.flush('cst', ['ident', 'bd', 'a128', 'tric', 'trip', 'KAaug0', 'KAaug1', 'triu_f'])
        P.flush('cst2', ['cmask', 'bfb'])
        wstate = {'n': 0}

        def wload(parts):
            i = wstate['n'] % NW
            wstate['n'] += 1
            name = 'wb%d' % i
            for src, off in parts:
                wdt = src.shape[1]
                P.dma('pool', wbuf[i][:, :, off:off + wdt], src.rearrange("(k p) c -> p k c", p=128), 'w%d' % i, writes=[name])
            return name, wbuf[i]

        def rms_to_hT(l):
            P.dma('sp', gnb[:], norm_gain[l:l + 1, :].partition_broadcast(128), 'gnb', writes=['gnb'])
            for i in range(NT):
                P.op('act', lambda e, i=i: e.activation(junk, x_sb[:, i, :], AF.Square, accum_out=ssx[:, i:i + 1]), reads=['x%d' % i], writes=['hT', 'ssx'])
            P.op('act', lambda e: e.activation(lnx[:], ssx[:], AF.Ln, bias=EPS, scale=1.0 / D), reads=['ssx'], writes=['lnx'])
            P.op('act', lambda e: e.activation(rsx[:], lnx[:], AF.Exp, scale=-0.5), reads=['lnx'], writes=['rsx'])
            for i in range(NT):
                hn, ht = hring.next()
                P.op('dve', lambda e, i=i, ht=ht: e.scalar_tensor_tensor(ht[:], x_sb[:, i, :], rsx[:, i:i + 1], gnb[:], ALU.mult, ALU.mult),
                     reads=['x%d' % i, 'rsx', 'gnb'], writes=[hn])
                for k in range(8):
                    P.op('pe', lambda e, k=k, ht=ht: e.transpose(Tps[:, k * 128:(k + 1) * 128], ht[:, k * 128:(k + 1) * 128], ident[:]),
                         reads=[hn, 'ident'], writes=['Tps'])
                eng = 'dve'
                src = Tps[:, :].rearrange("p (k t) -> p k t", k=8)
                dst = hT[:, :, i * 128:(i + 1) * 128]
                if eng == 'dve':
                    P.op('dve', lambda e, src=src, dst=dst: e.tensor_copy(dst, src), reads=['Tps'], writes=['hT'])
                else:
                    P.op('act', lambda e, src=src, dst=dst: e.copy(dst, src), reads=['Tps'], writes=['hT'])

        def inproj_fm(wname, wt, woff, tc, Mn, Mt, M=128):
            for k in range(8):
                P.op('pe', lambda e, k=k: e.matmul(Mt[0:M, :], wt[:, k, woff:woff + M], hT[:, k, tc * 512:(tc + 1) * 512], start=(k == 0), stop=(k == 7)),
                     reads=[wname, 'hT'], writes=[Mn])

        def gates(W, gcols_list):
            for blk in range(4):
                wname, wt = wload([(W[:, gcols_list[2 * blk]:gcols_list[2 * blk] + 128], 0), (W[:, gcols_list[2 * blk + 1]:gcols_list[2 * blk + 1] + 128], 128)])
                for c in range(2):
                    p = 2 * blk + c
                    for tc in range(4):
                        Mn, Mt = Mring.next()
                        inproj_fm(wname, wt, c * 128, tc, Mn, Mt)
                        P.op('act', lambda e, Mt=Mt, p=p, tc=tc: e.activation(yT[:, p, tc * 512:(tc + 1) * 512], Mt[:], AF.Silu), reads=[Mn], writes=['yT%d' % p])

        def qk_pairs(jobs):
            blocks = [(job, tc) for job in jobs for tc in range(4)]
            st = {}

            def A1(b):
                (wname, wt, woff, gcol, dst, dstnames), tc = blocks[b]
                Mn, Mt = M5ring.next()
                inproj_fm(wname, wt, woff, tc, Mn, Mt)
                st[b] = [Mn, Mt]

            def A2(b):
                Mn, Mt = st[b]
                sn, sq = sqring.next()
                P.op('act', lambda e, Mt=Mt, sq=sq: e.activation(sq[:], Mt[:], AF.Square), reads=[Mn], writes=[sn])
                Sn, St = Sring.next()
                P.op('pe', lambda e, St=St, sq=sq: e.matmul(St[:], bd[:], sq[:], start=True, stop=True), reads=[sn, 'bd'], writes=[Sn])
                st[b] += [Sn, St]

            def B(b):
                (wname, wt, woff, gcol, dst, dstnames), tc = blocks[b]
                Mn, Mt, Sn, St = st.pop(b)
                ln_n, ln_t = lnring.next()
                P.op('act', lambda e, St=St, ln_t=ln_t: e.activation(ln_t[:], St[:], AF.Ln, bias=EPS), reads=[Sn], writes=[ln_n])
                P.op('act', lambda e, ln_t=ln_t: e.activation(ln_t[:], ln_t[:], AF.Exp, scale=-0.5), reads=[ln_n], writes=[ln_n])
                rn, rt = ln_n, ln_t
                cs = slice(tc * 512, (tc + 1) * 512)
                P.op('dve', lambda e, Mt=Mt, rt=rt, cs=cs, dst=dst, gcol=gcol: e.scalar_tensor_tensor(dst[0][0:64, cs], Mt[0:64, :], gcols[0:64, gcol:gcol + 1], rt[0:64, :], ALU.mult, ALU.mult),
                     reads=[Mn, rn, 'gcols'], writes=[dstnames[0]])
                P.op('dve', lambda e, Mt=Mt, rt=rt, cs=cs, dst=dst, gcol=gcol: e.scalar_tensor_tensor(dst[1][0:64, cs], Mt[64:128, :], gcols[64:128, gcol:gcol + 1], rt[64:128, :], ALU.mult, ALU.mult),
                     reads=[Mn, rn, 'gcols'], writes=[dstnames[1]])

            n = len(blocks)
            for t in range(n + 2):
                if t < n:
                    A1(t)
                if 0 <= t - 1 < n:
                    A2(t - 1)
                if 0 <= t - 2 < n:
                    B(t - 2)

        def load_alibi(hslot, sidx):
            P.dma('pool', QA[hslot][72:76, :], c_qalibi[sidx], 'qal%d' % hslot, writes=['QAa%d' % hslot])

        def dense_plan(w=None):
            plan = []
            for g in range(4):
                lst = []
                for j in range(4 * g + 4):
                    if w is not None and j + w < 4 * g:
                        continue
                    i_lo = max(j, 4 * g)
                    i_hi = 4 * g + 3 if w is None else min(4 * g + 3, j + w)
                    c0 = (i_lo - 4 * g) * 128
                    c1 = (i_hi - 4 * g + 1) * 128
                    masks = [(tric, 'tric', c0)] if j >= 4 * g else []
                    lst.append((j, c0, c1, masks))
                plan.append(lst)
            return plan

        def alibi_window(slope):
            w = int(math.ceil((50.0 / slope - 1.0) / 128.0))
            return None if w >= 15 else w

        def swa_plan():
            plan = []
            for g in range(4):
                lst = []
                for j in range(max(0, 4 * g - 1), 4 * g + 4):
                    masks = []
                    cols = []
                    for i, tab, tn in ((j, tric, 'tric'), (j + 1, trip, 'trip')):
                        if 4 * g <= i <= 4 * g + 3:
                            c = (i - 4 * g) * 128
                            masks.append((tab, tn, c))
                            cols.append(c)
                    lst.append((j, min(cols), max(cols) + 128, masks))
                plan.append(lst)
            return plan

        DENSE = dense_plan()
        DENSE_W = {}

        def dense_for(slope):
            w = alibi_window(slope)
            if w not in DENSE_W:
                DENSE_W[w] = dense_plan(w)
            return DENSE_W[w]
        SWA = swa_plan()

        def attn_group(qslot, kslot, g, plan_g, lhsT_fns, bias_fn, qreads, kreads, vreads):
            Os = [Oring.next() for _ in lhsT_fns]
            first = [True] * len(lhsT_fns)
            for (j, c0, c1, masks) in plan_g:
                Sn, St = Sring.next()
                fm = True
                for tab, tn, c in masks:
                    P.op('pe', lambda e, St=St, tab=tab, c=c, fm=fm: e.matmul(St[:, c:c + 128], ident[:], tab[:], start=fm, stop=False, skip_group_check=True),
                         reads=['ident', tn], writes=[Sn])
                    fm = False
                P.op('pe', lambda e, St=St, j=j, c0=c0, c1=c1, fm=fm: e.matmul(St[:, c0:c1], KA[kslot][0:KR, j * 128:(j + 1) * 128],
                                                                              QA[qslot][0:KR, g * 512 + c0:g * 512 + c1], start=fm, stop=True, skip_group_check=True),
                     reads=qreads + kreads, writes=[Sn])
                Pn, Ptile = Pring.next()
                b = bias_fn(j, g) if bias_fn is not None else None
                if b is None:
                    P.op('act', lambda e, St=St, Ptile=Ptile, c0=c0, c1=c1: e.activation(Ptile[:, c0:c1], St[:, c0:c1], AF.Exp), reads=[Sn], writes=[Pn])
                else:
                    bap, bname = b
                    P.op('act', lambda e, St=St, Ptile=Ptile, c0=c0, c1=c1, bap=bap: e.activation(Ptile[:, c0:c1], St[:, c0:c1], AF.Exp, bias=bap), reads=[Sn, bname], writes=[Pn])
                for v, lf in enumerate(lhsT_fns):
                    On, Ot = Os[v]
                    P.op('pe', lambda e, Ot=Ot, lf=lf, j=j, c0=c0, c1=c1, Ptile=Ptile, f=first[v]: e.matmul(Ot[:, c0:c1], lf(j), Ptile[:, c0:c1], start=f, stop=False, skip_group_check=True),
                         reads=[Pn] + vreads, writes=[On])
                    first[v] = False
            return Os

        def finalize_std(O, p, hf, g, extra_den=None):
            On, Ot = O
            b0 = 64 * hf
            cs = slice(g * 512, (g + 1) * 512)
            rn, rt = rdring.next()
            if extra_den is not None:
                P.op('dve', lambda e: e.tensor_scalar(rt[b0:b0 + 64, :], Ot[64:128, :], extra_den, None, ALU.add), reads=[On, 'esink'], writes=[rn])
                P.op('dve', lambda e: e.reciprocal(rt[b0:b0 + 64, :], rt[b0:b0 + 64, :]), reads=[rn], writes=[rn])
            else:
                P.op('dve', lambda e: e.reciprocal(rt[b0:b0 + 64, :], Ot[64:128, :]), reads=[On], writes=[rn])
            gn, gt = rgring.next()
            P.op('pool', lambda e: e.tensor_tensor(gt[b0:b0 + 64, :], rt[b0:b0 + 64, :], yT[b0:b0 + 64, p, cs], ALU.mult), reads=[rn, 'yT%d' % p], writes=[gn])
            P.op('dve', lambda e: e.tensor_tensor(yT[b0:b0 + 64, p, cs], Ot[0:64, :], gt[b0:b0 + 64, :], ALU.mult), reads=[On, gn], writes=['yT%d' % p])

        def v_block(W, vbase, ncols, layout):
            wname, wt = wload([(W[:, vbase:vbase + ncols], 0)])
            for i in range(NT):
                Mn, Mt = Mring.next()
                for k in range(8):
                    P.op('pe', lambda e, k=k, i=i, Mt=Mt: e.matmul(Mt[:, 0:ncols], hT[:, k, i * 128:(i + 1) * 128], wt[:, k, 0:ncols], start=(k == 0), stop=(k == 7)),
                         reads=[wname, 'hT'], writes=[Mn])
                nh, w0, stride_dst, dst0 = layout
                src = Mt[:, 0:ncols].rearrange("p (h d) -> p h d", h=nh)
                dstv = bview(VA[:, i, dst0:dst0 + 1], [[stride_dst, nh], [1, w0]])
                P.op('dve', lambda e, src=src, dstv=dstv: e.tensor_copy(dstv, src), reads=[Mn], writes=['VA'])

        def set_va_ones(regions):
            for (c0, strd, n, wdt) in regions:
                v = bview(VA[:, 0, c0:c0 + 1], [[512, NT], [strd, n], [1, wdt]])
                P.op('pool', lambda e, v=v: e.memset(v, 1.0), writes=['VA'])

        def moba_masks(hs):
            qn, kn = 'QA%d' % hs, 'KA%d' % hs
            P.op('dve', lambda e: e.tensor_reduce(kmf[0:64, :], KA[hs][0:64, :].rearrange("p (n l) -> p n l", n=8), AX.X, ALU.add), reads=[kn], writes=['kmf'])
            P.op('dve', lambda e: e.tensor_copy(KM[0:64, 0:8], kmf[0:64, :]), reads=['kmf'], writes=['KM'])
            P.op('dve', lambda e: e.tensor_tensor(kmf2[0:64, :], kmf[0:64, :], KM[0:64, 0:8], ALU.subtract), reads=['kmf', 'KM'], writes=['kmf2'])
            P.op('dve', lambda e: e.tensor_copy(KM[0:64, 8:16], kmf2[0:64, :]), reads=['kmf2'], writes=['KM'])
            Mn, Mt = Mring.next()
            for i in range(NT):
                P.op('pe', lambda e, i=i, Mt=Mt: e.matmul(Mt[:, i * 16:(i + 1) * 16], QA[hs][0:64, i * 128:(i + 1) * 128], KM[0:64, :], start=(i == 0), stop=(i == NT - 1), skip_group_check=True),
                     reads=[qn, 'KM'], writes=[Mn])
            P.op('dve', lambda e, Mt=Mt: e.tensor_copy(Gsb[:], Mt[:, 0:256]), reads=[Mn], writes=['Gsb'])
            gv = Gsb[:].rearrange("p (i c) -> p i c", c=16)
            P.op('dve', lambda e: e.tensor_tensor(Gm[:].rearrange("p (i n) -> p i n", n=8), gv[:, :, 0:8], gv[:, :, 8:16], ALU.add), reads=['Gsb'], writes=['Gm'])
            P.op('dve', lambda e: e.tensor_tensor(Gm[:], Gm[:], cmask[:], ALU.add), reads=['Gm', 'cmask'], writes=['Gm'])
            in0 = bview(Gm[:, 0:1], [[8, NT], [0, 8], [1, 8]])
            in1 = bview(Gm[:, 0:1], [[8, NT], [1, 8], [0, 8]])
            P.op('dve', lambda e: e.tensor_tensor(cmpt[:].rearrange("p (i n m) -> p i n m", n=8, m=8), in0, in1, ALU.is_gt), reads=['Gm'], writes=['gnb'])
            P.op('dve', lambda e: e.tensor_reduce(rank[:].rearrange("p (i n) -> p i n", n=8), cmpt[:].rearrange("p (i n m) -> p i n m", n=8, m=8), AX.X, ALU.add), reads=['gnb'], writes=['rank'])
            P.op('dve', lambda e: e.tensor_scalar(MT[:], rank[:], 3.5, NEG, ALU.is_gt, ALU.mult), reads=['rank'], writes=['MT'])
            for half in range(2):
                for ii in range(8):
                    i = half * 8 + ii
                    P.op('pe', lambda e, i=i, ii=ii: e.transpose(Tps[0:8, ii * 128:(ii + 1) * 128], MT[:, i * 8:(i + 1) * 8], ident[:]), reads=['MT', 'ident'], writes=['Tps'])
                P.op('dve', lambda e, half=half: e.tensor_copy(QA[hs][64:72, half * 1024:(half + 1) * 1024], Tps[0:8, :]), reads=['Tps'], writes=['QAm%d' % hs])

        def fox_prep(W, j):
            wname, wt = wload([(W[:, 4096:4104], 0)])
            Mn, Mt = Mring.next()
            for i in range(NT):
                for k in range(8):
                    P.op('pe', lambda e, i=i, k=k, Mt=Mt: e.matmul(Mt[:, i * 8:(i + 1) * 8], hT[:, k, i * 128:(i + 1) * 128], wt[:, k, 0:8],
                                                                start=(i == 0 and k == 0), stop=(i == NT - 1 and k == 7), skip_group_check=True),
                         reads=[wname, 'hT'], writes=[Mn])
            bb = bview(bfb[:, j, 0:1], [[0, NT], [1, 8]])
            P.op('dve', lambda e, Mt=Mt: e.tensor_tensor(zf[:].rearrange("p (i h) -> p i h", h=8), Mt[:, 0:128].rearrange("p (i h) -> p i h", h=8), bb, ALU.add), reads=[Mn, 'bfb'], writes=['zf'])
            P.op('act', lambda e: e.activation(zf[:], zf[:], AF.Exp, scale=-1.0), reads=['zf'], writes=['zf'])
            P.op('act', lambda e: e.activation(Lf[:], zf[:], AF.Ln, bias=1.0), reads=['zf'], writes=['Lf'])
            Mn2, Mt2 = Mring.next()
            P.op('dve', lambda e: e.tensor_copy(Lb[:, 0, :], Lf[:]), reads=['Lf'], writes=['Lb'])
            P.op('dve', lambda e: e.tensor_tensor(zf[:], Lf[:], Lb[:, 0, :], ALU.subtract), reads=['Lf', 'Lb'], writes=['zf'])
            P.op('dve', lambda e: e.tensor_copy(Lb[:, 1, :], zf[:]), reads=['zf'], writes=['Lb'])
            P.op('dve', lambda e: e.tensor_tensor(Lf[:], zf[:], Lb[:, 1, :], ALU.subtract), reads=['zf', 'Lb'], writes=['Lf'])
            P.op('dve', lambda e: e.tensor_copy(Lb[:, 2, :], Lf[:]), reads=['Lf'], writes=['Lb'])
            for c in range(3):
                P.op('pe', lambda e, c=c: e.matmul(Mt2[:, 0:128], triu_f[:], Lb[:, c, :], start=(c == 0), stop=(c == 2), skip_group_check=True), reads=['triu_f', 'Lb'], writes=[Mn2])
            for c in range(3):
                P.op('pe', lambda e, c=c: e.matmul(Mt2[:, 128:256], ones_f[:], Lb[:, c, :], start=False, stop=(c == 2), skip_group_check=True), reads=['ones_f', 'Lb'], writes=[Mn2])
            P.op('act', lambda e: e.copy(cwt[:], Mt2[:, 0:256]), reads=[Mn2], writes=['cwt'])
            P.op('dve', lambda e: e.memset(cpre[:, 0:8], 0.0), writes=['cpre'])
            for i in range(1, NT):
                P.op('dve', lambda e, i=i: e.tensor_tensor(cpre[:, i * 8:(i + 1) * 8], cpre[:, (i - 1) * 8:i * 8], cwt[:, 128 + (i - 1) * 8:128 + i * 8], ALU.add),
                     reads=['cpre', 'cwt'], writes=['cpre'])
            P.op('dve', lambda e: e.tensor_tensor(cneg[:], cwt[:, 0:128], cpre[:], ALU.add), reads=['cwt', 'cpre'], writes=['cneg'])
            for g in range(4):
                i = 4 * g + 3
                P.op('dve', lambda e, g=g, i=i: e.tensor_tensor(cend[:, g, :], cpre[:, i * 8:(i + 1) * 8], cwt[:, 128 + i * 8:128 + (i + 1) * 8], ALU.add), reads=['cpre', 'cwt'], writes=['cend'])
            for g in range(4):
                outv = bview(FB[:, 0, 0, g:g + 1], [[NT * 4, 8], [4, NT]])
                in0 = bview(cneg[:, 0:1], [[1, 8], [8, NT]])
                in1 = bview(cend[:, g, 0:1], [[1, 8], [0, NT]])
                P.op('dve', lambda e, outv=outv, in0=in0, in1=in1: e.tensor_tensor(outv, in0, in1, ALU.subtract), reads=['cneg', 'cend'], writes=['FB'])

        def layer_ab(l):
            j = l // 2
            W = w_in_ab[j]
            rms_to_hT(l)
            gates(W, [1536 + 128 * p for p in range(4)] + [3584 + 128 * p for p in range(4)])
            set_va_ones([(64, 128, 4, 64)])
            for mixer in range(2):
                if l == 2 and ('m%d' % mixer) in SKIP:
                    continue
                qb, kb, vb = (0, 512, 1024) if mixer == 0 else (2048, 2560, 3072)
                gq = GC[("moba_q_gain" if mixer == 0 else "fox_q_gain", j)]
                gk = GC[("moba_k_gain" if mixer == 0 else "fox_k_gain", j)]
                if mixer == 1:
                    fox_prep(W, j)
                for sbt in range(2):
                    v_block(W, vb + 256 * sbt, 256, (4, 64, 128, 0))
                    for pp in range(2):
                        pair = 2 * sbt + pp
                        wname, wt = wload([(W[:, qb + 128 * pair:qb + 128 * pair + 128], 0), (W[:, kb + 128 * pair:kb + 128 * pair + 128], 128)])
                        qk_pairs([(wname, wt, 0, gq, QA, ['QA0', 'QA1']), (wname, wt, 128, gk, KA, ['KA0', 'KA1'])])
                        for hs in range(2):
                            h = 2 * pair + hs
                            if mixer == 0:
                                load_alibi(hs, h + 1)
                                moba_masks(hs)
                                bias_fn = None
                            else:
                                load_alibi(hs, 0)
                                if pair == 0:
                                    P.op('dve', lambda e, hs=hs: e.memset(QA[hs][64:72, :], 0.0), writes=['QAm%d' % hs])
                                bias_fn = (lambda jj, g, h=h: (FB[:, h, jj, g:g + 1], 'FB'))
                            hv = 2 * pp + hs
                            lf = lambda jj, hv=hv: VA[:, jj, hv * 128:(hv + 1) * 128]
                            plan = dense_for(2.0 ** -(h + 1)) if mixer == 0 else DENSE
                            for g in range(4):
                                Os = attn_group(hs, hs, g, plan[g], [lf], bias_fn, ['QA%d' % hs, 'QAm%d' % hs, 'QAa%d' % hs], ['KA%d' % hs, 'KAaug%d' % hs], ['VA'])
                                finalize_std(Os[0], 4 * mixer + pair, hs, g)
            out_proj(w_out_ab[j])

        def layer_cd(l):
            j = l // 2
            W = w_in_cd[j]
            rms_to_hT(l)
            gates(W, [1536 + 128 * p for p in range(4)] + [2816 + 128 * p for p in range(4)])
            set_va_ones([(64, 256, 2, 128)])
            gq, gk = GC[("diff_q_gain", j)], GC[("diff_k_gain", j)]
            for hs in range(2):
                P.op('dve', lambda e, hs=hs: e.memset(QA[hs][64:72, :], 0.0), writes=['QAm%d' % hs])
            sub_c = GC[('subln', j)]
            for sbt in range(2):
                wname, wt = wload([(W[:, 1024 + 256 * sbt:1024 + 256 * sbt + 256], 0)])
                for i in range(NT):
                    Mn, Mt = Mring.next()
                    for k in range(8):
                        P.op('pe', lambda e, k=k, i=i, Mt=Mt, wt=wt: e.matmul(Mt[:, 0:256], hT[:, k, i * 128:(i + 1) * 128], wt[:, k, 0:256], start=(k == 0), stop=(k == 7)),
                             reads=[wname, 'hT'], writes=[Mn])
                    src = Mt[:, 0:256].rearrange("p (h c d) -> p h c d", h=2, c=2)
                    dstv = bview(VA[:, i, 0:1], [[256, 2], [192, 2], [1, 64]])
                    P.op('dve', lambda e, src=src, dstv=dstv: e.tensor_copy(dstv, src), reads=[Mn], writes=['VA'])
                for pp in range(2):
                    h = 2 * sbt + pp
                    wname, wt = wload([(W[:, 128 * h:128 * h + 128], 0), (W[:, 512 + 128 * h:512 + 128 * h + 128], 128)])
                    qk_pairs([(wname, wt, 0, gq, QA, ['QA0', 'QA1']), (wname, wt, 128, gk, KA, ['KA0', 'KA1'])])
                    for c in range(2):
                        load_alibi(c, 2 * h + 2)
                    lfs = [lambda jj, pp=pp: VA[:, jj, pp * 256:pp * 256 + 128], lambda jj, pp=pp: VA[:, jj, pp * 256 + 128:pp * 256 + 256]]
                    plan = dense_for(2.0 ** -(2 * h + 2))
                    for g in range(4):
                        cs = slice(g * 512, (g + 1) * 512)
                        for c in range(2):
                            Os = attn_group(c, c, g, plan[g], lfs, None, ['QA%d' % c, 'QAm%d' % c, 'QAa%d' % c], ['KA%d' % c, 'KAaug%d' % c], ['VA'])
                            (On0, Ot0), (On1, Ot1) = Os
                            rn, rt = rdring.next()
                            P.op('dve', lambda e, rt=rt, Ot0=Ot0: e.reciprocal(rt[0:64, :], Ot0[64:128, :]), reads=[On0], writes=[rn])
                            P.op('dve', lambda e, rt=rt, Ot1=Ot1: e.reciprocal(rt[64:128, :], Ot1[0:64, :]), reads=[On1], writes=[rn])
                            dstt = n1t if c == 0 else n2t
                            dn = 'n1t' if c == 0 else 'n2t'
                            P.op('dve', lambda e, rt=rt, Ot0=Ot0, dstt=dstt: e.tensor_tensor(dstt[0:64, :], Ot0[0:64, :], rt[0:64, :], ALU.mult), reads=[On0, rn], writes=[dn])
                            P.op('dve', lambda e, rt=rt, Ot1=Ot1, dstt=dstt: e.tensor_tensor(dstt[64:128, :], Ot1[64:128, :], rt[64:128, :], ALU.mult), reads=[On1, rn], writes=[dn])
                        P.op('dve', lambda e: e.scalar_tensor_tensor(n1t[:], n2t[:], nlam[:, j:j + 1], n1t[:], ALU.mult, ALU.add), reads=['n1t', 'n2t', 'nlam'], writes=['n1t'])
                        sn, sq = sqring.next()
                        P.op('act', lambda e, sq=sq: e.activation(sq[:], n1t[:], AF.Square), reads=['n1t'], writes=[sn])
                        Sn, St = Sring.next()
                        P.op('pe', lambda e, St=St, sq=sq: e.matmul(St[:], a128[:], sq[:], start=True, stop=True), reads=[sn, 'a128'], writes=[Sn])
                        ln_n, ln_t = lnring.next()
                        P.op('act', lambda e, St=St, ln_t=ln_t: e.activation(ln_t[:], St[:], AF.Ln, bias=EPS), reads=[Sn], writes=[ln_n])
                        rn2, rt2 = ln_n, ln_t
                        P.op('act', lambda e, ln_t=ln_t: e.activation(ln_t[:], ln_t[:], AF.Exp, scale=-0.5), reads=[ln_n], writes=[ln_n])
                        gn, gt = rgring.next()
                        P.op('pool', lambda e, gt=gt, rt2=rt2, cs=cs, h=h: e.tensor_tensor(gt[:], rt2[:], yT[:, h, cs], ALU.mult), reads=[rn2, 'yT%d' % h], writes=[gn])
                        P.op('dve', lambda e, gt=gt, cs=cs, h=h: e.scalar_tensor_tensor(yT[:, h, cs], n1t[:], gcols[:, sub_c:sub_c + 1], gt[:], ALU.mult, ALU.mult),
                             reads=['n1t', gn, 'gcols'], writes=['yT%d' % h])
            set_va_ones([(64, 128, 2, 64)])
            gq, gk = GC[("swa_q_gain", j)], GC[("swa_k_gain", j)]
            v_block(W, 2688, 128, (2, 64, 128, 0))
            wname, wt = wload([(W[:, 2560:2688], 0)])
            qk_pairs([(wname, wt, 0, gk, KA, ['KA0', 'KA1'])])
            for blk in range(2):
                wname, wt = wload([(W[:, 2048 + 256 * blk:2048 + 256 * blk + 256], 0)])
                for pp in range(2):
                    pair = 2 * blk + pp
                    qk_pairs([(wname, wt, 128 * pp, gq, QA, ['QA0', 'QA1'])])
                    kv = pair // 2
                    for hs in range(2):
                        hq = 2 * pair + hs
                        load_alibi(hs, hq + 1)
                        lf = lambda jj, kv=kv: VA[:, jj, kv * 128:(kv + 1) * 128]
                        for g in range(4):
                            Os = attn_group(hs, kv, g, SWA[g], [lf], None, ['QA%d' % hs, 'QAm%d' % hs, 'QAa%d' % hs], ['KA%d' % kv, 'KAaug%d' % kv], ['VA'])
                            finalize_std(Os[0], 4 + pair, hs, g, extra_den=esink[64:128, j, hq:hq + 1])
            out_proj(w_out_cd[j])

        def out_proj(Wo):
            for blk in range(4):
                wname, wt = wload([(Wo[:, 256 * blk:256 * blk + 256], 0)])
                for i in range(NT):
                    Mn, Mt = Mring.next()
                    for k in range(8):
                        P.op('pe', lambda e, k=k, i=i, Mt=Mt, wt=wt: e.matmul(Mt[:, 0:256], yT[:, k, i * 128:(i + 1) * 128], wt[:, k, :], start=(k == 0), stop=(k == 7)),
                             reads=[wname] + ['yT%d' % k for k in range(8)] if k == 0 else [wname], writes=[Mn])
                    xs = x_sb[:, i, 256 * blk:256 * blk + 256]
                    P.op('dve', lambda e, xs=xs, Mt=Mt: e.tensor_tensor(xs, xs, Mt[:, 0:256], ALU.add), reads=[Mn, 'x%d' % i], writes=['x%d' % i])

        for s in range(nseq):
            for i in range(NT):
                P.dma('sp', x_sb[:, i, :], xin[s, i * 128:(i + 1) * 128, :], 'xld%d' % i, reads=[], writes=['x%d' % i])
            for l in layers:
                if l % 2 == 0:
                    layer_ab(l)
                else:
                    layer_cd(l)
            for i in range(NT):
                P.dma('sp', yout[s, i * 128:(i + 1) * 128, :], x_sb[:, i, :], 'xst%d' % i, reads=['x%d' % i], writes=['out%d' % i])
        P.wait_all('sp', ['out%d' % i for i in range(NT)])
        P.emit()
    return nc


def make_consts():
    c = {}
    c["c_ident"] = np.eye(128, dtype=np.float32)
    bdm = np.zeros((128, 128), np.float32)
    bdm[0:64, 0:64] = 1.0 / 64
    bdm[64:128, 64:128] = 1.0 / 64
    c["c_bd"] = bdm
    c["c_a128"] = np.full((128, 128), 1.0 / 128, np.float32)
    s = np.arange(128)[:, None]
    t = np.arange(128)[None, :]
    c["c_tric"] = np.where(s <= t, 0.0, NEG).astype(np.float32)
    c["c_trip"] = np.where(s > t, 0.0, NEG).astype(np.float32)
    c["c_triu"] = (s <= t).astype(np.float32)
    pos = np.arange(S)
    kaug = np.zeros((12, S), np.float32)
    for n in range(8):
        kaug[n] = (pos // 256 == n)
    kaug[8] = 128 * (pos // 128)
    kaug[9] = pos % 128
    kaug[10] = 1.0
    kaug[11] = 1.0
    c["c_kaug"] = kaug
    qal = np.zeros((9, 4, S), np.float32)
    for i in range(1, 9):
        sl = 2.0 ** (-i)
        qal[i, 0] = sl
        qal[i, 1] = sl
        qal[i, 2] = -sl * 128 * (pos // 128)
        qal[i, 3] = -sl * (pos % 128)
    c["c_qalibi"] = qal
    cm = np.zeros((128, 16, 8), np.float32)
    for i in range(16):
        qb = i // 2
        for n in range(8):
            cm[:, i, n] = 0.0 if n < qb else (1e30 if n == qb else -1e30)
    c["c_cmask"] = cm.reshape(128, 128)
    return c


PARAMS = ["norm_gain", "w_in_ab", "b_forget", "moba_q_gain", "moba_k_gain", "fox_q_gain", "fox_k_gain", "w_out_ab", "w_in_cd",
          "diff_q_gain", "diff_k_gain", "diff_lambda", "diff_subln_gain", "swa_q_gain", "swa_k_gain", "swa_sinks", "w_out_cd"]


LAUNCH_GROUPS = [(0, 1, 2, 3)]


def kernel(**inputs):
    x = np.ascontiguousarray(np.asarray(inputs["x"], dtype=np.float32))
    n = 8
    per = x.shape[0] // n
    consts = make_consts()
    base = {k: np.ascontiguousarray(np.asarray(inputs[k], dtype=np.float32)) for k in PARAMS}
    base.update(consts)
    cur = x
    for grp in LAUNCH_GROUPS:
        nc = build(nseq=per, layers=grp)
        in_maps = []
        for c in range(n):
            m = dict(base)
            m["xin"] = np.ascontiguousarray(cur[c * per:(c + 1) * per])
            in_maps.append(m)
        res = run_bass_kernel_spmd(nc, in_maps, core_ids=list(range(n)))
        cur = np.concatenate([r["yout"] for r in res.results], axis=0)
    return cur
```

```python
import math
import numpy as np
from contextlib import ExitStack
import concourse.bass as bass
import concourse.mybir as mybir
from concourse.ap import AP
from concourse.bass_utils import run_bass_kernel_spmd

F32 = mybir.dt.float32
BF16 = mybir.dt.bfloat16
ALU = mybir.AluOpType
AF = mybir.ActivationFunctionType
AX = mybir.AxisListType

ENGS = ['pe', 'act', 'dve', 'pool', 'sp']
S = 2048
D = 1024
NT = 16
KR = 76
EPS = 1e-6
NEG = -30000.0


class Prog:
    def __init__(self, nc, stack):
        self.nc = nc
        self.stack = stack
        self.q = {k: [] for k in ENGS}
        self.cnt = {}
        self.sems = {}
        self.seen = {k: {} for k in ENGS}
        self.w = {}
        self.r = {}
        for k in ENGS:
            self._sem('c_' + k)

    def _sem(self, name):
        if name not in self.sems:
            self.sems[name] = self.stack.enter_context(self.nc.semaphore(name))
            self.cnt[name] = 0
        return self.sems[name]

    def sb(self, name, shape, dt):
        return self.stack.enter_context(self.nc.sbuf_tensor(name, shape, dt))

    def ps(self, name, shape, dt=F32):
        return self.stack.enter_context(self.nc.psum_tensor(name, shape, dt))

    def _deps(self, eng, reads, writes):
        deps = {}

        def add(sig):
            if sig is None:
                return
            s, v = sig
            if deps.get(s, 0) < v:
                deps[s] = v
        for r in reads:
            add(self.w.get(r))
        for w_ in writes:
            add(self.w.get(w_))
            for sig in self.r.get(w_, ()):
                add(sig)
        out = []
        own = 'c_' + eng
        for s, v in deps.items():
            if eng == 'pe' and s == own:
                continue
            if self.seen[eng].get(s, 0) < v:
                self.seen[eng][s] = v
                out.append((s, v))
        return out

    def _record(self, sig, reads, writes):
        for r in reads:
            lst = self.r.setdefault(r, [])
            lst[:] = [x for x in lst if x[0] != sig[0]] + [sig]
        for w_ in writes:
            self.w[w_] = sig
            self.r[w_] = []

    def op(self, eng, fn, reads=(), writes=()):
        waits = self._deps(eng, reads, writes)
        name = 'c_' + eng
        self.cnt[name] += 1
        sig = (name, self.cnt[name])
        self.q[eng].append((waits, fn, name, 1))
        self._record(sig, reads, writes)
        return sig

    def dma(self, eng, out, in_, sem, reads=(), writes=()):
        waits = self._deps(eng, reads, writes)
        self._sem(sem)
        self.cnt[sem] += 16
        sig = (sem, self.cnt[sem])
        self.q[eng].append((waits, lambda e: e.dma_start(out=out, in_=in_), sem, 16))
        self._record(sig, reads, writes)
        return sig

    def flush(self, sem, resources):
        for r in resources:
            self.w[r] = (sem, self.cnt[sem])

    def wait_all(self, eng, resources):
        waits = self._deps(eng, resources, ())
        self.q[eng].append((waits, None, None, 0))

    def emit(self):
        nc = self.nc
        names = {'pe': 'tensor', 'act': 'scalar', 'dve': 'vector', 'pool': 'gpsimd', 'sp': 'sync'}
        with nc.Block() as block:
            for k in ENGS:
                lst = self.q[k]
                if not lst:
                    continue

                def body(e, lst=lst):
                    for waits, fn, sname, inc in lst:
                        for s, v in waits:
                            e.wait_ge(self.sems[s], v)
                        if fn is not None:
                            fn(e).then_inc(self.sems[sname], inc)
                getattr(block, names[k])(body)


class Ring:
    def __init__(self, items):
        self.items = items
        self.i = 0

    def next(self):
        it = self.items[self.i % len(self.items)]
        self.i += 1
        return it


def alibi_slopes(n):
    return [2.0 ** (-8.0 * (i + 1) / n) for i in range(n)]


def bview(ap, pattern):
    p = ap.ap[0]
    return AP(ap.tensor, ap.offset, [[p[0], p[1]]] + [list(x) for x in pattern])


SKIP = set()


def build(nseq=2, layers=(0, 1, 2, 3), dbg=False, phase=(0, 0, 0, 0)):
    nc = bass.Bass("TRN2", target_bir_lowering=False)
    dt_in = lambda name, shape: nc.dram_tensor(name, list(shape), F32, kind="ExternalInput").ap()
    xin = dt_in("xin", [nseq, S, D])
    norm_gain = dt_in("norm_gain", [4, D])
    w_in_ab = dt_in("w_in_ab", [2, D, 4104])
    b_forget = dt_in("b_forget", [2, 8])
    w_out_ab = dt_in("w_out_ab", [2, D, D])
    w_in_cd = dt_in("w_in_cd", [2, D, 3328])
    w_out_cd = dt_in("w_out_cd", [2, D, D])
    hg = {n: dt_in(n, [2, 64]) for n in ["moba_q_gain", "moba_k_gain", "fox_q_gain", "fox_k_gain",
                                        "diff_q_gain", "diff_k_gain", "swa_q_gain", "swa_k_gain"]}
    diff_lambda = dt_in("diff_lambda", [2, 4, 64])
    diff_subln_gain = dt_in("diff_subln_gain", [2, 128])
    swa_sinks = dt_in("swa_sinks", [2, 8])
    c_ident = dt_in("c_ident", [128, 128])
    c_bd = dt_in("c_bd", [128, 128])
    c_a128 = dt_in("c_a128", [128, 128])
    c_tric = dt_in("c_tric", [128, 128])
    c_trip = dt_in("c_trip", [128, 128])
    c_triu = dt_in("c_triu", [128, 128])
    c_kaug = dt_in("c_kaug", [12, S])
    c_qalibi = dt_in("c_qalibi", [9, 4, S])
    c_cmask = dt_in("c_cmask", [128, 128])
    yout = nc.dram_tensor("yout", [nseq, S, D], F32, kind="ExternalOutput").ap()

    with ExitStack() as st:
        P = Prog(nc, st)
        x_sb = P.sb("x_sb", [128, NT, D], F32)
        hT = P.sb("hT", [128, 8, S], BF16)
        yT = P.sb("yT", [128, 8, S], BF16)
        NW = 3
        wbuf = [P.sb("wb%d" % i, [128, 8, 256], BF16) for i in range(NW)]
        QA = [P.sb("QA%d" % i, [128, S], BF16) for i in range(2)]
        KA = [P.sb("KA%d" % i, [128, S], BF16) for i in range(2)]
        VA = P.sb("VA", [128, NT, 512], BF16)
        NPR = 3
        Pt = [P.sb("Pt%d" % i, [128, 512], BF16) for i in range(NPR)]
        sqr = [P.sb("sq%d" % i, [128, 512], BF16) for i in range(2)]
        lnr = [P.sb("ln%d" % i, [128, 512], F32) for i in range(2)]
        rdr = [P.sb("rd%d" % i, [128, 512], F32) for i in range(1)]
        rgr = [P.sb("rg%d" % i, [128, 512], F32) for i in range(1)]
        n1t = P.sb("n1t", [128, 512], F32)
        n2t = P.sb("n2t", [128, 512], F32)
        gnb = P.sb("gnb", [128, D], F32)
        hrow = [P.sb("hrow%d" % i, [128, D], BF16) for i in range(1)]
        junk = hT[:, 0, 0:D]
        ssx = P.sb("ssx", [128, NT], F32)
        lnx = P.sb("lnx", [128, NT], F32)
        rsx = P.sb("rsx", [128, NT], F32)
        ident = P.sb("ident", [128, 128], BF16)
        bd = P.sb("bd", [128, 128], BF16)
        a128 = P.sb("a128", [128, 128], BF16)
        tric = P.sb("tric", [128, 128], BF16)
        trip = P.sb("trip", [128, 128], BF16)
        triu_f = P.sb("triu_b", [128, 128], BF16)
        ones_f = P.sb("ones_b", [128, 128], BF16)
        Lb = P.sb("Lb", [128, 3, 128], BF16)
        cmask = P.sb("cmask", [128, 128], F32)
        gcols = P.sb("gcols", [128, 20], F32)
        bfb = P.sb("bfb", [128, 2, 8], F32)
        lamb = n1t[:, :].rearrange("p (a b c) -> p a b c", a=2, b=4)
        lamw = n2t[:, 0:256].rearrange("p (a b c) -> p a b c", a=2, b=2)
        lams = P.sb("lams", [128, 2, 2], F32)
        lame = P.sb("lame", [128, 2, 2], F32)
        nlam = P.sb("nlam", [128, 2], F32)
        sinkb = P.sb("sinkb", [128, 2, 8], F32)
        esink = P.sb("esink", [128, 2, 8], F32)
        KM = P.sb("KM", [128, 16], BF16)
        kmf = P.sb("kmf", [128, 8], F32)
        kmf2 = P.sb("kmf2", [128, 8], F32)
        Gsb = P.sb("Gsb", [128, 256], F32)
        Gm = P.sb("Gm", [128, 128], F32)
        cmpt = gnb
        rank = P.sb("rank", [128, 128], F32)
        MT = P.sb("MT", [128, 128], BF16)
        zf = P.sb("zf", [128, 128], F32)
        Lf = P.sb("Lf", [128, 128], F32)
        cwt = P.sb("cwt", [128, 256], F32)
        cpre = P.sb("cpre", [128, 128], F32)
        cneg = P.sb("cneg", [128, 128], F32)
        cend = P.sb("cend", [128, 4, 8], F32)
        FB = P.sb("FB", [128, 8, NT, 4], F32)
        Sps = [P.ps("S%d" % i, [128, 512]) for i in range(2)]
        Ops = [P.ps("O%d" % i, [128, 512]) for i in range(3)]
        Mps = [P.ps("M%d" % i, [128, 512]) for i in range(2)]
        Tps = P.ps("Tps", [128, 1024], BF16)
        Sring = Ring([('S%d' % i, Sps[i]) for i in range(2)])
        Oring = Ring([('O%d' % i, Ops[i]) for i in range(3)])
        Mring = Ring([('M%d' % i, Mps[i]) for i in range(2)])
        M5ring = Ring([('M%d' % i, Mps[i]) for i in range(2)] + [('O%d' % i, Ops[i]) for i in range(3)])
        Pring = Ring([('Pt%d' % i, Pt[i]) for i in range(NPR)])
        sqring = Ring([('sq%d' % i, sqr[i]) for i in range(2)])
        lnring = Ring([('ln%d' % i, lnr[i]) for i in range(2)])
        rdring = Ring([('rd%d' % i, rdr[i]) for i in range(1)])
        rgring = Ring([('rg%d' % i, rgr[i]) for i in range(1)])
        hring = Ring([('hrow%d' % i, hrow[i]) for i in range(1)])
        Oring.i, Pring.i, Sring.i, Mring.i = phase

        P.dma('pool', ident[:], c_ident, 'cst', writes=['ident'])
        P.dma('pool', bd[:], c_bd, 'cst', writes=['bd'])
        P.dma('pool', a128[:], c_a128, 'cst', writes=['a128'])
        P.dma('pool', tric[:], c_tric, 'cst', writes=['tric'])
        P.dma('pool', trip[:], c_trip, 'cst', writes=['trip'])
        P.dma('pool', triu_f[:], c_triu, 'cst', writes=['triu_f'])
        P.dma('sp', cmask[:], c_cmask, 'cst2', writes=['cmask'])
        P.op('dve', lambda e: e.memset(ones_f[:], 1.0), writes=['ones_f'])
        for i in range(2):
            P.op('dve', lambda e, i=i: e.memset(QA[i][:], 0.0), writes=['QA%d' % i, 'QAm%d' % i, 'QAa%d' % i])
            P.op('dve', lambda e, i=i: e.memset(KA[i][:], 0.0), writes=['KA%d' % i, 'KAaug%d' % i])
            P.dma('pool', KA[i][64:76, :], c_kaug, 'cst', reads=[], writes=['KAaug%d' % i])
        gnames = ["moba_q_gain", "moba_k_gain", "fox_q_gain", "fox_k_gain", "diff_q_gain", "diff_k_gain", "swa_q_gain", "swa_k_gain"]
        GC = {}
        col = 0
        P.op('dve', lambda e: e.memset(gcols[:], 0.0), writes=['gcols'])
        for j in range(2):
            for n in gnames:
                GC[(n, j)] = col
                src = hg[n][j].rearrange("(p o) -> p o", o=1)
                P.dma('sp', gcols[0:64, col:col + 1], src, 'cstg', writes=['gcols'])
                P.dma('sp', gcols[64:128, col:col + 1], src, 'cstg', writes=['gcols'])
                col += 1
        for j in range(2):
            GC[('subln', j)] = col
            P.dma('sp', gcols[:, col:col + 1], diff_subln_gain[j].rearrange("(p o) -> p o", o=1), 'cstg', writes=['gcols'])
            col += 1
        P.flush('cstg', ['gcols'])
        for j in range(2):
            for n in gnames:
                if n.endswith("q_gain"):
                    c = GC[(n, j)]
                    P.op('dve', lambda e, c=c: e.tensor_scalar(gcols[:, c:c + 1], gcols[:, c:c + 1], 0.125, None, ALU.mult), reads=['gcols'], writes=['gcols'])
            layer = 2 * j + 1
            lam_init = 0.8 - 0.6 * math.exp(-0.3 * layer)
            c = GC[('subln', j)]
            P.op('dve', lambda e, c=c, v=1.0 - lam_init: e.tensor_scalar(gcols[:, c:c + 1], gcols[:, c:c + 1], float(v), None, ALU.mult), reads=['gcols'], writes=['gcols'])
        P.dma('sp', bfb[:].rearrange("p a b -> p (a b)"), b_forget.rearrange("(o a) b -> o (a b)", o=1).partition_broadcast(128), 'cst2', writes=['bfb'])
        P.dma('sp', sinkb[:].rearrange("p a b -> p (a b)"), swa_sinks.rearrange("(o a) b -> o (a b)", o=1).partition_broadcast(128), 'cst4', writes=['sinkb'])
        P.dma('sp', n1t[:, :], diff_lambda.rearrange("(o a) b c -> o (a b c)", o=1).partition_broadcast(128), 'cst3', writes=['n1t'])
        P.op('act', lambda e: e.activation(esink[:], sinkb[:], AF.Exp), reads=['sinkb'], writes=['esink'])
        P.op('dve', lambda e: e.tensor_tensor(lamw, bview(lamb[:, 0, 0, :], [[256, 2], [128, 2], [1, 64]]),
                                              bview(lamb[:, 0, 1, :], [[256, 2], [128, 2], [1, 64]]), ALU.mult), reads=['n1t'], writes=['n2t'])
        P.op('dve', lambda e: e.tensor_reduce(lams[:], lamw, AX.X, ALU.add), reads=['n2t'], writes=['lams'])
        P.op('act', lambda e: e.activation(lame[:], lams[:], AF.Exp), reads=['lams'], writes=['lame'])
        for j in range(2):
            lam_init = 0.8 - 0.6 * math.exp(-0.3 * (2 * j + 1))
            P.op('dve', lambda e, j=j, li=lam_init: e.scalar_tensor_tensor(nlam[:, j:j + 1], lame[:, j, 1:2], float(-li), lame[:, j, 0:1], ALU.add, ALU.subtract),
                 reads=['lame'], writes=['nlam'])

        P.flush('cst', ['ident', 'bd', 'a128', 'tric', 'trip', 'KAaug0', 'KAaug1', 'triu_f'])
        P.flush('cst2', ['cmask', 'bfb'])
        wstate = {'n': 0}

        def wload(parts):
            i = wstate['n'] % NW
            wstate['n'] += 1
            name = 'wb%d' % i
            for src, off in parts:
                wdt = src.shape[1]
                P.dma('pool', wbuf[i][:, :, off:off + wdt], src.rearrange("(k p) c -> p k c", p=128), 'w%d' % i, writes=[name])
            return name, wbuf[i]

        def rms_to_hT(l):
            P.dma('sp', gnb[:], norm_gain[l:l + 1, :].partition_broadcast(128), 'gnb', writes=['gnb'])
            for i in range(NT):
                P.op('act', lambda e, i=i: e.activation(junk, x_sb[:, i, :], AF.Square, accum_out=ssx[:, i:i + 1]), reads=['x%d' % i], writes=['hT', 'ssx'])
            P.op('act', lambda e: e.activation(lnx[:], ssx[:], AF.Ln, bias=EPS, scale=1.0 / D), reads=['ssx'], writes=['lnx'])
            P.op('act', lambda e: e.activation(rsx[:], lnx[:], AF.Exp, scale=-0.5), reads=['lnx'], writes=['rsx'])
            for i in range(NT):
                hn, ht = hring.next()
                P.op('dve', lambda e, i=i, ht=ht: e.scalar_tensor_tensor(ht[:], x_sb[:, i, :], rsx[:, i:i + 1], gnb[:], ALU.mult, ALU.mult),
                     reads=['x%d' % i, 'rsx', 'gnb'], writes=[hn])
                for k in range(8):
                    P.op('pe', lambda e, k=k, ht=ht: e.transpose(Tps[:, k * 128:(k + 1) * 128], ht[:, k * 128:(k + 1) * 128], ident[:]),
                         reads=[hn, 'ident'], writes=['Tps'])
                eng = 'dve'
                src = Tps[:, :].rearrange("p (k t) -> p k t", k=8)
                dst = hT[:, :, i * 128:(i + 1) * 128]
                if eng == 'dve':
                    P.op('dve', lambda e, src=src, dst=dst: e.tensor_copy(dst, src), reads=['Tps'], writes=['hT'])
                else:
                    P.op('act', lambda e, src=src, dst=dst: e.copy(dst, src), reads=['Tps'], writes=['hT'])

        def inproj_fm(wname, wt, woff, tc, Mn, Mt, M=128):
            for k in range(8):
                P.op('pe', lambda e, k=k: e.matmul(Mt[0:M, :], wt[:, k, woff:woff + M], hT[:, k, tc * 512:(tc + 1) * 512], start=(k == 0), stop=(k == 7)),
                     reads=[wname, 'hT'], writes=[Mn])

        def gates(W, gcols_list):
            for blk in range(4):
                wname, wt = wload([(W[:, gcols_list[2 * blk]:gcols_list[2 * blk] + 128], 0), (W[:, gcols_list[2 * blk + 1]:gcols_list[2 * blk + 1] + 128], 128)])
                for c in range(2):
                    p = 2 * blk + c
                    for tc in range(4):
                        Mn, Mt = Mring.next()
                        inproj_fm(wname, wt, c * 128, tc, Mn, Mt)
                        P.op('act', lambda e, Mt=Mt, p=p, tc=tc: e.activation(yT[:, p, tc * 512:(tc + 1) * 512], Mt[:], AF.Silu), reads=[Mn], writes=['yT%d' % p])

        def qk_pairs(jobs):
            blocks = [(job, tc) for job in jobs for tc in range(4)]
            st = {}

            def A1(b):
                (wname, wt, woff, gcol, dst, dstnames), tc = blocks[b]
                Mn, Mt = M5ring.next()
                inproj_fm(wname, wt, woff, tc, Mn, Mt)
                st[b] = [Mn, Mt]

            def A2(b):
                Mn, Mt = st[b]
                sn, sq = sqring.next()
                P.op('act', lambda e, Mt=Mt, sq=sq: e.activation(sq[:], Mt[:], AF.Square), reads=[Mn], writes=[sn])
                Sn, St = Sring.next()
                P.op('pe', lambda e, St=St, sq=sq: e.matmul(St[:], bd[:], sq[:], start=True, stop=True), reads=[sn, 'bd'], writes=[Sn])
                st[b] += [Sn, St]

            def B(b):
                (wname, wt, woff, gcol, dst, dstnames), tc = blocks[b]
                Mn, Mt, Sn, St = st.pop(b)
                ln_n, ln_t = lnring.next()
                P.op('act', lambda e, St=St, ln_t=ln_t: e.activation(ln_t[:], St[:], AF.Ln, bias=EPS), reads=[Sn], writes=[ln_n])
                P.op('act', lambda e, ln_t=ln_t: e.activation(ln_t[:], ln_t[:], AF.Exp, scale=-0.5), reads=[ln_n], writes=[ln_n])
                rn, rt = ln_n, ln_t
                cs = slice(tc * 512, (tc + 1) * 512)
                P.op('dve', lambda e, Mt=Mt, rt=rt, cs=cs, dst=dst, gcol=gcol: e.scalar_tensor_tensor(dst[0][0:64, cs], Mt[0:64, :], gcols[0:64, gcol:gcol + 1], rt[0:64, :], ALU.mult, ALU.mult),
                     reads=[Mn, rn, 'gcols'], writes=[dstnames[0]])
                P.op('dve', lambda e, Mt=Mt, rt=rt, cs=cs, dst=dst, gcol=gcol: e.scalar_tensor_tensor(dst[1][0:64, cs], Mt[64:128, :], gcols[64:128, gcol:gcol + 1], rt[64:128, :], ALU.mult, ALU.mult),
                     reads=[Mn, rn, 'gcols'], writes=[dstnames[1]])

            n = len(blocks)
            for t in range(n + 2):
                if t < n:
                    A1(t)
                if 0 <= t - 1 < n:
                    A2(t - 1)
                if 0 <= t - 2 < n:
                    B(t - 2)

        def load_alibi(hslot, sidx):
            P.dma('pool', QA[hslot][72:76, :], c_qalibi[sidx], 'qal%d' % hslot, writes=['QAa%d' % hslot])

        def dense_plan(w=None):
            plan = []
            for g in range(4):
                lst = []
                for j in range(4 * g + 4):
                    if w is not None and j + w < 4 * g:
                        continue
                    i_lo = max(j, 4 * g)
                    i_hi = 4 * g + 3 if w is None else min(4 * g + 3, j + w)
                    c0 = (i_lo - 4 * g) * 128
                    c1 = (i_hi - 4 * g + 1) * 128
                    masks = [(tric, 'tric', c0)] if j >= 4 * g else []
                    lst.append((j, c0, c1, masks))
                plan.append(lst)
            return plan

        def alibi_window(slope):
            w = int(math.ceil((50.0 / slope - 1.0) / 128.0))
            return None if w >= 15 else w

        def swa_plan():
            plan = []
            for g in range(4):
                lst = []
                for j in range(max(0, 4 * g - 1), 4 * g + 4):
                    masks = []
                    cols = []
                    for i, tab, tn in ((j, tric, 'tric'), (j + 1, trip, 'trip')):
                        if 4 * g <= i <= 4 * g + 3:
                            c = (i - 4 * g) * 128
                            masks.append((tab, tn, c))
                            cols.append(c)
                    lst.append((j, min(cols), max(cols) + 128, masks))
                plan.append(lst)
            return plan

        DENSE = dense_plan()
        DENSE_W = {}

        def dense_for(slope):
            w = alibi_window(slope)
            if w not in DENSE_W:
                DENSE_W[w] = dense_plan(w)
            return DENSE_W[w]
        SWA = swa_plan()

        def attn_seq(segments):
            items = []
            for si, seg in enumerate(segments):
                for it in seg['plan']:
                    items.append((si, it))

            def emit_qk(n):
                si, (j, c0, c1, masks) = items[n]
                seg = segments[si]
                qslot, kslot, g = seg['qslot'], seg['kslot'], seg['g']
                Sn, St = Sring.next()
                fm = True
                for tab, tn, c in masks:
                    P.op('pe', lambda e, St=St, tab=tab, c=c, fm=fm: e.matmul(St[:, c:c + 128], ident[:], tab[:], start=fm, stop=False, skip_group_check=True),
                         reads=['ident', tn], writes=[Sn])
                    fm = False
                P.op('pe', lambda e, St=St, j=j, c0=c0, c1=c1, fm=fm, qslot=qslot, kslot=kslot, g=g: e.matmul(
                    St[:, c0:c1], KA[kslot][0:KR, j * 128:(j + 1) * 128], QA[qslot][0:KR, g * 512 + c0:g * 512 + c1], start=fm, stop=True, skip_group_check=True),
                     reads=seg['qreads'] + seg['kreads'], writes=[Sn])
                return Sn, St

            state = {}
            nxt = emit_qk(0) if items else None
            for n, (si, (j, c0, c1, masks)) in enumerate(items):
                seg = segments[si]
                g = seg['g']
                Sn, St = nxt
                if n + 1 < len(items):
                    nxt = emit_qk(n + 1)
                if si not in state:
                    state[si] = ([Oring.next() for _ in seg['lhsT_fns']], [True] * len(seg['lhsT_fns']))
                Os, first = state[si]
                Pn, Ptile = Pring.next()
                b = seg['bias_fn'](j, g) if seg['bias_fn'] is not None else None
                if b is None:
                    P.op('act', lambda e, St=St, Ptile=Ptile, c0=c0, c1=c1: e.activation(Ptile[:, c0:c1], St[:, c0:c1], AF.Exp), reads=[Sn], writes=[Pn])
                else:
                    bap, bname = b
                    P.op('act', lambda e, St=St, Ptile=Ptile, c0=c0, c1=c1, bap=bap: e.activation(Ptile[:, c0:c1], St[:, c0:c1], AF.Exp, bias=bap), reads=[Sn, bname], writes=[Pn])
                for v, lf in enumerate(seg['lhsT_fns']):
                    On, Ot = Os[v]
                    P.op('pe', lambda e, Ot=Ot, lf=lf, j=j, c0=c0, c1=c1, Ptile=Ptile, f=first[v]: e.matmul(Ot[:, c0:c1], lf(j), Ptile[:, c0:c1], start=f, stop=False, skip_group_check=True),
                         reads=[Pn] + seg['vreads'], writes=[On])
                    first[v] = False
                last_of_seg = (n + 1 == len(items)) or (items[n + 1][0] != si)
                if last_of_seg:
                    seg['done'](Os)

        def finalize_std(O, p, hf, g, extra_den=None):
            On, Ot = O
            b0 = 64 * hf
            cs = slice(g * 512, (g + 1) * 512)
            rn, rt = rdring.next()
            if extra_den is not None:
                P.op('dve', lambda e: e.tensor_scalar(rt[b0:b0 + 64, :], Ot[64:128, :], extra_den, None, ALU.add), reads=[On, 'esink'], writes=[rn])
                P.op('dve', lambda e: e.reciprocal(rt[b0:b0 + 64, :], rt[b0:b0 + 64, :]), reads=[rn], writes=[rn])
            else:
                P.op('dve', lambda e: e.reciprocal(rt[b0:b0 + 64, :], Ot[64:128, :]), reads=[On], writes=[rn])
            gn, gt = rgring.next()
            P.op('pool', lambda e: e.tensor_tensor(gt[b0:b0 + 64, :], rt[b0:b0 + 64, :], yT[b0:b0 + 64, p, cs], ALU.mult), reads=[rn, 'yT%d' % p], writes=[gn])
            P.op('dve', lambda e: e.tensor_tensor(yT[b0:b0 + 64, p, cs], Ot[0:64, :], gt[b0:b0 + 64, :], ALU.mult), reads=[On, gn], writes=['yT%d' % p])

        def v_block(W, vbase, ncols, layout):
            wname, wt = wload([(W[:, vbase:vbase + ncols], 0)])
            for i in range(NT):
                Mn, Mt = Mring.next()
                for k in range(8):
                    P.op('pe', lambda e, k=k, i=i, Mt=Mt: e.matmul(Mt[:, 0:ncols], hT[:, k, i * 128:(i + 1) * 128], wt[:, k, 0:ncols], start=(k == 0), stop=(k == 7)),
                         reads=[wname, 'hT'], writes=[Mn])
                nh, w0, stride_dst, dst0 = layout
                src = Mt[:, 0:ncols].rearrange("p (h d) -> p h d", h=nh)
                dstv = bview(VA[:, i, dst0:dst0 + 1], [[stride_dst, nh], [1, w0]])
                P.op('dve', lambda e, src=src, dstv=dstv: e.tensor_copy(dstv, src), reads=[Mn], writes=['VA'])

        def set_va_ones(regions):
            for (c0, strd, n, wdt) in regions:
                v = bview(VA[:, 0, c0:c0 + 1], [[512, NT], [strd, n], [1, wdt]])
                P.op('pool', lambda e, v=v: e.memset(v, 1.0), writes=['VA'])

        def moba_masks(hs):
            qn, kn = 'QA%d' % hs, 'KA%d' % hs
            P.op('dve', lambda e: e.tensor_reduce(kmf[0:64, :], KA[hs][0:64, :].rearrange("p (n l) -> p n l", n=8), AX.X, ALU.add), reads=[kn], writes=['kmf'])
            P.op('dve', lambda e: e.tensor_copy(KM[0:64, 0:8], kmf[0:64, :]), reads=['kmf'], writes=['KM'])
            P.op('dve', lambda e: e.tensor_tensor(kmf2[0:64, :], kmf[0:64, :], KM[0:64, 0:8], ALU.subtract), reads=['kmf', 'KM'], writes=['kmf2'])
            P.op('dve', lambda e: e.tensor_copy(KM[0:64, 8:16], kmf2[0:64, :]), reads=['kmf2'], writes=['KM'])
            Mn, Mt = Mring.next()
            for i in range(NT):
                P.op('pe', lambda e, i=i, Mt=Mt: e.matmul(Mt[:, i * 16:(i + 1) * 16], QA[hs][0:64, i * 128:(i + 1) * 128], KM[0:64, :], start=(i == 0), stop=(i == NT - 1), skip_group_check=True),
                     reads=[qn, 'KM'], writes=[Mn])
            P.op('dve', lambda e, Mt=Mt: e.tensor_copy(Gsb[:], Mt[:, 0:256]), reads=[Mn], writes=['Gsb'])
            gv = Gsb[:].rearrange("p (i c) -> p i c", c=16)
            P.op('dve', lambda e: e.tensor_tensor(Gm[:].rearrange("p (i n) -> p i n", n=8), gv[:, :, 0:8], gv[:, :, 8:16], ALU.add), reads=['Gsb'], writes=['Gm'])
            P.op('dve', lambda e: e.tensor_tensor(Gm[:], Gm[:], cmask[:], ALU.add), reads=['Gm', 'cmask'], writes=['Gm'])
            in0 = bview(Gm[:, 0:1], [[8, NT], [0, 8], [1, 8]])
            in1 = bview(Gm[:, 0:1], [[8, NT], [1, 8], [0, 8]])
            P.op('dve', lambda e: e.tensor_tensor(cmpt[:].rearrange("p (i n m) -> p i n m", n=8, m=8), in0, in1, ALU.is_gt), reads=['Gm'], writes=['gnb'])
            P.op('dve', lambda e: e.tensor_reduce(rank[:].rearrange("p (i n) -> p i n", n=8), cmpt[:].rearrange("p (i n m) -> p i n m", n=8, m=8), AX.X, ALU.add), reads=['gnb'], writes=['rank'])
            P.op('dve', lambda e: e.tensor_scalar(MT[:], rank[:], 3.5, NEG, ALU.is_gt, ALU.mult), reads=['rank'], writes=['MT'])
            for half in range(2):
                for ii in range(8):
                    i = half * 8 + ii
                    P.op('pe', lambda e, i=i, ii=ii: e.transpose(Tps[0:8, ii * 128:(ii + 1) * 128], MT[:, i * 8:(i + 1) * 8], ident[:]), reads=['MT', 'ident'], writes=['Tps'])
                P.op('dve', lambda e, half=half: e.tensor_copy(QA[hs][64:72, half * 1024:(half + 1) * 1024], Tps[0:8, :]), reads=['Tps'], writes=['QAm%d' % hs])

        def fox_prep(W, j):
            wname, wt = wload([(W[:, 4096:4104], 0)])
            Mn, Mt = Mring.next()
            for i in range(NT):
                for k in range(8):
                    P.op('pe', lambda e, i=i, k=k, Mt=Mt: e.matmul(Mt[:, i * 8:(i + 1) * 8], hT[:, k, i * 128:(i + 1) * 128], wt[:, k, 0:8],
                                                                start=(i == 0 and k == 0), stop=(i == NT - 1 and k == 7), skip_group_check=True),
                         reads=[wname, 'hT'], writes=[Mn])
            bb = bview(bfb[:, j, 0:1], [[0, NT], [1, 8]])
            P.op('dve', lambda e, Mt=Mt: e.tensor_tensor(zf[:].rearrange("p (i h) -> p i h", h=8), Mt[:, 0:128].rearrange("p (i h) -> p i h", h=8), bb, ALU.add), reads=[Mn, 'bfb'], writes=['zf'])
            P.op('act', lambda e: e.activation(zf[:], zf[:], AF.Exp, scale=-1.0), reads=['zf'], writes=['zf'])
            P.op('act', lambda e: e.activation(Lf[:], zf[:], AF.Ln, bias=1.0), reads=['zf'], writes=['Lf'])
            Mn2, Mt2 = Mring.next()
            P.op('dve', lambda e: e.tensor_copy(Lb[:, 0, :], Lf[:]), reads=['Lf'], writes=['Lb'])
            P.op('dve', lambda e: e.tensor_tensor(zf[:], Lf[:], Lb[:, 0, :], ALU.subtract), reads=['Lf', 'Lb'], writes=['zf'])
            P.op('dve', lambda e: e.tensor_copy(Lb[:, 1, :], zf[:]), reads=['zf'], writes=['Lb'])
            P.op('dve', lambda e: e.tensor_tensor(Lf[:], zf[:], Lb[:, 1, :], ALU.subtract), reads=['zf', 'Lb'], writes=['Lf'])
            P.op('dve', lambda e: e.tensor_copy(Lb[:, 2, :], Lf[:]), reads=['Lf'], writes=['Lb'])
            for c in range(3):
                P.op('pe', lambda e, c=c: e.matmul(Mt2[:, 0:128], triu_f[:], Lb[:, c, :], start=(c == 0), stop=(c == 2), skip_group_check=True), reads=['triu_f', 'Lb'], writes=[Mn2])
            for c in range(3):
                P.op('pe', lambda e, c=c: e.matmul(Mt2[:, 128:256], ones_f[:], Lb[:, c, :], start=False, stop=(c == 2), skip_group_check=True), reads=['ones_f', 'Lb'], writes=[Mn2])
            P.op('act', lambda e: e.copy(cwt[:], Mt2[:, 0:256]), reads=[Mn2], writes=['cwt'])
            P.op('dve', lambda e: e.memset(cpre[:, 0:8], 0.0), writes=['cpre'])
            for i in range(1, NT):
                P.op('dve', lambda e, i=i: e.tensor_tensor(cpre[:, i * 8:(i + 1) * 8], cpre[:, (i - 1) * 8:i * 8], cwt[:, 128 + (i - 1) * 8:128 + i * 8], ALU.add),
                     reads=['cpre', 'cwt'], writes=['cpre'])
            P.op('dve', lambda e: e.tensor_tensor(cneg[:], cwt[:, 0:128], cpre[:], ALU.add), reads=['cwt', 'cpre'], writes=['cneg'])
            for g in range(4):
                i = 4 * g + 3
                P.op('dve', lambda e, g=g, i=i: e.tensor_tensor(cend[:, g, :], cpre[:, i * 8:(i + 1) * 8], cwt[:, 128 + i * 8:128 + (i + 1) * 8], ALU.add), reads=['cpre', 'cwt'], writes=['cend'])
            for g in range(4):
                outv = bview(FB[:, 0, 0, g:g + 1], [[NT * 4, 8], [4, NT]])
                in0 = bview(cneg[:, 0:1], [[1, 8], [8, NT]])
                in1 = bview(cend[:, g, 0:1], [[1, 8], [0, NT]])
                P.op('dve', lambda e, outv=outv, in0=in0, in1=in1: e.tensor_tensor(outv, in0, in1, ALU.subtract), reads=['cneg', 'cend'], writes=['FB'])

        def layer_ab(l):
            j = l // 2
            W = w_in_ab[j]
            rms_to_hT(l)
            gates(W, [1536 + 128 * p for p in range(4)] + [3584 + 128 * p for p in range(4)])
            set_va_ones([(64, 128, 4, 64)])
            for mixer in range(2):
                if l == 2 and ('m%d' % mixer) in SKIP:
                    continue
                qb, kb, vb = (0, 512, 1024) if mixer == 0 else (2048, 2560, 3072)
                gq = GC[("moba_q_gain" if mixer == 0 else "fox_q_gain", j)]
                gk = GC[("moba_k_gain" if mixer == 0 else "fox_k_gain", j)]
                if mixer == 1:
                    fox_prep(W, j)
                for sbt in range(2):
                    v_block(W, vb + 256 * sbt, 256, (4, 64, 128, 0))
                    for pp in range(2):
                        pair = 2 * sbt + pp
                        wname, wt = wload([(W[:, qb + 128 * pair:qb + 128 * pair + 128], 0), (W[:, kb + 128 * pair:kb + 128 * pair + 128], 128)])
                        qk_pairs([(wname, wt, 0, gq, QA, ['QA0', 'QA1']), (wname, wt, 128, gk, KA, ['KA0', 'KA1'])])
                        for hs in range(2):
                            h = 2 * pair + hs
                            if mixer == 0:
                                load_alibi(hs, h + 1)
                                moba_masks(hs)
                                bias_fn = None
                            else:
                                load_alibi(hs, 0)
                                if pair == 0:
                                    P.op('dve', lambda e, hs=hs: e.memset(QA[hs][64:72, :], 0.0), writes=['QAm%d' % hs])
                                bias_fn = (lambda jj, g, h=h: (FB[:, h, jj, g:g + 1], 'FB'))
                            hv = 2 * pp + hs
                            lf = lambda jj, hv=hv: VA[:, jj, hv * 128:(hv + 1) * 128]
                            plan = dense_for(2.0 ** -(h + 1)) if mixer == 0 else DENSE
                            segs = []
                            for g in range(4):
                                segs.append(dict(qslot=hs, kslot=hs, g=g, plan=plan[g], lhsT_fns=[lf], bias_fn=bias_fn,
                                                 qreads=['QA%d' % hs, 'QAm%d' % hs, 'QAa%d' % hs], kreads=['KA%d' % hs, 'KAaug%d' % hs], vreads=['VA'],
                                                 done=(lambda Os, g=g, p=4 * mixer + pair, hs=hs: finalize_std(Os[0], p, hs, g))))
                            attn_seq(segs)
            out_proj(w_out_ab[j])

        def layer_cd(l):
            j = l // 2
            W = w_in_cd[j]
            rms_to_hT(l)
            gates(W, [1536 + 128 * p for p in range(4)] + [2816 + 128 * p for p in range(4)])
            set_va_ones([(64, 256, 2, 128)])
            gq, gk = GC[("diff_q_gain", j)], GC[("diff_k_gain", j)]
            for hs in range(2):
                P.op('dve', lambda e, hs=hs: e.memset(QA[hs][64:72, :], 0.0), writes=['QAm%d' % hs])
            sub_c = GC[('subln', j)]
            for sbt in range(2):
                wname, wt = wload([(W[:, 1024 + 256 * sbt:1024 + 256 * sbt + 256], 0)])
                for i in range(NT):
                    Mn, Mt = Mring.next()
                    for k in range(8):
                        P.op('pe', lambda e, k=k, i=i, Mt=Mt, wt=wt: e.matmul(Mt[:, 0:256], hT[:, k, i * 128:(i + 1) * 128], wt[:, k, 0:256], start=(k == 0), stop=(k == 7)),
                             reads=[wname, 'hT'], writes=[Mn])
                    src = Mt[:, 0:256].rearrange("p (h c d) -> p h c d", h=2, c=2)
                    dstv = bview(VA[:, i, 0:1], [[256, 2], [192, 2], [1, 64]])
                    P.op('dve', lambda e, src=src, dstv=dstv: e.tensor_copy(dstv, src), reads=[Mn], writes=['VA'])
                for pp in range(2):
                    h = 2 * sbt + pp
                    wname, wt = wload([(W[:, 128 * h:128 * h + 128], 0), (W[:, 512 + 128 * h:512 + 128 * h + 128], 128)])
                    qk_pairs([(wname, wt, 0, gq, QA, ['QA0', 'QA1']), (wname, wt, 128, gk, KA, ['KA0', 'KA1'])])
                    for c in range(2):
                        load_alibi(c, 2 * h + 2)
                    lfs = [lambda jj, pp=pp: VA[:, jj, pp * 256:pp * 256 + 128], lambda jj, pp=pp: VA[:, jj, pp * 256 + 128:pp * 256 + 256]]
                    plan = dense_for(2.0 ** -(2 * h + 2))
                    def diff_comp_done(Os, c):
                        (On0, Ot0), (On1, Ot1) = Os
                        rn, rt = rdring.next()
                        P.op('dve', lambda e, rt=rt, Ot0=Ot0: e.reciprocal(rt[0:64, :], Ot0[64:128, :]), reads=[On0], writes=[rn])
                        P.op('dve', lambda e, rt=rt, Ot1=Ot1: e.reciprocal(rt[64:128, :], Ot1[0:64, :]), reads=[On1], writes=[rn])
                        dstt = n1t if c == 0 else n2t
                        dn = 'n1t' if c == 0 else 'n2t'
                        P.op('dve', lambda e, rt=rt, Ot0=Ot0, dstt=dstt: e.tensor_tensor(dstt[0:64, :], Ot0[0:64, :], rt[0:64, :], ALU.mult), reads=[On0, rn], writes=[dn])
                        P.op('dve', lambda e, rt=rt, Ot1=Ot1, dstt=dstt: e.tensor_tensor(dstt[64:128, :], Ot1[64:128, :], rt[64:128, :], ALU.mult), reads=[On1, rn], writes=[dn])

                    def diff_group_done(g, h):
                        cs = slice(g * 512, (g + 1) * 512)
                        P.op('dve', lambda e: e.scalar_tensor_tensor(n1t[:], n2t[:], nlam[:, j:j + 1], n1t[:], ALU.mult, ALU.add), reads=['n1t', 'n2t', 'nlam'], writes=['n1t'])
                        sn, sq = sqring.next()
                        P.op('act', lambda e, sq=sq: e.activation(sq[:], n1t[:], AF.Square), reads=['n1t'], writes=[sn])
                        Mn, Mt = Mring.next()
                        P.op('pe', lambda e, Mt=Mt, sq=sq: e.matmul(Mt[:], a128[:], sq[:], start=True, stop=True), reads=[sn, 'a128'], writes=[Mn])
                        ln_n, ln_t = lnring.next()
                        P.op('act', lambda e, Mt=Mt, ln_t=ln_t: e.activation(ln_t[:], Mt[:], AF.Ln, bias=EPS), reads=[Mn], writes=[ln_n])
                        P.op('act', lambda e, ln_t=ln_t: e.activation(ln_t[:], ln_t[:], AF.Exp, scale=-0.5), reads=[ln_n], writes=[ln_n])
                        gn, gt = rgring.next()
                        P.op('pool', lambda e, gt=gt, ln_t=ln_t, cs=cs, h=h: e.tensor_tensor(gt[:], ln_t[:], yT[:, h, cs], ALU.mult), reads=[ln_n, 'yT%d' % h], writes=[gn])
                        P.op('dve', lambda e, gt=gt, cs=cs, h=h: e.scalar_tensor_tensor(yT[:, h, cs], n1t[:], gcols[:, sub_c:sub_c + 1], gt[:], ALU.mult, ALU.mult),
                             reads=['n1t', gn, 'gcols'], writes=['yT%d' % h])

                    segs = []
                    for g in range(4):
                        for c in range(2):
                            def done(Os, g=g, c=c, h=h):
                                diff_comp_done(Os, c)
                                if c == 1:
                                    diff_group_done(g, h)
                            segs.append(dict(qslot=c, kslot=c, g=g, plan=plan[g], lhsT_fns=lfs, bias_fn=None,
                                             qreads=['QA%d' % c, 'QAm%d' % c, 'QAa%d' % c], kreads=['KA%d' % c, 'KAaug%d' % c], vreads=['VA'], done=done))
                    attn_seq(segs)
            set_va_ones([(64, 128, 2, 64)])
            gq, gk = GC[("swa_q_gain", j)], GC[("swa_k_gain", j)]
            v_block(W, 2688, 128, (2, 64, 128, 0))
            wname, wt = wload([(W[:, 2560:2688], 0)])
            qk_pairs([(wname, wt, 0, gk, KA, ['KA0', 'KA1'])])
            for blk in range(2):
                wname, wt = wload([(W[:, 2048 + 256 * blk:2048 + 256 * blk + 256], 0)])
                for pp in range(2):
                    pair = 2 * blk + pp
                    qk_pairs([(wname, wt, 128 * pp, gq, QA, ['QA0', 'QA1'])])
                    kv = pair // 2
                    for hs in range(2):
                        hq = 2 * pair + hs
                        load_alibi(hs, hq + 1)
                        lf = lambda jj, kv=kv: VA[:, jj, kv * 128:(kv + 1) * 128]
                        segs = []
                        for g in range(4):
                            segs.append(dict(qslot=hs, kslot=kv, g=g, plan=SWA[g], lhsT_fns=[lf], bias_fn=None,
                                             qreads=['QA%d' % hs, 'QAm%d' % hs, 'QAa%d' % hs], kreads=['KA%d' % kv, 'KAaug%d' % kv], vreads=['VA'],
                                             done=(lambda Os, g=g, p=4 + pair, hs=hs, hq=hq: finalize_std(Os[0], p, hs, g, extra_den=esink[64:128, j, hq:hq + 1]))))
                        attn_seq(segs)
            out_proj(w_out_cd[j])

        def out_proj(Wo):
            for blk in range(4):
                wname, wt = wload([(Wo[:, 256 * blk:256 * blk + 256], 0)])
                for i in range(NT):
                    Mn, Mt = Mring.next()
                    for k in range(8):
                        P.op('pe', lambda e, k=k, i=i, Mt=Mt, wt=wt: e.matmul(Mt[:, 0:256], yT[:, k, i * 128:(i + 1) * 128], wt[:, k, :], start=(k == 0), stop=(k == 7)),
                             reads=[wname] + ['yT%d' % k for k in range(8)] if k == 0 else [wname], writes=[Mn])
                    xs = x_sb[:, i, 256 * blk:256 * blk + 256]
                    P.op('dve', lambda e, xs=xs, Mt=Mt: e.tensor_tensor(xs, xs, Mt[:, 0:256], ALU.add), reads=[Mn, 'x%d' % i], writes=['x%d' % i])

        for s in range(nseq):
            for i in range(NT):
                P.dma('sp', x_sb[:, i, :], xin[s, i * 128:(i + 1) * 128, :], 'xld%d' % i, reads=[], writes=['x%d' % i])
            for l in layers:
                if l % 2 == 0:
                    layer_ab(l)
                else:
                    layer_cd(l)
            for i in range(NT):
                P.dma('sp', yout[s, i * 128:(i + 1) * 128, :], x_sb[:, i, :], 'xst%d' % i, reads=['x%d' % i], writes=['out%d' % i])
        P.wait_all('sp', ['out%d' % i for i in range(NT)])
        P.emit()
    return nc


def make_consts():
    c = {}
    c["c_ident"] = np.eye(128, dtype=np.float32)
    bdm = np.zeros((128, 128), np.float32)
    bdm[0:64, 0:64] = 1.0 / 64
    bdm[64:128, 64:128] = 1.0 / 64
    c["c_bd"] = bdm
    c["c_a128"] = np.full((128, 128), 1.0 / 128, np.float32)
    s = np.arange(128)[:, None]
    t = np.arange(128)[None, :]
    c["c_tric"] = np.where(s <= t, 0.0, NEG).astype(np.float32)
    c["c_trip"] = np.where(s > t, 0.0, NEG).astype(np.float32)
    c["c_triu"] = (s <= t).astype(np.float32)
    pos = np.arange(S)
    kaug = np.zeros((12, S), np.float32)
    for n in range(8):
        kaug[n] = (pos // 256 == n)
    kaug[8] = 128 * (pos // 128)
    kaug[9] = pos % 128
    kaug[10] = 1.0
    kaug[11] = 1.0
    c["c_kaug"] = kaug
    qal = np.zeros((9, 4, S), np.float32)
    for i in range(1, 9):
        sl = 2.0 ** (-i)
        qal[i, 0] = sl
        qal[i, 1] = sl
        qal[i, 2] = -sl * 128 * (pos // 128)
        qal[i, 3] = -sl * (pos % 128)
    c["c_qalibi"] = qal
    cm = np.zeros((128, 16, 8), np.float32)
    for i in range(16):
        qb = i // 2
        for n in range(8):
            cm[:, i, n] = 0.0 if n < qb else (1e30 if n == qb else -1e30)
    c["c_cmask"] = cm.reshape(128, 128)
    return c


PARAMS = ["norm_gain", "w_in_ab", "b_forget", "moba_q_gain", "moba_k_gain", "fox_q_gain", "fox_k_gain", "w_out_ab", "w_in_cd",
          "diff_q_gain", "diff_k_gain", "diff_lambda", "diff_subln_gain", "swa_q_gain", "swa_k_gain", "swa_sinks", "w_out_cd"]


LAUNCH_GROUPS = [(0, 1, 2, 3)]


def kernel(**inputs):
    x = np.ascontiguousarray(np.asarray(inputs["x"], dtype=np.float32))
    n = 8
    per = x.shape[0] // n
    consts = make_consts()
    base = {k: np.ascontiguousarray(np.asarray(inputs[k], dtype=np.float32)) for k in PARAMS}
    base.update(consts)
    cur = x
    for grp in LAUNCH_GROUPS:
        nc = build(nseq=per, layers=grp)
        in_maps = []
        for c in range(n):
            m = dict(base)
            m["xin"] = np.ascontiguousarray(cur[c * per:(c + 1) * per])
            in_maps.append(m)
        res = run_bass_kernel_spmd(nc, in_maps, core_ids=list(range(n)))
        cur = np.concatenate([r["yout"] for r in res.results], axis=0)
    return cur
```

```python
import math
import numpy as np
from contextlib import ExitStack
import concourse.bass as bass
import concourse.mybir as mybir
from concourse.ap import AP
from concourse.bass_utils import run_bass_kernel_spmd

F32 = mybir.dt.float32
BF16 = mybir.dt.bfloat16
ALU = mybir.AluOpType
AF = mybir.ActivationFunctionType
AX = mybir.AxisListType

ENGS = ['pe', 'act', 'dve', 'pool', 'sp']
S = 2048
D = 1024
NT = 16
KR = 128
EPS = 1e-6
NEG = -30000.0


class Prog:
    def __init__(self, nc, stack):
        self.nc = nc
        self.stack = stack
        self.q = {k: [] for k in ENGS}
        self.cnt = {}
        self.sems = {}
        self.seen = {k: {} for k in ENGS}
        self.w = {}
        self.r = {}
        for k in ENGS:
            self._sem('c_' + k)

    def _sem(self, name):
        if name not in self.sems:
            self.sems[name] = self.stack.enter_context(self.nc.semaphore(name))
            self.cnt[name] = 0
        return self.sems[name]

    def sb(self, name, shape, dt):
        return self.stack.enter_context(self.nc.sbuf_tensor(name, shape, dt))

    def ps(self, name, shape, dt=F32):
        return self.stack.enter_context(self.nc.psum_tensor(name, shape, dt))

    def _deps(self, eng, reads, writes):
        deps = {}

        def add(sig):
            if sig is None:
                return
            s, v = sig
            if deps.get(s, 0) < v:
                deps[s] = v
        for r in reads:
            add(self.w.get(r))
        for w_ in writes:
            add(self.w.get(w_))
            for sig in self.r.get(w_, ()):
                add(sig)
        out = []
        own = 'c_' + eng
        for s, v in deps.items():
            if eng == 'pe' and s == own:
                continue
            if self.seen[eng].get(s, 0) < v:
                self.seen[eng][s] = v
                out.append((s, v))
        return out

    def _record(self, sig, reads, writes):
        for r in reads:
            lst = self.r.setdefault(r, [])
            lst[:] = [x for x in lst if x[0] != sig[0]] + [sig]
        for w_ in writes:
            self.w[w_] = sig
            self.r[w_] = []

    def op(self, eng, fn, reads=(), writes=()):
        waits = self._deps(eng, reads, writes)
        name = 'c_' + eng
        self.cnt[name] += 1
        sig = (name, self.cnt[name])
        self.q[eng].append((waits, fn, name, 1))
        self._record(sig, reads, writes)
        return sig

    def dma(self, eng, out, in_, sem, reads=(), writes=()):
        waits = self._deps(eng, reads, writes)
        self._sem(sem)
        self.cnt[sem] += 16
        sig = (sem, self.cnt[sem])
        self.q[eng].append((waits, lambda e: e.dma_start(out=out, in_=in_), sem, 16))
        self._record(sig, reads, writes)
        return sig

    def flush(self, sem, resources):
        for r in resources:
            self.w[r] = (sem, self.cnt[sem])

    def wait_all(self, eng, resources):
        waits = self._deps(eng, resources, ())
        self.q[eng].append((waits, None, None, 0))

    def emit(self):
        nc = self.nc
        names = {'pe': 'tensor', 'act': 'scalar', 'dve': 'vector', 'pool': 'gpsimd', 'sp': 'sync'}
        with nc.Block() as block:
            for k in ENGS:
                lst = self.q[k]
                if not lst:
                    continue

                def body(e, lst=lst):
                    for waits, fn, sname, inc in lst:
                        for s, v in waits:
                            e.wait_ge(self.sems[s], v)
                        if fn is not None:
                            fn(e).then_inc(self.sems[sname], inc)
                getattr(block, names[k])(body)


class Ring:
    def __init__(self, items):
        self.items = items
        self.i = 0

    def next(self):
        it = self.items[self.i % len(self.items)]
        self.i += 1
        return it


def alibi_slopes(n):
    return [2.0 ** (-8.0 * (i + 1) / n) for i in range(n)]


def bview(ap, pattern):
    p = ap.ap[0]
    return AP(ap.tensor, ap.offset, [[p[0], p[1]]] + [list(x) for x in pattern])


SKIP = set()


def build(nseq=2, layers=(0, 1, 2, 3), dbg=False, phase=(0, 0, 0, 0)):
    nc = bass.Bass("TRN2", target_bir_lowering=False)
    dt_in = lambda name, shape: nc.dram_tensor(name, list(shape), F32, kind="ExternalInput").ap()
    xin = dt_in("xin", [nseq, S, D])
    norm_gain = dt_in("norm_gain", [4, D])
    w_in_ab = dt_in("w_in_ab", [2, D, 4104])
    b_forget = dt_in("b_forget", [2, 8])
    w_out_ab = dt_in("w_out_ab", [2, D, D])
    w_in_cd = dt_in("w_in_cd", [2, D, 3328])
    w_out_cd = dt_in("w_out_cd", [2, D, D])
    hg = {n: dt_in(n, [2, 64]) for n in ["moba_q_gain", "moba_k_gain", "fox_q_gain", "fox_k_gain",
                                        "diff_q_gain", "diff_k_gain", "swa_q_gain", "swa_k_gain"]}
    diff_lambda = dt_in("diff_lambda", [2, 4, 64])
    diff_subln_gain = dt_in("diff_subln_gain", [2, 128])
    swa_sinks = dt_in("swa_sinks", [2, 8])
    c_ident = dt_in("c_ident", [128, 128])
    c_bd = dt_in("c_bd", [128, 128])
    c_a128 = dt_in("c_a128", [128, 128])
    c_tric = dt_in("c_tric", [128, 128])
    c_trip = dt_in("c_trip", [128, 128])
    c_triu = dt_in("c_triu", [128, 128])
    c_kaug = dt_in("c_kaug", [12, S])
    c_qalibi = dt_in("c_qalibi", [9, 4, S])
    c_cmask = dt_in("c_cmask", [128, 128])
    yout = nc.dram_tensor("yout", [nseq, S, D], F32, kind="ExternalOutput").ap()

    with ExitStack() as st:
        P = Prog(nc, st)
        x_sb = P.sb("x_sb", [128, NT, D], F32)
        hT = P.sb("hT", [128, 8, S], BF16)
        yT = P.sb("yT", [128, 8, S], BF16)
        NW = 3
        wbuf = [P.sb("wb%d" % i, [128, 8, 256], BF16) for i in range(NW)]
        QA = [P.sb("QA%d" % i, [128, S], BF16) for i in range(2)]
        KA = [P.sb("KA%d" % i, [128, S], BF16) for i in range(2)]
        VA = P.sb("VA", [128, NT, 512], BF16)
        NPR = 3
        Pt = [P.sb("Pt%d" % i, [128, 512], BF16) for i in range(NPR)]
        sqr = [P.sb("sq%d" % i, [128, 512], BF16) for i in range(2)]
        lnr = [P.sb("ln%d" % i, [128, 512], F32) for i in range(2)]
        rdr = [P.sb("rd%d" % i, [128, 512], F32) for i in range(1)]
        rgr = [P.sb("rg%d" % i, [128, 512], F32) for i in range(1)]
        n1t = P.sb("n1t", [128, 512], F32)
        n2t = P.sb("n2t", [128, 512], F32)
        gnb = P.sb("gnb", [128, D], F32)
        hrow = [P.sb("hrow%d" % i, [128, D], BF16) for i in range(1)]
        junk = hT[:, 0, 0:D]
        ssx = P.sb("ssx", [128, NT], F32)
        lnx = P.sb("lnx", [128, NT], F32)
        rsx = P.sb("rsx", [128, NT], F32)
        ident = P.sb("ident", [128, 128], BF16)
        bd = P.sb("bd", [128, 128], BF16)
        a128 = P.sb("a128", [128, 128], BF16)
        tric = P.sb("tric", [128, 128], BF16)
        trip = P.sb("trip", [128, 128], BF16)
        triu_f = P.sb("triu_b", [128, 128], BF16)
        ones_f = P.sb("ones_b", [128, 128], BF16)
        Lb = P.sb("Lb", [128, 3, 128], BF16)
        cmask = P.sb("cmask", [128, 128], F32)
        gcols = P.sb("gcols", [128, 20], F32)
        bfb = P.sb("bfb", [128, 2, 8], F32)
        lamb = n1t[:, :].rearrange("p (a b c) -> p a b c", a=2, b=4)
        lamw = n2t[:, 0:256].rearrange("p (a b c) -> p a b c", a=2, b=2)
        lams = P.sb("lams", [128, 2, 2], F32)
        lame = P.sb("lame", [128, 2, 2], F32)
        nlam = P.sb("nlam", [128, 2], F32)
        sinkb = P.sb("sinkb", [128, 2, 8], F32)
        esink = P.sb("esink", [128, 2, 8], F32)
        KM = P.sb("KM", [128, 16], BF16)
        kmf = P.sb("kmf", [128, 8], F32)
        kmf2 = P.sb("kmf2", [128, 8], F32)
        Gsb = P.sb("Gsb", [128, 256], F32)
        Gm = P.sb("Gm", [128, 128], F32)
        cmpt = gnb
        rank = P.sb("rank", [128, 128], F32)
        MT = P.sb("MT", [128, 128], BF16)
        zf = P.sb("zf", [128, 128], F32)
        Lf = P.sb("Lf", [128, 128], F32)
        cwt = P.sb("cwt", [128, 256], F32)
        cpre = P.sb("cpre", [128, 128], F32)
        cneg = P.sb("cneg", [128, 128], F32)
        cend = P.sb("cend", [128, 4, 8], F32)
        FB = P.sb("FB", [128, 8, NT, 4], F32)
        Sps = [P.ps("S%d" % i, [128, 512]) for i in range(2)]
        Ops = [P.ps("O%d" % i, [128, 512]) for i in range(3)]
        Mps = [P.ps("M%d" % i, [128, 512]) for i in range(2)]
        Tps = P.ps("Tps", [128, 1024], BF16)
        Sring = Ring([('S%d' % i, Sps[i]) for i in range(2)])
        Oring = Ring([('O%d' % i, Ops[i]) for i in range(3)])
        Mring = Ring([('M%d' % i, Mps[i]) for i in range(2)])
        M5ring = Ring([('M%d' % i, Mps[i]) for i in range(2)] + [('O%d' % i, Ops[i]) for i in range(3)])
        Pring = Ring([('Pt%d' % i, Pt[i]) for i in range(NPR)])
        sqring = Ring([('sq%d' % i, sqr[i]) for i in range(2)])
        lnring = Ring([('ln%d' % i, lnr[i]) for i in range(2)])
        rdring = Ring([('rd%d' % i, rdr[i]) for i in range(1)])
        rgring = Ring([('rg%d' % i, rgr[i]) for i in range(1)])
        hring = Ring([('hrow%d' % i, hrow[i]) for i in range(1)])
        Oring.i, Pring.i, Sring.i, Mring.i = phase

        P.dma('pool', ident[:], c_ident, 'cst', writes=['ident'])
        P.dma('pool', bd[:], c_bd, 'cst', writes=['bd'])
        P.dma('pool', a128[:], c_a128, 'cst', writes=['a128'])
        P.dma('pool', tric[:], c_tric, 'cst', writes=['tric'])
        P.dma('pool', trip[:], c_trip, 'cst', writes=['trip'])
        P.dma('pool', triu_f[:], c_triu, 'cst', writes=['triu_f'])
        P.dma('sp', cmask[:], c_cmask, 'cst2', writes=['cmask'])
        P.op('dve', lambda e: e.memset(ones_f[:], 1.0), writes=['ones_f'])
        for i in range(2):
            P.op('dve', lambda e, i=i: e.memset(QA[i][:], 0.0), writes=['QA%d' % i, 'QAm%d' % i, 'QAa%d' % i])
            P.op('dve', lambda e, i=i: e.memset(KA[i][:], 0.0), writes=['KA%d' % i, 'KAaug%d' % i])
            P.dma('pool', KA[i][64:76, :], c_kaug, 'cst', reads=[], writes=['KAaug%d' % i])
        gnames = ["moba_q_gain", "moba_k_gain", "fox_q_gain", "fox_k_gain", "diff_q_gain", "diff_k_gain", "swa_q_gain", "swa_k_gain"]
        GC = {}
        col = 0
        P.op('dve', lambda e: e.memset(gcols[:], 0.0), writes=['gcols'])
        for j in range(2):
            for n in gnames:
                GC[(n, j)] = col
                src = hg[n][j].rearrange("(p o) -> p o", o=1)
                P.dma('sp', gcols[0:64, col:col + 1], src, 'cstg', writes=['gcols'])
                P.dma('sp', gcols[64:128, col:col + 1], src, 'cstg', writes=['gcols'])
                col += 1
        for j in range(2):
            GC[('subln', j)] = col
            P.dma('sp', gcols[:, col:col + 1], diff_subln_gain[j].rearrange("(p o) -> p o", o=1), 'cstg', writes=['gcols'])
            col += 1
        P.flush('cstg', ['gcols'])
        for j in range(2):
            for n in gnames:
                if n.endswith("q_gain"):
                    c = GC[(n, j)]
                    P.op('dve', lambda e, c=c: e.tensor_scalar(gcols[:, c:c + 1], gcols[:, c:c + 1], 0.125, None, ALU.mult), reads=['gcols'], writes=['gcols'])
            layer = 2 * j + 1
            lam_init = 0.8 - 0.6 * math.exp(-0.3 * layer)
            c = GC[('subln', j)]
            P.op('dve', lambda e, c=c, v=1.0 - lam_init: e.tensor_scalar(gcols[:, c:c + 1], gcols[:, c:c + 1], float(v), None, ALU.mult), reads=['gcols'], writes=['gcols'])
        P.dma('sp', bfb[:].rearrange("p a b -> p (a b)"), b_forget.rearrange("(o a) b -> o (a b)", o=1).partition_broadcast(128), 'cst2', writes=['bfb'])
        P.dma('sp', sinkb[:].rearrange("p a b -> p (a b)"), swa_sinks.rearrange("(o a) b -> o (a b)", o=1).partition_broadcast(128), 'cst4', writes=['sinkb'])
        P.dma('sp', n1t[:, :], diff_lambda.rearrange("(o a) b c -> o (a b c)", o=1).partition_broadcast(128), 'cst3', writes=['n1t'])
        P.op('act', lambda e: e.activation(esink[:], sinkb[:], AF.Exp), reads=['sinkb'], writes=['esink'])
        P.op('dve', lambda e: e.tensor_tensor(lamw, bview(lamb[:, 0, 0, :], [[256, 2], [128, 2], [1, 64]]),
                                              bview(lamb[:, 0, 1, :], [[256, 2], [128, 2], [1, 64]]), ALU.mult), reads=['n1t'], writes=['n2t'])
        P.op('dve', lambda e: e.tensor_reduce(lams[:], lamw, AX.X, ALU.add), reads=['n2t'], writes=['lams'])
        P.op('act', lambda e: e.activation(lame[:], lams[:], AF.Exp), reads=['lams'], writes=['lame'])
        for j in range(2):
            lam_init = 0.8 - 0.6 * math.exp(-0.3 * (2 * j + 1))
            P.op('dve', lambda e, j=j, li=lam_init: e.scalar_tensor_tensor(nlam[:, j:j + 1], lame[:, j, 1:2], float(-li), lame[:, j, 0:1], ALU.add, ALU.subtract),
                 reads=['lame'], writes=['nlam'])

        P.flush('cst', ['ident', 'bd', 'a128', 'tric', 'trip', 'KAaug0', 'KAaug1', 'triu_f'])
        P.flush('cst2', ['cmask', 'bfb'])
        wstate = {'n': 0}

        def wload(parts):
            i = wstate['n'] % NW
            wstate['n'] += 1
            name = 'wb%d' % i
            for src, off in parts:
                wdt = src.shape[1]
                P.dma('pool', wbuf[i][:, :, off:off + wdt], src.rearrange("(k p) c -> p k c", p=128), 'w%d' % i, writes=[name])
            return name, wbuf[i]

        def rms_to_hT(l):
            P.dma('sp', gnb[:], norm_gain[l:l + 1, :].partition_broadcast(128), 'gnb', writes=['gnb'])
            for i in range(NT):
                P.op('act', lambda e, i=i: e.activation(junk, x_sb[:, i, :], AF.Square, accum_out=ssx[:, i:i + 1]), reads=['x%d' % i], writes=['hT', 'ssx'])
            P.op('act', lambda e: e.activation(lnx[:], ssx[:], AF.Ln, bias=EPS, scale=1.0 / D), reads=['ssx'], writes=['lnx'])
            P.op('act', lambda e: e.activation(rsx[:], lnx[:], AF.Exp, scale=-0.5), reads=['lnx'], writes=['rsx'])
            for i in range(NT):
                hn, ht = hring.next()
                P.op('dve', lambda e, i=i, ht=ht: e.scalar_tensor_tensor(ht[:], x_sb[:, i, :], rsx[:, i:i + 1], gnb[:], ALU.mult, ALU.mult),
                     reads=['x%d' % i, 'rsx', 'gnb'], writes=[hn])
                for k in range(8):
                    P.op('pe', lambda e, k=k, ht=ht: e.transpose(Tps[:, k * 128:(k + 1) * 128], ht[:, k * 128:(k + 1) * 128], ident[:]),
                         reads=[hn, 'ident'], writes=['Tps'])
                eng = 'dve'
                src = Tps[:, :].rearrange("p (k t) -> p k t", k=8)
                dst = hT[:, :, i * 128:(i + 1) * 128]
                if eng == 'dve':
                    P.op('dve', lambda e, src=src, dst=dst: e.tensor_copy(dst, src), reads=['Tps'], writes=['hT'])
                else:
                    P.op('act', lambda e, src=src, dst=dst: e.copy(dst, src), reads=['Tps'], writes=['hT'])

        def inproj_fm(wname, wt, woff, tc, Mn, Mt, M=128):
            for k in range(8):
                P.op('pe', lambda e, k=k: e.matmul(Mt[0:M, :], wt[:, k, woff:woff + M], hT[:, k, tc * 512:(tc + 1) * 512], start=(k == 0), stop=(k == 7)),
                     reads=[wname, 'hT'], writes=[Mn])

        def gates(W, gcols_list):
            for blk in range(4):
                wname, wt = wload([(W[:, gcols_list[2 * blk]:gcols_list[2 * blk] + 128], 0), (W[:, gcols_list[2 * blk + 1]:gcols_list[2 * blk + 1] + 128], 128)])
                for c in range(2):
                    p = 2 * blk + c
                    for tc in range(4):
                        Mn, Mt = Mring.next()
                        inproj_fm(wname, wt, c * 128, tc, Mn, Mt)
                        P.op('act', lambda e, Mt=Mt, p=p, tc=tc: e.activation(yT[:, p, tc * 512:(tc + 1) * 512], Mt[:], AF.Silu), reads=[Mn], writes=['yT%d' % p])

        def qk_pairs(jobs):
            blocks = [(job, tc) for job in jobs for tc in range(4)]
            st = {}

            def A1(b):
                (wname, wt, woff, gcol, dst, dstnames), tc = blocks[b]
                Mn, Mt = M5ring.next()
                inproj_fm(wname, wt, woff, tc, Mn, Mt)
                st[b] = [Mn, Mt]

            def A2(b):
                Mn, Mt = st[b]
                sn, sq = sqring.next()
                P.op('act', lambda e, Mt=Mt, sq=sq: e.activation(sq[:], Mt[:], AF.Square), reads=[Mn], writes=[sn])
                Sn, St = Sring.next()
                P.op('pe', lambda e, St=St, sq=sq: e.matmul(St[:], bd[:], sq[:], start=True, stop=True), reads=[sn, 'bd'], writes=[Sn])
                st[b] += [Sn, St]

            def B(b):
                (wname, wt, woff, gcol, dst, dstnames), tc = blocks[b]
                Mn, Mt, Sn, St = st.pop(b)
                ln_n, ln_t = lnring.next()
                P.op('act', lambda e, St=St, ln_t=ln_t: e.activation(ln_t[:], St[:], AF.Ln, bias=EPS), reads=[Sn], writes=[ln_n])
                P.op('act', lambda e, ln_t=ln_t: e.activation(ln_t[:], ln_t[:], AF.Exp, scale=-0.5), reads=[ln_n], writes=[ln_n])
                rn, rt = ln_n, ln_t
                cs = slice(tc * 512, (tc + 1) * 512)
                P.op('dve', lambda e, Mt=Mt, rt=rt, cs=cs, dst=dst, gcol=gcol: e.scalar_tensor_tensor(dst[0][0:64, cs], Mt[0:64, :], gcols[0:64, gcol:gcol + 1], rt[0:64, :], ALU.mult, ALU.mult),
                     reads=[Mn, rn, 'gcols'], writes=[dstnames[0]])
                P.op('dve', lambda e, Mt=Mt, rt=rt, cs=cs, dst=dst, gcol=gcol: e.scalar_tensor_tensor(dst[1][0:64, cs], Mt[64:128, :], gcols[64:128, gcol:gcol + 1], rt[64:128, :], ALU.mult, ALU.mult),
                     reads=[Mn, rn, 'gcols'], writes=[dstnames[1]])

            n = len(blocks)
            for t in range(n + 2):
                if t < n:
                    A1(t)
                if 0 <= t - 1 < n:
                    A2(t - 1)
                if 0 <= t - 2 < n:
                    B(t - 2)

        def load_alibi(hslot, sidx):
            P.dma('pool', QA[hslot][72:76, :], c_qalibi[sidx], 'qal%d' % hslot, writes=['QAa%d' % hslot])

        def dense_plan(w=None):
            plan = []
            for g in range(4):
                lst = []
                for j in range(4 * g + 4):
                    if w is not None and j + w < 4 * g:
                        continue
                    i_lo = max(j, 4 * g)
                    i_hi = 4 * g + 3 if w is None else min(4 * g + 3, j + w)
                    c0 = (i_lo - 4 * g) * 128
                    c1 = (i_hi - 4 * g + 1) * 128
                    masks = [(tric, 'tric', c0)] if j >= 4 * g else []
                    lst.append((j, c0, c1, masks))
                plan.append(lst)
            return plan

        def alibi_window(slope):
            w = int(math.ceil((50.0 / slope - 1.0) / 128.0))
            return None if w >= 15 else w

        def swa_plan():
            plan = []
            for g in range(4):
                lst = []
                for j in range(max(0, 4 * g - 1), 4 * g + 4):
                    masks = []
                    cols = []
                    for i, tab, tn in ((j, tric, 'tric'), (j + 1, trip, 'trip')):
                        if 4 * g <= i <= 4 * g + 3:
                            c = (i - 4 * g) * 128
                            masks.append((tab, tn, c))
                            cols.append(c)
                    lst.append((j, min(cols), max(cols) + 128, masks))
                plan.append(lst)
            return plan

        DENSE = dense_plan()
        DENSE_W = {}

        def dense_for(slope):
            w = alibi_window(slope)
            if w not in DENSE_W:
                DENSE_W[w] = dense_plan(w)
            return DENSE_W[w]
        SWA = swa_plan()

        def attn_seq(segments):
            items = []
            for si, seg in enumerate(segments):
                for it in seg['plan']:
                    items.append((si, it))

            def emit_qk(n):
                si, (j, c0, c1, masks) = items[n]
                seg = segments[si]
                qslot, kslot, g = seg['qslot'], seg['kslot'], seg['g']
                Sn, St = Sring.next()
                fm = True
                for tab, tn, c in masks:
                    P.op('pe', lambda e, St=St, tab=tab, c=c, fm=fm: e.matmul(St[:, c:c + 128], ident[:], tab[:], start=fm, stop=False, skip_group_check=True),
                         reads=['ident', tn], writes=[Sn])
                    fm = False
                P.op('pe', lambda e, St=St, j=j, c0=c0, c1=c1, fm=fm, qslot=qslot, kslot=kslot, g=g: e.matmul(
                    St[:, c0:c1], KA[kslot][0:KR, j * 128:(j + 1) * 128], QA[qslot][0:KR, g * 512 + c0:g * 512 + c1], start=fm, stop=True, skip_group_check=True),
                     reads=seg['qreads'] + seg['kreads'], writes=[Sn])
                return Sn, St

            state = {}
            nxt = emit_qk(0) if items else None
            for n, (si, (j, c0, c1, masks)) in enumerate(items):
                seg = segments[si]
                g = seg['g']
                Sn, St = nxt
                if n + 1 < len(items):
                    nxt = emit_qk(n + 1)
                if si not in state:
                    state[si] = ([Oring.next() for _ in seg['lhsT_fns']], [True] * len(seg['lhsT_fns']))
                Os, first = state[si]
                Pn, Ptile = Pring.next()
                b = seg['bias_fn'](j, g) if seg['bias_fn'] is not None else None
                if b is None:
                    P.op('act', lambda e, St=St, Ptile=Ptile, c0=c0, c1=c1: e.activation(Ptile[:, c0:c1], St[:, c0:c1], AF.Exp), reads=[Sn], writes=[Pn])
                else:
                    bap, bname = b
                    P.op('act', lambda e, St=St, Ptile=Ptile, c0=c0, c1=c1, bap=bap: e.activation(Ptile[:, c0:c1], St[:, c0:c1], AF.Exp, bias=bap), reads=[Sn, bname], writes=[Pn])
                for v, lf in enumerate(seg['lhsT_fns']):
                    On, Ot = Os[v]
                    P.op('pe', lambda e, Ot=Ot, lf=lf, j=j, c0=c0, c1=c1, Ptile=Ptile, f=first[v]: e.matmul(Ot[:, c0:c1], lf(j), Ptile[:, c0:c1], start=f, stop=False, skip_group_check=True),
                         reads=[Pn] + seg['vreads'], writes=[On])
                    first[v] = False
                last_of_seg = (n + 1 == len(items)) or (items[n + 1][0] != si)
                if last_of_seg:
                    seg['done'](Os)

        def finalize_std(O, p, hf, g, extra_den=None):
            On, Ot = O
            b0 = 64 * hf
            cs = slice(g * 512, (g + 1) * 512)
            rn, rt = rdring.next()
            if extra_den is not None:
                P.op('dve', lambda e: e.tensor_scalar(rt[b0:b0 + 64, :], Ot[64:128, :], extra_den, None, ALU.add), reads=[On, 'esink'], writes=[rn])
                P.op('dve', lambda e: e.reciprocal(rt[b0:b0 + 64, :], rt[b0:b0 + 64, :]), reads=[rn], writes=[rn])
            else:
                P.op('dve', lambda e: e.reciprocal(rt[b0:b0 + 64, :], Ot[64:128, :]), reads=[On], writes=[rn])
            gn, gt = rgring.next()
            P.op('pool', lambda e: e.tensor_tensor(gt[b0:b0 + 64, :], rt[b0:b0 + 64, :], yT[b0:b0 + 64, p, cs], ALU.mult), reads=[rn, 'yT%d' % p], writes=[gn])
            P.op('dve', lambda e: e.tensor_tensor(yT[b0:b0 + 64, p, cs], Ot[0:64, :], gt[b0:b0 + 64, :], ALU.mult), reads=[On, gn], writes=['yT%d' % p])

        def v_block(W, vbase, ncols, layout):
            wname, wt = wload([(W[:, vbase:vbase + ncols], 0)])
            for i in range(NT):
                Mn, Mt = Mring.next()
                for k in range(8):
                    P.op('pe', lambda e, k=k, i=i, Mt=Mt: e.matmul(Mt[:, 0:ncols], hT[:, k, i * 128:(i + 1) * 128], wt[:, k, 0:ncols], start=(k == 0), stop=(k == 7)),
                         reads=[wname, 'hT'], writes=[Mn])
                nh, w0, stride_dst, dst0 = layout
                src = Mt[:, 0:ncols].rearrange("p (h d) -> p h d", h=nh)
                dstv = bview(VA[:, i, dst0:dst0 + 1], [[stride_dst, nh], [1, w0]])
                P.op('dve', lambda e, src=src, dstv=dstv: e.tensor_copy(dstv, src), reads=[Mn], writes=['VA'])

        def set_va_ones(regions):
            for (c0, strd, n, wdt) in regions:
                v = bview(VA[:, 0, c0:c0 + 1], [[512, NT], [strd, n], [1, wdt]])
                P.op('pool', lambda e, v=v: e.memset(v, 1.0), writes=['VA'])

        def moba_masks(hs):
            qn, kn = 'QA%d' % hs, 'KA%d' % hs
            P.op('dve', lambda e: e.tensor_reduce(kmf[0:64, :], KA[hs][0:64, :].rearrange("p (n l) -> p n l", n=8), AX.X, ALU.add), reads=[kn], writes=['kmf'])
            P.op('dve', lambda e: e.tensor_copy(KM[0:64, 0:8], kmf[0:64, :]), reads=['kmf'], writes=['KM'])
            P.op('dve', lambda e: e.tensor_tensor(kmf2[0:64, :], kmf[0:64, :], KM[0:64, 0:8], ALU.subtract), reads=['kmf', 'KM'], writes=['kmf2'])
            P.op('dve', lambda e: e.tensor_copy(KM[0:64, 8:16], kmf2[0:64, :]), reads=['kmf2'], writes=['KM'])
            Mn, Mt = Mring.next()
            for i in range(NT):
                P.op('pe', lambda e, i=i, Mt=Mt: e.matmul(Mt[:, i * 16:(i + 1) * 16], QA[hs][0:64, i * 128:(i + 1) * 128], KM[0:64, :], start=(i == 0), stop=(i == NT - 1), skip_group_check=True),
                     reads=[qn, 'KM'], writes=[Mn])
            P.op('dve', lambda e, Mt=Mt: e.tensor_copy(Gsb[:], Mt[:, 0:256]), reads=[Mn], writes=['Gsb'])
            gv = Gsb[:].rearrange("p (i c) -> p i c", c=16)
            P.op('dve', lambda e: e.tensor_tensor(Gm[:].rearrange("p (i n) -> p i n", n=8), gv[:, :, 0:8], gv[:, :, 8:16], ALU.add), reads=['Gsb'], writes=['Gm'])
            P.op('dve', lambda e: e.tensor_tensor(Gm[:], Gm[:], cmask[:], ALU.add), reads=['Gm', 'cmask'], writes=['Gm'])
            in0 = bview(Gm[:, 0:1], [[8, NT], [0, 8], [1, 8]])
            in1 = bview(Gm[:, 0:1], [[8, NT], [1, 8], [0, 8]])
            P.op('dve', lambda e: e.tensor_tensor(cmpt[:].rearrange("p (i n m) -> p i n m", n=8, m=8), in0, in1, ALU.is_gt), reads=['Gm'], writes=['gnb'])
            P.op('dve', lambda e: e.tensor_reduce(rank[:].rearrange("p (i n) -> p i n", n=8), cmpt[:].rearrange("p (i n m) -> p i n m", n=8, m=8), AX.X, ALU.add), reads=['gnb'], writes=['rank'])
            P.op('dve', lambda e: e.tensor_scalar(MT[:], rank[:], 3.5, NEG, ALU.is_gt, ALU.mult), reads=['rank'], writes=['MT'])
            for half in range(2):
                for ii in range(8):
                    i = half * 8 + ii
                    P.op('pe', lambda e, i=i, ii=ii: e.transpose(Tps[0:8, ii * 128:(ii + 1) * 128], MT[:, i * 8:(i + 1) * 8], ident[:]), reads=['MT', 'ident'], writes=['Tps'])
                P.op('dve', lambda e, half=half: e.tensor_copy(QA[hs][64:72, half * 1024:(half + 1) * 1024], Tps[0:8, :]), reads=['Tps'], writes=['QAm%d' % hs])

        def fox_prep(W, j):
            wname, wt = wload([(W[:, 4096:4104], 0)])
            Mn, Mt = Mring.next()
            for i in range(NT):
                for k in range(8):
                    P.op('pe', lambda e, i=i, k=k, Mt=Mt: e.matmul(Mt[:, i * 8:(i + 1) * 8], hT[:, k, i * 128:(i + 1) * 128], wt[:, k, 0:8],
                                                                start=(i == 0 and k == 0), stop=(i == NT - 1 and k == 7), skip_group_check=True),
                         reads=[wname, 'hT'], writes=[Mn])
            bb = bview(bfb[:, j, 0:1], [[0, NT], [1, 8]])
            P.op('dve', lambda e, Mt=Mt: e.tensor_tensor(zf[:].rearrange("p (i h) -> p i h", h=8), Mt[:, 0:128].rearrange("p (i h) -> p i h", h=8), bb, ALU.add), reads=[Mn, 'bfb'], writes=['zf'])
            P.op('act', lambda e: e.activation(zf[:], zf[:], AF.Exp, scale=-1.0), reads=['zf'], writes=['zf'])
            P.op('act', lambda e: e.activation(Lf[:], zf[:], AF.Ln, bias=1.0), reads=['zf'], writes=['Lf'])
            Mn2, Mt2 = Mring.next()
            P.op('dve', lambda e: e.tensor_copy(Lb[:, 0, :], Lf[:]), reads=['Lf'], writes=['Lb'])
            P.op('dve', lambda e: e.tensor_tensor(zf[:], Lf[:], Lb[:, 0, :], ALU.subtract), reads=['Lf', 'Lb'], writes=['zf'])
            P.op('dve', lambda e: e.tensor_copy(Lb[:, 1, :], zf[:]), reads=['zf'], writes=['Lb'])
            P.op('dve', lambda e: e.tensor_tensor(Lf[:], zf[:], Lb[:, 1, :], ALU.subtract), reads=['zf', 'Lb'], writes=['Lf'])
            P.op('dve', lambda e: e.tensor_copy(Lb[:, 2, :], Lf[:]), reads=['Lf'], writes=['Lb'])
            for c in range(3):
                P.op('pe', lambda e, c=c: e.matmul(Mt2[:, 0:128], triu_f[:], Lb[:, c, :], start=(c == 0), stop=(c == 2), skip_group_check=True), reads=['triu_f', 'Lb'], writes=[Mn2])
            for c in range(3):
                P.op('pe', lambda e, c=c: e.matmul(Mt2[:, 128:256], ones_f[:], Lb[:, c, :], start=False, stop=(c == 2), skip_group_check=True), reads=['ones_f', 'Lb'], writes=[Mn2])
            P.op('act', lambda e: e.copy(cwt[:], Mt2[:, 0:256]), reads=[Mn2], writes=['cwt'])
            P.op('dve', lambda e: e.memset(cpre[:, 0:8], 0.0), writes=['cpre'])
            for i in range(1, NT):
                P.op('dve', lambda e, i=i: e.tensor_tensor(cpre[:, i * 8:(i + 1) * 8], cpre[:, (i - 1) * 8:i * 8], cwt[:, 128 + (i - 1) * 8:128 + i * 8], ALU.add),
                     reads=['cpre', 'cwt'], writes=['cpre'])
            P.op('dve', lambda e: e.tensor_tensor(cneg[:], cwt[:, 0:128], cpre[:], ALU.add), reads=['cwt', 'cpre'], writes=['cneg'])
            for g in range(4):
                i = 4 * g + 3
                P.op('dve', lambda e, g=g, i=i: e.tensor_tensor(cend[:, g, :], cpre[:, i * 8:(i + 1) * 8], cwt[:, 128 + i * 8:128 + (i + 1) * 8], ALU.add), reads=['cpre', 'cwt'], writes=['cend'])
            for g in range(4):
                outv = bview(FB[:, 0, 0, g:g + 1], [[NT * 4, 8], [4, NT]])
                in0 = bview(cneg[:, 0:1], [[1, 8], [8, NT]])
                in1 = bview(cend[:, g, 0:1], [[1, 8], [0, NT]])
                P.op('dve', lambda e, outv=outv, in0=in0, in1=in1: e.tensor_tensor(outv, in0, in1, ALU.subtract), reads=['cneg', 'cend'], writes=['FB'])

        def layer_ab(l):
            j = l // 2
            W = w_in_ab[j]
            rms_to_hT(l)
            gates(W, [1536 + 128 * p for p in range(4)] + [3584 + 128 * p for p in range(4)])
            set_va_ones([(64, 128, 4, 64)])
            for mixer in range(2):
                if l == 2 and ('m%d' % mixer) in SKIP:
                    continue
                qb, kb, vb = (0, 512, 1024) if mixer == 0 else (2048, 2560, 3072)
                gq = GC[("moba_q_gain" if mixer == 0 else "fox_q_gain", j)]
                gk = GC[("moba_k_gain" if mixer == 0 else "fox_k_gain", j)]
                if mixer == 1:
                    fox_prep(W, j)
                for sbt in range(2):
                    v_block(W, vb + 256 * sbt, 256, (4, 64, 128, 0))
                    for pp in range(2):
                        pair = 2 * sbt + pp
                        wname, wt = wload([(W[:, qb + 128 * pair:qb + 128 * pair + 128], 0), (W[:, kb + 128 * pair:kb + 128 * pair + 128], 128)])
                        qk_pairs([(wname, wt, 0, gq, QA, ['QA0', 'QA1']), (wname, wt, 128, gk, KA, ['KA0', 'KA1'])])
                        for hs in range(2):
                            h = 2 * pair + hs
                            if mixer == 0:
                                load_alibi(hs, h + 1)
                                moba_masks(hs)
                                bias_fn = None
                            else:
                                load_alibi(hs, 0)
                                if pair == 0:
                                    P.op('dve', lambda e, hs=hs: e.memset(QA[hs][64:72, :], 0.0), writes=['QAm%d' % hs])
                                bias_fn = (lambda jj, g, h=h: (FB[:, h, jj, g:g + 1], 'FB'))
                            hv = 2 * pp + hs
                            lf = lambda jj, hv=hv: VA[:, jj, hv * 128:(hv + 1) * 128]
                            plan = dense_for(2.0 ** -(h + 1)) if mixer == 0 else DENSE
                            segs = []
                            for g in range(4):
                                segs.append(dict(qslot=hs, kslot=hs, g=g, plan=plan[g], lhsT_fns=[lf], bias_fn=bias_fn,
                                                 qreads=['QA%d' % hs, 'QAm%d' % hs, 'QAa%d' % hs], kreads=['KA%d' % hs, 'KAaug%d' % hs], vreads=['VA'],
                                                 done=(lambda Os, g=g, p=4 * mixer + pair, hs=hs: finalize_std(Os[0], p, hs, g))))
                            attn_seq(segs)
            out_proj(w_out_ab[j])

        def layer_cd(l):
            j = l // 2
            W = w_in_cd[j]
            rms_to_hT(l)
            gates(W, [1536 + 128 * p for p in range(4)] + [2816 + 128 * p for p in range(4)])
            set_va_ones([(64, 256, 2, 128)])
            gq, gk = GC[("diff_q_gain", j)], GC[("diff_k_gain", j)]
            for hs in range(2):
                P.op('dve', lambda e, hs=hs: e.memset(QA[hs][64:72, :], 0.0), writes=['QAm%d' % hs])
            sub_c = GC[('subln', j)]
            for sbt in range(2):
                wname, wt = wload([(W[:, 1024 + 256 * sbt:1024 + 256 * sbt + 256], 0)])
                for i in range(NT):
                    Mn, Mt = Mring.next()
                    for k in range(8):
                        P.op('pe', lambda e, k=k, i=i, Mt=Mt, wt=wt: e.matmul(Mt[:, 0:256], hT[:, k, i * 128:(i + 1) * 128], wt[:, k, 0:256], start=(k == 0), stop=(k == 7)),
                             reads=[wname, 'hT'], writes=[Mn])
                    src = Mt[:, 0:256].rearrange("p (h c d) -> p h c d", h=2, c=2)
                    dstv = bview(VA[:, i, 0:1], [[256, 2], [192, 2], [1, 64]])
                    P.op('dve', lambda e, src=src, dstv=dstv: e.tensor_copy(dstv, src), reads=[Mn], writes=['VA'])
                for pp in range(2):
                    h = 2 * sbt + pp
                    wname, wt = wload([(W[:, 128 * h:128 * h + 128], 0), (W[:, 512 + 128 * h:512 + 128 * h + 128], 128)])
                    qk_pairs([(wname, wt, 0, gq, QA, ['QA0', 'QA1']), (wname, wt, 128, gk, KA, ['KA0', 'KA1'])])
                    for c in range(2):
                        load_alibi(c, 2 * h + 2)
                    lfs = [lambda jj, pp=pp: VA[:, jj, pp * 256:pp * 256 + 128], lambda jj, pp=pp: VA[:, jj, pp * 256 + 128:pp * 256 + 256]]
                    plan = dense_for(2.0 ** -(2 * h + 2))
                    def diff_comp_done(Os, c):
                        (On0, Ot0), (On1, Ot1) = Os
                        rn, rt = rdring.next()
                        P.op('dve', lambda e, rt=rt, Ot0=Ot0: e.reciprocal(rt[0:64, :], Ot0[64:128, :]), reads=[On0], writes=[rn])
                        P.op('dve', lambda e, rt=rt, Ot1=Ot1: e.reciprocal(rt[64:128, :], Ot1[0:64, :]), reads=[On1], writes=[rn])
                        dstt = n1t if c == 0 else n2t
                        dn = 'n1t' if c == 0 else 'n2t'
                        P.op('dve', lambda e, rt=rt, Ot0=Ot0, dstt=dstt: e.tensor_tensor(dstt[0:64, :], Ot0[0:64, :], rt[0:64, :], ALU.mult), reads=[On0, rn], writes=[dn])
                        P.op('dve', lambda e, rt=rt, Ot1=Ot1, dstt=dstt: e.tensor_tensor(dstt[64:128, :], Ot1[64:128, :], rt[64:128, :], ALU.mult), reads=[On1, rn], writes=[dn])

                    def diff_group_done(g, h):
                        cs = slice(g * 512, (g + 1) * 512)
                        P.op('dve', lambda e: e.scalar_tensor_tensor(n1t[:], n2t[:], nlam[:, j:j + 1], n1t[:], ALU.mult, ALU.add), reads=['n1t', 'n2t', 'nlam'], writes=['n1t'])
                        sn, sq = sqring.next()
                        P.op('act', lambda e, sq=sq: e.activation(sq[:], n1t[:], AF.Square), reads=['n1t'], writes=[sn])
                        Mn, Mt = Mring.next()
                        P.op('pe', lambda e, Mt=Mt, sq=sq: e.matmul(Mt[:], a128[:], sq[:], start=True, stop=True), reads=[sn, 'a128'], writes=[Mn])
                        ln_n, ln_t = lnring.next()
                        P.op('act', lambda e, Mt=Mt, ln_t=ln_t: e.activation(ln_t[:], Mt[:], AF.Ln, bias=EPS), reads=[Mn], writes=[ln_n])
                        P.op('act', lambda e, ln_t=ln_t: e.activation(ln_t[:], ln_t[:], AF.Exp, scale=-0.5), reads=[ln_n], writes=[ln_n])
                        gn, gt = rgring.next()
                        P.op('pool', lambda e, gt=gt, ln_t=ln_t, cs=cs, h=h: e.tensor_tensor(gt[:], ln_t[:], yT[:, h, cs], ALU.mult), reads=[ln_n, 'yT%d' % h], writes=[gn])
                        P.op('dve', lambda e, gt=gt, cs=cs, h=h: e.scalar_tensor_tensor(yT[:, h, cs], n1t[:], gcols[:, sub_c:sub_c + 1], gt[:], ALU.mult, ALU.mult),
                             reads=['n1t', gn, 'gcols'], writes=['yT%d' % h])

                    segs = []
                    for g in range(4):
                        for c in range(2):
                            def done(Os, g=g, c=c, h=h):
                                diff_comp_done(Os, c)
                                if c == 1:
                                    diff_group_done(g, h)
                            segs.append(dict(qslot=c, kslot=c, g=g, plan=plan[g], lhsT_fns=lfs, bias_fn=None,
                                             qreads=['QA%d' % c, 'QAm%d' % c, 'QAa%d' % c], kreads=['KA%d' % c, 'KAaug%d' % c], vreads=['VA'], done=done))
                    attn_seq(segs)
            set_va_ones([(64, 128, 2, 64)])
            gq, gk = GC[("swa_q_gain", j)], GC[("swa_k_gain", j)]
            v_block(W, 2688, 128, (2, 64, 128, 0))
            wname, wt = wload([(W[:, 2560:2688], 0)])
            qk_pairs([(wname, wt, 0, gk, KA, ['KA0', 'KA1'])])
            for blk in range(2):
                wname, wt = wload([(W[:, 2048 + 256 * blk:2048 + 256 * blk + 256], 0)])
                for pp in range(2):
                    pair = 2 * blk + pp
                    qk_pairs([(wname, wt, 128 * pp, gq, QA, ['QA0', 'QA1'])])
                    kv = pair // 2
                    for hs in range(2):
                        hq = 2 * pair + hs
                        load_alibi(hs, hq + 1)
                        lf = lambda jj, kv=kv: VA[:, jj, kv * 128:(kv + 1) * 128]
                        segs = []
                        for g in range(4):
                            segs.append(dict(qslot=hs, kslot=kv, g=g, plan=SWA[g], lhsT_fns=[lf], bias_fn=None,
                                             qreads=['QA%d' % hs, 'QAm%d' % hs, 'QAa%d' % hs], kreads=['KA%d' % kv, 'KAaug%d' % kv], vreads=['VA'],
                                             done=(lambda Os, g=g, p=4 + pair, hs=hs, hq=hq: finalize_std(Os[0], p, hs, g, extra_den=esink[64:128, j, hq:hq + 1]))))
                        attn_seq(segs)
            out_proj(w_out_cd[j])

        def out_proj(Wo):
            for blk in range(4):
                wname, wt = wload([(Wo[:, 256 * blk:256 * blk + 256], 0)])
                for i in range(NT):
                    Mn, Mt = Mring.next()
                    for k in range(8):
                        P.op('pe', lambda e, k=k, i=i, Mt=Mt, wt=wt: e.matmul(Mt[:, 0:256], yT[:, k, i * 128:(i + 1) * 128], wt[:, k, :], start=(k == 0), stop=(k == 7)),
                             reads=[wname] + ['yT%d' % k for k in range(8)] if k == 0 else [wname], writes=[Mn])
                    xs = x_sb[:, i, 256 * blk:256 * blk + 256]
                    P.op('dve', lambda e, xs=xs, Mt=Mt: e.tensor_tensor(xs, xs, Mt[:, 0:256], ALU.add), reads=[Mn, 'x%d' % i], writes=['x%d' % i])

        for s in range(nseq):
            for i in range(NT):
                P.dma('sp', x_sb[:, i, :], xin[s, i * 128:(i + 1) * 128, :], 'xld%d' % i, reads=[], writes=['x%d' % i])
            for l in layers:
                if l % 2 == 0:
                    layer_ab(l)
                else:
                    layer_cd(l)
            for i in range(NT):
                P.dma('sp', yout[s, i * 128:(i + 1) * 128, :], x_sb[:, i, :], 'xst%d' % i, reads=['x%d' % i], writes=['out%d' % i])
        P.wait_all('sp', ['out%d' % i for i in range(NT)])
        P.emit()
    return nc


def make_consts():
    c = {}
    c["c_ident"] = np.eye(128, dtype=np.float32)
    bdm = np.zeros((128, 128), np.float32)
    bdm[0:64, 0:64] = 1.0 / 64
    bdm[64:128, 64:128] = 1.0 / 64
    c["c_bd"] = bdm
    c["c_a128"] = np.full((128, 128), 1.0 / 128, np.float32)
    s = np.arange(128)[:, None]
    t = np.arange(128)[None, :]
    c["c_tric"] = np.where(s <= t, 0.0, NEG).astype(np.float32)
    c["c_trip"] = np.where(s > t, 0.0, NEG).astype(np.float32)
    c["c_triu"] = (s <= t).astype(np.float32)
    pos = np.arange(S)
    kaug = np.zeros((12, S), np.float32)
    for n in range(8):
        kaug[n] = (pos // 256 == n)
    kaug[8] = 128 * (pos // 128)
    kaug[9] = pos % 128
    kaug[10] = 1.0
    kaug[11] = 1.0
    c["c_kaug"] = kaug
    qal = np.zeros((9, 4, S), np.float32)
    for i in range(1, 9):
        sl = 2.0 ** (-i)
        qal[i, 0] = sl
        qal[i, 1] = sl
        qal[i, 2] = -sl * 128 * (pos // 128)
        qal[i, 3] = -sl * (pos % 128)
    c["c_qalibi"] = qal
    cm = np.zeros((128, 16, 8), np.float32)
    for i in range(16):
        qb = i // 2
        for n in range(8):
            cm[:, i, n] = 0.0 if n < qb else (1e30 if n == qb else -1e30)
    c["c_cmask"] = cm.reshape(128, 128)
    return c


PARAMS = ["norm_gain", "w_in_ab", "b_forget", "moba_q_gain", "moba_k_gain", "fox_q_gain", "fox_k_gain", "w_out_ab", "w_in_cd",
          "diff_q_gain", "diff_k_gain", "diff_lambda", "diff_subln_gain", "swa_q_gain", "swa_k_gain", "swa_sinks", "w_out_cd"]


LAUNCH_GROUPS = [(0, 1, 2, 3)]


def kernel(**inputs):
    x = np.ascontiguousarray(np.asarray(inputs["x"], dtype=np.float32))
    n = 8
    per = x.shape[0] // n
    consts = make_consts()
    base = {k: np.ascontiguousarray(np.asarray(inputs[k], dtype=np.float32)) for k in PARAMS}
    base.update(consts)
    cur = x
    for grp in LAUNCH_GROUPS:
        nc = build(nseq=per, layers=grp)
        in_maps = []
        for c in range(n):
            m = dict(base)
            m["xin"] = np.ascontiguousarray(cur[c * per:(c + 1) * per])
            in_maps.append(m)
        res = run_bass_kernel_spmd(nc, in_maps, core_ids=list(range(n)))
        cur = np.concatenate([r["yout"] for r in res.results], axis=0)
    return cur
```

```python
import math
import numpy as np
from contextlib import ExitStack
import concourse.bass as bass
import concourse.mybir as mybir
from concourse.ap import AP
from concourse.bass_utils import run_bass_kernel_spmd

F32 = mybir.dt.float32
BF16 = mybir.dt.bfloat16
ALU = mybir.AluOpType
AF = mybir.ActivationFunctionType
AX = mybir.AxisListType

ENGS = ['pe', 'act', 'dve', 'pool', 'sp']
S = 2048
D = 1024
NT = 16
KR = 128
EPS = 1e-6
NEG = -30000.0


class Prog:
    def __init__(self, nc, stack):
        self.nc = nc
        self.stack = stack
        self.q = {k: [] for k in ENGS}
        self.cnt = {}
        self.sems = {}
        self.seen = {k: {} for k in ENGS}
        self.w = {}
        self.r = {}
        for k in ENGS:
            self._sem('c_' + k)

    def _sem(self, name):
        if name not in self.sems:
            self.sems[name] = self.stack.enter_context(self.nc.semaphore(name))
            self.cnt[name] = 0
        return self.sems[name]

    def sb(self, name, shape, dt):
        return self.stack.enter_context(self.nc.sbuf_tensor(name, shape, dt))

    def ps(self, name, shape, dt=F32):
        return self.stack.enter_context(self.nc.psum_tensor(name, shape, dt))

    def _deps(self, eng, reads, writes):
        deps = {}

        def add(sig):
            if sig is None:
                return
            s, v = sig
            if deps.get(s, 0) < v:
                deps[s] = v
        for r in reads:
            add(self.w.get(r))
        for w_ in writes:
            add(self.w.get(w_))
            for sig in self.r.get(w_, ()):
                add(sig)
        out = []
        own = 'c_' + eng
        for s, v in deps.items():
            if eng == 'pe' and s == own:
                continue
            if self.seen[eng].get(s, 0) < v:
                self.seen[eng][s] = v
                out.append((s, v))
        return out

    def _record(self, sig, reads, writes):
        for r in reads:
            lst = self.r.setdefault(r, [])
            lst[:] = [x for x in lst if x[0] != sig[0]] + [sig]
        for w_ in writes:
            self.w[w_] = sig
            self.r[w_] = []

    def op(self, eng, fn, reads=(), writes=()):
        waits = self._deps(eng, reads, writes)
        name = 'c_' + eng
        self.cnt[name] += 1
        sig = (name, self.cnt[name])
        self.q[eng].append((waits, fn, name, 1))
        self._record(sig, reads, writes)
        return sig

    def dma(self, eng, out, in_, sem, reads=(), writes=()):
        waits = self._deps(eng, reads, writes)
        self._sem(sem)
        self.cnt[sem] += 16
        sig = (sem, self.cnt[sem])
        self.q[eng].append((waits, lambda e: e.dma_start(out=out, in_=in_), sem, 16))
        self._record(sig, reads, writes)
        return sig

    def flush(self, sem, resources):
        for r in resources:
            self.w[r] = (sem, self.cnt[sem])

    def wait_all(self, eng, resources):
        waits = self._deps(eng, resources, ())
        self.q[eng].append((waits, None, None, 0))

    def emit(self):
        nc = self.nc
        names = {'pe': 'tensor', 'act': 'scalar', 'dve': 'vector', 'pool': 'gpsimd', 'sp': 'sync'}
        with nc.Block() as block:
            for k in ENGS:
                lst = self.q[k]
                if not lst:
                    continue

                def body(e, lst=lst):
                    for waits, fn, sname, inc in lst:
                        for s, v in waits:
                            e.wait_ge(self.sems[s], v)
                        if fn is not None:
                            fn(e).then_inc(self.sems[sname], inc)
                getattr(block, names[k])(body)


class Ring:
    def __init__(self, items):
        self.items = items
        self.i = 0

    def next(self):
        it = self.items[self.i % len(self.items)]
        self.i += 1
        return it


def alibi_slopes(n):
    return [2.0 ** (-8.0 * (i + 1) / n) for i in range(n)]


def bview(ap, pattern):
    p = ap.ap[0]
    return AP(ap.tensor, ap.offset, [[p[0], p[1]]] + [list(x) for x in pattern])


SKIP = set()


def build(nseq=2, layers=(0, 1, 2, 3), dbg=False, phase=(0, 0, 0, 0)):
    nc = bass.Bass("TRN2", target_bir_lowering=False)
    dt_in = lambda name, shape: nc.dram_tensor(name, list(shape), F32, kind="ExternalInput").ap()
    xin = dt_in("xin", [nseq, S, D])
    norm_gain = dt_in("norm_gain", [4, D])
    w_in_ab = dt_in("w_in_ab", [2, D, 4104])
    b_forget = dt_in("b_forget", [2, 8])
    w_out_ab = dt_in("w_out_ab", [2, D, D])
    w_in_cd = dt_in("w_in_cd", [2, D, 3328])
    w_out_cd = dt_in("w_out_cd", [2, D, D])
    hg = {n: dt_in(n, [2, 64]) for n in ["moba_q_gain", "moba_k_gain", "fox_q_gain", "fox_k_gain",
                                        "diff_q_gain", "diff_k_gain", "swa_q_gain", "swa_k_gain"]}
    diff_lambda = dt_in("diff_lambda", [2, 4, 64])
    diff_subln_gain = dt_in("diff_subln_gain", [2, 128])
    swa_sinks = dt_in("swa_sinks", [2, 8])
    c_ident = dt_in("c_ident", [128, 128])
    c_bd = dt_in("c_bd", [128, 128])
    c_a128 = dt_in("c_a128", [128, 128])
    c_tric = dt_in("c_tric", [128, 128])
    c_trip = dt_in("c_trip", [128, 128])
    c_triu = dt_in("c_triu", [128, 128])
    c_kaug = dt_in("c_kaug", [12, S])
    c_qalibi = dt_in("c_qalibi", [9, 4, S])
    c_cmask = dt_in("c_cmask", [128, 128])
    yout = nc.dram_tensor("yout", [nseq, S, D], F32, kind="ExternalOutput").ap()

    with ExitStack() as st:
        P = Prog(nc, st)
        x_sb = P.sb("x_sb", [128, NT, D], F32)
        hT = P.sb("hT", [128, 8, S], BF16)
        yT = P.sb("yT", [128, 8, S], BF16)
        NW = 3
        wbuf = [P.sb("wb%d" % i, [128, 8, 256], BF16) for i in range(NW)]
        QA = [P.sb("QA%d" % i, [128, S], BF16) for i in range(2)]
        KA = [P.sb("KA%d" % i, [128, S], BF16) for i in range(2)]
        VA = P.sb("VA", [128, NT, 512], BF16)
        NPR = 3
        Pt = [P.sb("Pt%d" % i, [128, 512], BF16) for i in range(NPR)]
        sqr = [P.sb("sq%d" % i, [128, 512], BF16) for i in range(2)]
        lnr = [P.sb("ln%d" % i, [128, 512], F32) for i in range(2)]
        rdr = [P.sb("rd%d" % i, [128, 512], F32) for i in range(1)]
        rgr = [P.sb("rg%d" % i, [128, 512], F32) for i in range(1)]
        n1t = P.sb("n1t", [128, 512], F32)
        n2t = P.sb("n2t", [128, 512], F32)
        gnb = P.sb("gnb", [128, D], F32)
        hrow = [P.sb("hrow%d" % i, [128, D], BF16) for i in range(1)]
        junk = hT[:, 0, 0:D]
        ssx = P.sb("ssx", [128, NT], F32)
        lnx = P.sb("lnx", [128, NT], F32)
        rsx = P.sb("rsx", [128, NT], F32)
        ident = P.sb("ident", [128, 128], BF16)
        bd = P.sb("bd", [128, 128], BF16)
        a128 = P.sb("a128", [128, 128], BF16)
        tric = P.sb("tric", [128, 128], BF16)
        trip = P.sb("trip", [128, 128], BF16)
        triu_f = P.sb("triu_b", [128, 128], BF16)
        ones_f = P.sb("ones_b", [128, 128], BF16)
        Lb = P.sb("Lb", [128, 3, 128], BF16)
        cmask = P.sb("cmask", [128, 128], F32)
        gcols = P.sb("gcols", [128, 20], F32)
        bfb = P.sb("bfb", [128, 2, 8], F32)
        lamb = n1t[:, :].rearrange("p (a b c) -> p a b c", a=2, b=4)
        lamw = n2t[:, 0:256].rearrange("p (a b c) -> p a b c", a=2, b=2)
        lams = P.sb("lams", [128, 2, 2], F32)
        lame = P.sb("lame", [128, 2, 2], F32)
        nlam = P.sb("nlam", [128, 2], F32)
        sinkb = P.sb("sinkb", [128, 2, 8], F32)
        esink = P.sb("esink", [128, 2, 8], F32)
        KM = P.sb("KM", [128, 16], BF16)
        kmf = P.sb("kmf", [128, 8], F32)
        kmf2 = P.sb("kmf2", [128, 8], F32)
        Gsb = P.sb("Gsb", [128, 256], F32)
        Gm = P.sb("Gm", [128, 128], F32)
        cmpt = gnb
        rank = P.sb("rank", [128, 128], F32)
        MT = P.sb("MT", [128, 128], BF16)
        zf = P.sb("zf", [128, 128], F32)
        Lf = P.sb("Lf", [128, 128], F32)
        cwt = P.sb("cwt", [128, 256], F32)
        cpre = P.sb("cpre", [128, 128], F32)
        cneg = P.sb("cneg", [128, 128], F32)
        cend = P.sb("cend", [128, 4, 8], F32)
        FB = P.sb("FB", [128, 8, NT, 4], F32)
        Sps = [P.ps("S%d" % i, [128, 512]) for i in range(2)]
        Ops = [P.ps("O%d" % i, [128, 512]) for i in range(3)]
        Mps = [P.ps("M%d" % i, [128, 512]) for i in range(2)]
        Tps = P.ps("Tps", [128, 1024], BF16)
        Sring = Ring([('S%d' % i, Sps[i]) for i in range(2)])
        Oring = Ring([('O%d' % i, Ops[i]) for i in range(3)])
        Mring = Ring([('M%d' % i, Mps[i]) for i in range(2)])
        M5ring = Ring([('M%d' % i, Mps[i]) for i in range(2)] + [('O%d' % i, Ops[i]) for i in range(3)])
        Pring = Ring([('Pt%d' % i, Pt[i]) for i in range(NPR)])
        sqring = Ring([('sq%d' % i, sqr[i]) for i in range(2)])
        lnring = Ring([('ln%d' % i, lnr[i]) for i in range(2)])
        rdring = Ring([('rd%d' % i, rdr[i]) for i in range(1)])
        rgring = Ring([('rg%d' % i, rgr[i]) for i in range(1)])
        hring = Ring([('hrow%d' % i, hrow[i]) for i in range(1)])
        Oring.i, Pring.i, Sring.i, Mring.i = phase

        P.dma('pool', ident[:], c_ident, 'cst', writes=['ident'])
        P.dma('pool', bd[:], c_bd, 'cst', writes=['bd'])
        P.dma('pool', a128[:], c_a128, 'cst', writes=['a128'])
        P.dma('pool', tric[:], c_tric, 'cst', writes=['tric'])
        P.dma('pool', trip[:], c_trip, 'cst', writes=['trip'])
        P.dma('pool', triu_f[:], c_triu, 'cst', writes=['triu_f'])
        P.dma('sp', cmask[:], c_cmask, 'cst2', writes=['cmask'])
        P.op('dve', lambda e: e.memset(ones_f[:], 1.0), writes=['ones_f'])
        for i in range(2):
            P.op('dve', lambda e, i=i: e.memset(QA[i][:], 0.0), writes=['QA%d' % i, 'QAm%d' % i, 'QAa%d' % i])
            P.op('dve', lambda e, i=i: e.memset(KA[i][:], 0.0), writes=['KA%d' % i, 'KAaug%d' % i])
            P.dma('pool', KA[i][64:76, :], c_kaug, 'cst', reads=[], writes=['KAaug%d' % i])
        gnames = ["moba_q_gain", "moba_k_gain", "fox_q_gain", "fox_k_gain", "diff_q_gain", "diff_k_gain", "swa_q_gain", "swa_k_gain"]
        GC = {}
        col = 0
        P.op('dve', lambda e: e.memset(gcols[:], 0.0), writes=['gcols'])
        for j in range(2):
            for n in gnames:
                GC[(n, j)] = col
                src = hg[n][j].rearrange("(p o) -> p o", o=1)
                P.dma('sp', gcols[0:64, col:col + 1], src, 'cstg', writes=['gcols'])
                P.dma('sp', gcols[64:128, col:col + 1], src, 'cstg', writes=['gcols'])
                col += 1
        for j in range(2):
            GC[('subln', j)] = col
            P.dma('sp', gcols[:, col:col + 1], diff_subln_gain[j].rearrange("(p o) -> p o", o=1), 'cstg', writes=['gcols'])
            col += 1
        P.flush('cstg', ['gcols'])
        for j in range(2):
            for n in gnames:
                if n.endswith("q_gain"):
                    c = GC[(n, j)]
                    P.op('dve', lambda e, c=c: e.tensor_scalar(gcols[:, c:c + 1], gcols[:, c:c + 1], 0.125, None, ALU.mult), reads=['gcols'], writes=['gcols'])
            layer = 2 * j + 1
            lam_init = 0.8 - 0.6 * math.exp(-0.3 * layer)
            c = GC[('subln', j)]
            P.op('dve', lambda e, c=c, v=1.0 - lam_init: e.tensor_scalar(gcols[:, c:c + 1], gcols[:, c:c + 1], float(v), None, ALU.mult), reads=['gcols'], writes=['gcols'])
        P.dma('sp', bfb[:].rearrange("p a b -> p (a b)"), b_forget.rearrange("(o a) b -> o (a b)", o=1).partition_broadcast(128), 'cst2', writes=['bfb'])
        P.dma('sp', sinkb[:].rearrange("p a b -> p (a b)"), swa_sinks.rearrange("(o a) b -> o (a b)", o=1).partition_broadcast(128), 'cst4', writes=['sinkb'])
        P.dma('sp', n1t[:, :], diff_lambda.rearrange("(o a) b c -> o (a b c)", o=1).partition_broadcast(128), 'cst3', writes=['n1t'])
        P.op('act', lambda e: e.activation(esink[:], sinkb[:], AF.Exp), reads=['sinkb'], writes=['esink'])
        P.op('dve', lambda e: e.tensor_tensor(lamw, bview(lamb[:, 0, 0, :], [[256, 2], [128, 2], [1, 64]]),
                                              bview(lamb[:, 0, 1, :], [[256, 2], [128, 2], [1, 64]]), ALU.mult), reads=['n1t'], writes=['n2t'])
        P.op('dve', lambda e: e.tensor_reduce(lams[:], lamw, AX.X, ALU.add), reads=['n2t'], writes=['lams'])
        P.op('act', lambda e: e.activation(lame[:], lams[:], AF.Exp), reads=['lams'], writes=['lame'])
        for j in range(2):
            lam_init = 0.8 - 0.6 * math.exp(-0.3 * (2 * j + 1))
            P.op('dve', lambda e, j=j, li=lam_init: e.scalar_tensor_tensor(nlam[:, j:j + 1], lame[:, j, 1:2], float(-li), lame[:, j, 0:1], ALU.add, ALU.subtract),
                 reads=['lame'], writes=['nlam'])

        P.flush('cst', ['ident', 'bd', 'a128', 'tric', 'trip', 'KAaug0', 'KAaug1', 'triu_f'])
        P.flush('cst2', ['cmask', 'bfb'])
        wstate = {'n': 0}

        def wload(parts):
            i = wstate['n'] % NW
            wstate['n'] += 1
            name = 'wb%d' % i
            for src, off in parts:
                wdt = src.shape[1]
                P.dma('pool', wbuf[i][:, :, off:off + wdt], src.rearrange("(k p) c -> p k c", p=128), 'w%d' % i, writes=[name])
            return name, wbuf[i]

        def rms_to_hT(l):
            P.dma('sp', gnb[:], norm_gain[l:l + 1, :].partition_broadcast(128), 'gnb', writes=['gnb'])
            for i in range(NT):
                P.op('act', lambda e, i=i: e.activation(junk, x_sb[:, i, :], AF.Square, accum_out=ssx[:, i:i + 1]), reads=['x%d' % i], writes=['hT', 'ssx'])
            P.op('act', lambda e: e.activation(lnx[:], ssx[:], AF.Ln, bias=EPS, scale=1.0 / D), reads=['ssx'], writes=['lnx'])
            P.op('act', lambda e: e.activation(rsx[:], lnx[:], AF.Exp, scale=-0.5), reads=['lnx'], writes=['rsx'])
            for i in range(NT):
                hn, ht = hring.next()
                P.op('dve', lambda e, i=i, ht=ht: e.scalar_tensor_tensor(ht[:], x_sb[:, i, :], rsx[:, i:i + 1], gnb[:], ALU.mult, ALU.mult),
                     reads=['x%d' % i, 'rsx', 'gnb'], writes=[hn])
                for k in range(8):
                    P.op('pe', lambda e, k=k, ht=ht: e.transpose(Tps[:, k * 128:(k + 1) * 128], ht[:, k * 128:(k + 1) * 128], ident[:]),
                         reads=[hn, 'ident'], writes=['Tps'])
                eng = 'dve'
                src = Tps[:, :].rearrange("p (k t) -> p k t", k=8)
                dst = hT[:, :, i * 128:(i + 1) * 128]
                if eng == 'dve':
                    P.op('dve', lambda e, src=src, dst=dst: e.tensor_copy(dst, src), reads=['Tps'], writes=['hT'])
                else:
                    P.op('act', lambda e, src=src, dst=dst: e.copy(dst, src), reads=['Tps'], writes=['hT'])

        def inproj_fm(wname, wt, woff, tc, Mn, Mt, M=128):
            for k in range(8):
                P.op('pe', lambda e, k=k: e.matmul(Mt[0:M, :], wt[:, k, woff:woff + M], hT[:, k, tc * 512:(tc + 1) * 512], start=(k == 0), stop=(k == 7)),
                     reads=[wname, 'hT'], writes=[Mn])

        def gates(W, gcols_list):
            for blk in range(4):
                wname, wt = wload([(W[:, gcols_list[2 * blk]:gcols_list[2 * blk] + 128], 0), (W[:, gcols_list[2 * blk + 1]:gcols_list[2 * blk + 1] + 128], 128)])
                for c in range(2):
                    p = 2 * blk + c
                    for tc in range(4):
                        Mn, Mt = M5ring.next()
                        inproj_fm(wname, wt, c * 128, tc, Mn, Mt)
                        P.op('act', lambda e, Mt=Mt, p=p, tc=tc: e.activation(yT[:, p, tc * 512:(tc + 1) * 512], Mt[:], AF.Silu), reads=[Mn], writes=['yT%d' % p])

        def qk_pairs(jobs):
            blocks = [(job, tc) for job in jobs for tc in range(4)]
            st = {}

            def A1(b):
                (wname, wt, woff, gcol, dst, dstnames), tc = blocks[b]
                Mn, Mt = M5ring.next()
                inproj_fm(wname, wt, woff, tc, Mn, Mt)
                st[b] = [Mn, Mt]

            def A2(b):
                Mn, Mt = st[b]
                sn, sq = sqring.next()
                P.op('act', lambda e, Mt=Mt, sq=sq: e.activation(sq[:], Mt[:], AF.Square), reads=[Mn], writes=[sn])
                Sn, St = Sring.next()
                P.op('pe', lambda e, St=St, sq=sq: e.matmul(St[:], bd[:], sq[:], start=True, stop=True), reads=[sn, 'bd'], writes=[Sn])
                st[b] += [Sn, St]

            def B(b):
                (wname, wt, woff, gcol, dst, dstnames), tc = blocks[b]
                Mn, Mt, Sn, St = st.pop(b)
                ln_n, ln_t = lnring.next()
                P.op('act', lambda e, St=St, ln_t=ln_t: e.activation(ln_t[:], St[:], AF.Ln, bias=EPS), reads=[Sn], writes=[ln_n])
                P.op('act', lambda e, ln_t=ln_t: e.activation(ln_t[:], ln_t[:], AF.Exp, scale=-0.5), reads=[ln_n], writes=[ln_n])
                rn, rt = ln_n, ln_t
                cs = slice(tc * 512, (tc + 1) * 512)
                P.op('dve', lambda e, Mt=Mt, rt=rt, cs=cs, dst=dst, gcol=gcol: e.scalar_tensor_tensor(dst[0][0:64, cs], Mt[0:64, :], gcols[0:64, gcol:gcol + 1], rt[0:64, :], ALU.mult, ALU.mult),
                     reads=[Mn, rn, 'gcols'], writes=[dstnames[0]])
                P.op('dve', lambda e, Mt=Mt, rt=rt, cs=cs, dst=dst, gcol=gcol: e.scalar_tensor_tensor(dst[1][0:64, cs], Mt[64:128, :], gcols[64:128, gcol:gcol + 1], rt[64:128, :], ALU.mult, ALU.mult),
                     reads=[Mn, rn, 'gcols'], writes=[dstnames[1]])

            n = len(blocks)
            for t in range(n + 2):
                if t < n:
                    A1(t)
                if 0 <= t - 1 < n:
                    A2(t - 1)
                if 0 <= t - 2 < n:
                    B(t - 2)

        def load_alibi(hslot, sidx):
            P.dma('pool', QA[hslot][72:76, :], c_qalibi[sidx], 'qal%d' % hslot, writes=['QAa%d' % hslot])

        def dense_plan(w=None):
            plan = []
            for g in range(4):
                lst = []
                for j in range(4 * g + 4):
                    if w is not None and j + w < 4 * g:
                        continue
                    i_lo = max(j, 4 * g)
                    i_hi = 4 * g + 3 if w is None else min(4 * g + 3, j + w)
                    c0 = (i_lo - 4 * g) * 128
                    c1 = (i_hi - 4 * g + 1) * 128
                    masks = [(tric, 'tric', c0)] if j >= 4 * g else []
                    lst.append((j, c0, c1, masks))
                plan.append(lst)
            return plan

        def alibi_window(slope):
            w = int(math.ceil((50.0 / slope - 1.0) / 128.0))
            return None if w >= 15 else w

        def swa_plan():
            plan = []
            for g in range(4):
                lst = []
                for j in range(max(0, 4 * g - 1), 4 * g + 4):
                    masks = []
                    cols = []
                    for i, tab, tn in ((j, tric, 'tric'), (j + 1, trip, 'trip')):
                        if 4 * g <= i <= 4 * g + 3:
                            c = (i - 4 * g) * 128
                            masks.append((tab, tn, c))
                            cols.append(c)
                    lst.append((j, min(cols), max(cols) + 128, masks))
                plan.append(lst)
            return plan

        DENSE = dense_plan()
        DENSE_W = {}

        def dense_for(slope):
            w = alibi_window(slope)
            if w not in DENSE_W:
                DENSE_W[w] = dense_plan(w)
            return DENSE_W[w]
        SWA = swa_plan()

        def attn_seq(segments):
            items = []
            for si, seg in enumerate(segments):
                for it in seg['plan']:
                    items.append((si, it))

            def emit_qk(n):
                si, (j, c0, c1, masks) = items[n]
                seg = segments[si]
                qslot, kslot, g = seg['qslot'], seg['kslot'], seg['g']
                Sn, St = Sring.next()
                fm = True
                for tab, tn, c in masks:
                    P.op('pe', lambda e, St=St, tab=tab, c=c, fm=fm: e.matmul(St[:, c:c + 128], ident[:], tab[:], start=fm, stop=False, skip_group_check=True),
                         reads=['ident', tn], writes=[Sn])
                    fm = False
                P.op('pe', lambda e, St=St, j=j, c0=c0, c1=c1, fm=fm, qslot=qslot, kslot=kslot, g=g: e.matmul(
                    St[:, c0:c1], KA[kslot][0:KR, j * 128:(j + 1) * 128], QA[qslot][0:KR, g * 512 + c0:g * 512 + c1], start=fm, stop=True, skip_group_check=True),
                     reads=seg['qreads'] + seg['kreads'], writes=[Sn])
                return Sn, St

            state = {}
            nxt = emit_qk(0) if items else None
            for n, (si, (j, c0, c1, masks)) in enumerate(items):
                seg = segments[si]
                g = seg['g']
                Sn, St = nxt
                if n + 1 < len(items):
                    nxt = emit_qk(n + 1)
                if si not in state:
                    state[si] = ([Oring.next() for _ in seg['lhsT_fns']], [True] * len(seg['lhsT_fns']))
                Os, first = state[si]
                Pn, Ptile = Pring.next()
                b = seg['bias_fn'](j, g) if seg['bias_fn'] is not None else None
                if b is None:
                    P.op('act', lambda e, St=St, Ptile=Ptile, c0=c0, c1=c1: e.activation(Ptile[:, c0:c1], St[:, c0:c1], AF.Exp), reads=[Sn], writes=[Pn])
                else:
                    bap, bname = b
                    P.op('act', lambda e, St=St, Ptile=Ptile, c0=c0, c1=c1, bap=bap: e.activation(Ptile[:, c0:c1], St[:, c0:c1], AF.Exp, bias=bap), reads=[Sn, bname], writes=[Pn])
                for v, lf in enumerate(seg['lhsT_fns']):
                    On, Ot = Os[v]
                    P.op('pe', lambda e, Ot=Ot, lf=lf, j=j, c0=c0, c1=c1, Ptile=Ptile, f=first[v]: e.matmul(Ot[:, c0:c1], lf(j), Ptile[:, c0:c1], start=f, stop=False, skip_group_check=True),
                         reads=[Pn] + seg['vreads'], writes=[On])
                    first[v] = False
                last_of_seg = (n + 1 == len(items)) or (items[n + 1][0] != si)
                if last_of_seg:
                    seg['done'](Os)

        def finalize_std(O, p, hf, g, extra_den=None):
            On, Ot = O
            b0 = 64 * hf
            cs = slice(g * 512, (g + 1) * 512)
            rn, rt = rdring.next()
            if extra_den is not None:
                P.op('dve', lambda e: e.tensor_scalar(rt[b0:b0 + 64, :], Ot[64:128, :], extra_den, None, ALU.add), reads=[On, 'esink'], writes=[rn])
                P.op('dve', lambda e: e.reciprocal(rt[b0:b0 + 64, :], rt[b0:b0 + 64, :]), reads=[rn], writes=[rn])
            else:
                P.op('dve', lambda e: e.reciprocal(rt[b0:b0 + 64, :], Ot[64:128, :]), reads=[On], writes=[rn])
            gn, gt = rgring.next()
            P.op('pool', lambda e: e.tensor_tensor(gt[b0:b0 + 64, :], rt[b0:b0 + 64, :], yT[b0:b0 + 64, p, cs], ALU.mult), reads=[rn, 'yT%d' % p], writes=[gn])
            P.op('dve', lambda e: e.tensor_tensor(yT[b0:b0 + 64, p, cs], Ot[0:64, :], gt[b0:b0 + 64, :], ALU.mult), reads=[On, gn], writes=['yT%d' % p])

        def v_block(W, vbase, ncols, layout):
            wname, wt = wload([(W[:, vbase:vbase + ncols], 0)])
            for i in range(NT):
                Mn, Mt = M5ring.next()
                for k in range(8):
                    P.op('pe', lambda e, k=k, i=i, Mt=Mt: e.matmul(Mt[:, 0:ncols], hT[:, k, i * 128:(i + 1) * 128], wt[:, k, 0:ncols], start=(k == 0), stop=(k == 7)),
                         reads=[wname, 'hT'], writes=[Mn])
                nh, w0, stride_dst, dst0 = layout
                src = Mt[:, 0:ncols].rearrange("p (h d) -> p h d", h=nh)
                dstv = bview(VA[:, i, dst0:dst0 + 1], [[stride_dst, nh], [1, w0]])
                P.op('dve', lambda e, src=src, dstv=dstv: e.tensor_copy(dstv, src), reads=[Mn], writes=['VA'])

        def set_va_ones(regions):
            for (c0, strd, n, wdt) in regions:
                v = bview(VA[:, 0, c0:c0 + 1], [[512, NT], [strd, n], [1, wdt]])
                P.op('pool', lambda e, v=v: e.memset(v, 1.0), writes=['VA'])

        def moba_masks(hs):
            qn, kn = 'QA%d' % hs, 'KA%d' % hs
            P.op('dve', lambda e: e.tensor_reduce(kmf[0:64, :], KA[hs][0:64, :].rearrange("p (n l) -> p n l", n=8), AX.X, ALU.add), reads=[kn], writes=['kmf'])
            P.op('dve', lambda e: e.tensor_copy(KM[0:64, 0:8], kmf[0:64, :]), reads=['kmf'], writes=['KM'])
            P.op('dve', lambda e: e.tensor_tensor(kmf2[0:64, :], kmf[0:64, :], KM[0:64, 0:8], ALU.subtract), reads=['kmf', 'KM'], writes=['kmf2'])
            P.op('dve', lambda e: e.tensor_copy(KM[0:64, 8:16], kmf2[0:64, :]), reads=['kmf2'], writes=['KM'])
            Mn, Mt = Mring.next()
            for i in range(NT):
                P.op('pe', lambda e, i=i, Mt=Mt: e.matmul(Mt[:, i * 16:(i + 1) * 16], QA[hs][0:64, i * 128:(i + 1) * 128], KM[0:64, :], start=(i == 0), stop=(i == NT - 1), skip_group_check=True),
                     reads=[qn, 'KM'], writes=[Mn])
            P.op('dve', lambda e, Mt=Mt: e.tensor_copy(Gsb[:], Mt[:, 0:256]), reads=[Mn], writes=['Gsb'])
            gv = Gsb[:].rearrange("p (i c) -> p i c", c=16)
            P.op('dve', lambda e: e.tensor_tensor(Gm[:].rearrange("p (i n) -> p i n", n=8), gv[:, :, 0:8], gv[:, :, 8:16], ALU.add), reads=['Gsb'], writes=['Gm'])
            P.op('dve', lambda e: e.tensor_tensor(Gm[:], Gm[:], cmask[:], ALU.add), reads=['Gm', 'cmask'], writes=['Gm'])
            in0 = bview(Gm[:, 0:1], [[8, NT], [0, 8], [1, 8]])
            in1 = bview(Gm[:, 0:1], [[8, NT], [1, 8], [0, 8]])
            P.op('dve', lambda e: e.tensor_tensor(cmpt[:].rearrange("p (i n m) -> p i n m", n=8, m=8), in0, in1, ALU.is_gt), reads=['Gm'], writes=['gnb'])
            P.op('dve', lambda e: e.tensor_reduce(rank[:].rearrange("p (i n) -> p i n", n=8), cmpt[:].rearrange("p (i n m) -> p i n m", n=8, m=8), AX.X, ALU.add), reads=['gnb'], writes=['rank'])
            P.op('dve', lambda e: e.tensor_scalar(MT[:], rank[:], 3.5, NEG, ALU.is_gt, ALU.mult), reads=['rank'], writes=['MT'])
            for half in range(2):
                for ii in range(8):
                    i = half * 8 + ii
                    P.op('pe', lambda e, i=i, ii=ii: e.transpose(Tps[0:8, ii * 128:(ii + 1) * 128], MT[:, i * 8:(i + 1) * 8], ident[:]), reads=['MT', 'ident'], writes=['Tps'])
                P.op('dve', lambda e, half=half: e.tensor_copy(QA[hs][64:72, half * 1024:(half + 1) * 1024], Tps[0:8, :]), reads=['Tps'], writes=['QAm%d' % hs])

        def fox_prep(W, j):
            wname, wt = wload([(W[:, 4096:4104], 0)])
            Mn, Mt = Mring.next()
            for i in range(NT):
                for k in range(8):
                    P.op('pe', lambda e, i=i, k=k, Mt=Mt: e.matmul(Mt[:, i * 8:(i + 1) * 8], hT[:, k, i * 128:(i + 1) * 128], wt[:, k, 0:8],
                                                                start=(i == 0 and k == 0), stop=(i == NT - 1 and k == 7), skip_group_check=True),
                         reads=[wname, 'hT'], writes=[Mn])
            bb = bview(bfb[:, j, 0:1], [[0, NT], [1, 8]])
            P.op('dve', lambda e, Mt=Mt: e.tensor_tensor(zf[:].rearrange("p (i h) -> p i h", h=8), Mt[:, 0:128].rearrange("p (i h) -> p i h", h=8), bb, ALU.add), reads=[Mn, 'bfb'], writes=['zf'])
            P.op('act', lambda e: e.activation(zf[:], zf[:], AF.Exp, scale=-1.0), reads=['zf'], writes=['zf'])
            P.op('act', lambda e: e.activation(Lf[:], zf[:], AF.Ln, bias=1.0), reads=['zf'], writes=['Lf'])
            Mn2, Mt2 = Mring.next()
            P.op('dve', lambda e: e.tensor_copy(Lb[:, 0, :], Lf[:]), reads=['Lf'], writes=['Lb'])
            P.op('dve', lambda e: e.tensor_tensor(zf[:], Lf[:], Lb[:, 0, :], ALU.subtract), reads=['Lf', 'Lb'], writes=['zf'])
            P.op('dve', lambda e: e.tensor_copy(Lb[:, 1, :], zf[:]), reads=['zf'], writes=['Lb'])
            P.op('dve', lambda e: e.tensor_tensor(Lf[:], zf[:], Lb[:, 1, :], ALU.subtract), reads=['zf', 'Lb'], writes=['Lf'])
            P.op('dve', lambda e: e.tensor_copy(Lb[:, 2, :], Lf[:]), reads=['Lf'], writes=['Lb'])
            for c in range(3):
                P.op('pe', lambda e, c=c: e.matmul(Mt2[:, 0:128], triu_f[:], Lb[:, c, :], start=(c == 0), stop=(c == 2), skip_group_check=True), reads=['triu_f', 'Lb'], writes=[Mn2])
            for c in range(3):
                P.op('pe', lambda e, c=c: e.matmul(Mt2[:, 128:256], ones_f[:], Lb[:, c, :], start=False, stop=(c == 2), skip_group_check=True), reads=['ones_f', 'Lb'], writes=[Mn2])
            P.op('act', lambda e: e.copy(cwt[:], Mt2[:, 0:256]), reads=[Mn2], writes=['cwt'])
            P.op('dve', lambda e: e.memset(cpre[:, 0:8], 0.0), writes=['cpre'])
            for i in range(1, NT):
                P.op('dve', lambda e, i=i: e.tensor_tensor(cpre[:, i * 8:(i + 1) * 8], cpre[:, (i - 1) * 8:i * 8], cwt[:, 128 + (i - 1) * 8:128 + i * 8], ALU.add),
                     reads=['cpre', 'cwt'], writes=['cpre'])
            P.op('dve', lambda e: e.tensor_tensor(cneg[:], cwt[:, 0:128], cpre[:], ALU.add), reads=['cwt', 'cpre'], writes=['cneg'])
            for g in range(4):
                i = 4 * g + 3
                P.op('dve', lambda e, g=g, i=i: e.tensor_tensor(cend[:, g, :], cpre[:, i * 8:(i + 1) * 8], cwt[:, 128 + i * 8:128 + (i + 1) * 8], ALU.add), reads=['cpre', 'cwt'], writes=['cend'])
            for g in range(4):
                outv = bview(FB[:, 0, 0, g:g + 1], [[NT * 4, 8], [4, NT]])
                in0 = bview(cneg[:, 0:1], [[1, 8], [8, NT]])
                in1 = bview(cend[:, g, 0:1], [[1, 8], [0, NT]])
                P.op('dve', lambda e, outv=outv, in0=in0, in1=in1: e.tensor_tensor(outv, in0, in1, ALU.subtract), reads=['cneg', 'cend'], writes=['FB'])

        def layer_ab(l):
            j = l // 2
            W = w_in_ab[j]
            rms_to_hT(l)
            gates(W, [1536 + 128 * p for p in range(4)] + [3584 + 128 * p for p in range(4)])
            set_va_ones([(64, 128, 4, 64)])
            for mixer in range(2):
                if l == 2 and ('m%d' % mixer) in SKIP:
                    continue
                qb, kb, vb = (0, 512, 1024) if mixer == 0 else (2048, 2560, 3072)
                gq = GC[("moba_q_gain" if mixer == 0 else "fox_q_gain", j)]
                gk = GC[("moba_k_gain" if mixer == 0 else "fox_k_gain", j)]
                if mixer == 1:
                    fox_prep(W, j)
                for sbt in range(2):
                    v_block(W, vb + 256 * sbt, 256, (4, 64, 128, 0))
                    for pp in range(2):
                        pair = 2 * sbt + pp
                        wname, wt = wload([(W[:, qb + 128 * pair:qb + 128 * pair + 128], 0), (W[:, kb + 128 * pair:kb + 128 * pair + 128], 128)])
                        qk_pairs([(wname, wt, 0, gq, QA, ['QA0', 'QA1']), (wname, wt, 128, gk, KA, ['KA0', 'KA1'])])
                        for hs in range(2):
                            h = 2 * pair + hs
                            if mixer == 0:
                                load_alibi(hs, h + 1)
                                moba_masks(hs)
                                bias_fn = None
                            else:
                                load_alibi(hs, 0)
                                if pair == 0:
                                    P.op('dve', lambda e, hs=hs: e.memset(QA[hs][64:72, :], 0.0), writes=['QAm%d' % hs])
                                bias_fn = (lambda jj, g, h=h: (FB[:, h, jj, g:g + 1], 'FB'))
                            hv = 2 * pp + hs
                            lf = lambda jj, hv=hv: VA[:, jj, hv * 128:(hv + 1) * 128]
                            plan = dense_for(2.0 ** -(h + 1)) if mixer == 0 else DENSE
                            segs = []
                            for g in range(4):
                                segs.append(dict(qslot=hs, kslot=hs, g=g, plan=plan[g], lhsT_fns=[lf], bias_fn=bias_fn,
                                                 qreads=['QA%d' % hs, 'QAm%d' % hs, 'QAa%d' % hs], kreads=['KA%d' % hs, 'KAaug%d' % hs], vreads=['VA'],
                                                 done=(lambda Os, g=g, p=4 * mixer + pair, hs=hs: finalize_std(Os[0], p, hs, g))))
                            attn_seq(segs)
            out_proj(w_out_ab[j])

        def layer_cd(l):
            j = l // 2
            W = w_in_cd[j]
            rms_to_hT(l)
            gates(W, [1536 + 128 * p for p in range(4)] + [2816 + 128 * p for p in range(4)])
            set_va_ones([(64, 256, 2, 128)])
            gq, gk = GC[("diff_q_gain", j)], GC[("diff_k_gain", j)]
            for hs in range(2):
                P.op('dve', lambda e, hs=hs: e.memset(QA[hs][64:72, :], 0.0), writes=['QAm%d' % hs])
            sub_c = GC[('subln', j)]
            for sbt in range(2):
                wname, wt = wload([(W[:, 1024 + 256 * sbt:1024 + 256 * sbt + 256], 0)])
                for i in range(NT):
                    Mn, Mt = Mring.next()
                    for k in range(8):
                        P.op('pe', lambda e, k=k, i=i, Mt=Mt, wt=wt: e.matmul(Mt[:, 0:256], hT[:, k, i * 128:(i + 1) * 128], wt[:, k, 0:256], start=(k == 0), stop=(k == 7)),
                             reads=[wname, 'hT'], writes=[Mn])
                    src = Mt[:, 0:256].rearrange("p (h c d) -> p h c d", h=2, c=2)
                    dstv = bview(VA[:, i, 0:1], [[256, 2], [192, 2], [1, 64]])
                    P.op('dve', lambda e, src=src, dstv=dstv: e.tensor_copy(dstv, src), reads=[Mn], writes=['VA'])
                for pp in range(2):
                    h = 2 * sbt + pp
                    wname, wt = wload([(W[:, 128 * h:128 * h + 128], 0), (W[:, 512 + 128 * h:512 + 128 * h + 128], 128)])
                    qk_pairs([(wname, wt, 0, gq, QA, ['QA0', 'QA1']), (wname, wt, 128, gk, KA, ['KA0', 'KA1'])])
                    for c in range(2):
                        load_alibi(c, 2 * h + 2)
                    lfs = [lambda jj, pp=pp: VA[:, jj, pp * 256:pp * 256 + 128], lambda jj, pp=pp: VA[:, jj, pp * 256 + 128:pp * 256 + 256]]
                    plan = dense_for(2.0 ** -(2 * h + 2))
                    def diff_comp_done(Os, c):
                        (On0, Ot0), (On1, Ot1) = Os
                        rn, rt = rdring.next()
                        P.op('dve', lambda e, rt=rt, Ot0=Ot0: e.reciprocal(rt[0:64, :], Ot0[64:128, :]), reads=[On0], writes=[rn])
                        P.op('dve', lambda e, rt=rt, Ot1=Ot1: e.reciprocal(rt[64:128, :], Ot1[0:64, :]), reads=[On1], writes=[rn])
                        dstt = n1t if c == 0 else n2t
                        dn = 'n1t' if c == 0 else 'n2t'
                        P.op('dve', lambda e, rt=rt, Ot0=Ot0, dstt=dstt: e.tensor_tensor(dstt[0:64, :], Ot0[0:64, :], rt[0:64, :], ALU.mult), reads=[On0, rn], writes=[dn])
                        P.op('dve', lambda e, rt=rt, Ot1=Ot1, dstt=dstt: e.tensor_tensor(dstt[64:128, :], Ot1[64:128, :], rt[64:128, :], ALU.mult), reads=[On1, rn], writes=[dn])

                    def diff_group_done(g, h):
                        cs = slice(g * 512, (g + 1) * 512)
                        P.op('dve', lambda e: e.scalar_tensor_tensor(n1t[:], n2t[:], nlam[:, j:j + 1], n1t[:], ALU.mult, ALU.add), reads=['n1t', 'n2t', 'nlam'], writes=['n1t'])
                        sn, sq = sqring.next()
                        P.op('act', lambda e, sq=sq: e.activation(sq[:], n1t[:], AF.Square), reads=['n1t'], writes=[sn])
                        Mn, Mt = Mring.next()
                        P.op('pe', lambda e, Mt=Mt, sq=sq: e.matmul(Mt[:], a128[:], sq[:], start=True, stop=True), reads=[sn, 'a128'], writes=[Mn])
                        ln_n, ln_t = lnring.next()
                        P.op('act', lambda e, Mt=Mt, ln_t=ln_t: e.activation(ln_t[:], Mt[:], AF.Ln, bias=EPS), reads=[Mn], writes=[ln_n])
                        P.op('act', lambda e, ln_t=ln_t: e.activation(ln_t[:], ln_t[:], AF.Exp, scale=-0.5), reads=[ln_n], writes=[ln_n])
                        gn, gt = rgring.next()
                        P.op('pool', lambda e, gt=gt, ln_t=ln_t, cs=cs, h=h: e.tensor_tensor(gt[:], ln_t[:], yT[:, h, cs], ALU.mult), reads=[ln_n, 'yT%d' % h], writes=[gn])
                        P.op('dve', lambda e, gt=gt, cs=cs, h=h: e.scalar_tensor_tensor(yT[:, h, cs], n1t[:], gcols[:, sub_c:sub_c + 1], gt[:], ALU.mult, ALU.mult),
                             reads=['n1t', gn, 'gcols'], writes=['yT%d' % h])

                    segs = []
                    for g in range(4):
                        for c in range(2):
                            def done(Os, g=g, c=c, h=h):
                                diff_comp_done(Os, c)
                                if c == 1:
                                    diff_group_done(g, h)
                            segs.append(dict(qslot=c, kslot=c, g=g, plan=plan[g], lhsT_fns=lfs, bias_fn=None,
                                             qreads=['QA%d' % c, 'QAm%d' % c, 'QAa%d' % c], kreads=['KA%d' % c, 'KAaug%d' % c], vreads=['VA'], done=done))
                    attn_seq(segs)
            set_va_ones([(64, 128, 2, 64)])
            gq, gk = GC[("swa_q_gain", j)], GC[("swa_k_gain", j)]
            v_block(W, 2688, 128, (2, 64, 128, 0))
            wname, wt = wload([(W[:, 2560:2688], 0)])
            qk_pairs([(wname, wt, 0, gk, KA, ['KA0', 'KA1'])])
            for blk in range(2):
                wname, wt = wload([(W[:, 2048 + 256 * blk:2048 + 256 * blk + 256], 0)])
                for pp in range(2):
                    pair = 2 * blk + pp
                    qk_pairs([(wname, wt, 128 * pp, gq, QA, ['QA0', 'QA1'])])
                    kv = pair // 2
                    for hs in range(2):
                        hq = 2 * pair + hs
                        load_alibi(hs, hq + 1)
                        lf = lambda jj, kv=kv: VA[:, jj, kv * 128:(kv + 1) * 128]
                        segs = []
                        for g in range(4):
                            segs.append(dict(qslot=hs, kslot=kv, g=g, plan=SWA[g], lhsT_fns=[lf], bias_fn=None,
                                             qreads=['QA%d' % hs, 'QAm%d' % hs, 'QAa%d' % hs], kreads=['KA%d' % kv, 'KAaug%d' % kv], vreads=['VA'],
                                             done=(lambda Os, g=g, p=4 + pair, hs=hs, hq=hq: finalize_std(Os[0], p, hs, g, extra_den=esink[64:128, j, hq:hq + 1]))))
                        attn_seq(segs)
            out_proj(w_out_cd[j])

        def out_proj(Wo):
            for blk in range(4):
                wname, wt = wload([(Wo[:, 256 * blk:256 * blk + 256], 0)])
                for i in range(NT):
                    Mn, Mt = M5ring.next()
                    for k in range(8):
                        P.op('pe', lambda e, k=k, i=i, Mt=Mt, wt=wt: e.matmul(Mt[:, 0:256], yT[:, k, i * 128:(i + 1) * 128], wt[:, k, :], start=(k == 0), stop=(k == 7)),
                             reads=[wname] + ['yT%d' % k for k in range(8)] if k == 0 else [wname], writes=[Mn])
                    xs = x_sb[:, i, 256 * blk:256 * blk + 256]
                    P.op('dve', lambda e, xs=xs, Mt=Mt: e.tensor_tensor(xs, xs, Mt[:, 0:256], ALU.add), reads=[Mn, 'x%d' % i], writes=['x%d' % i])

        for s in range(nseq):
            for i in range(NT):
                P.dma('sp', x_sb[:, i, :], xin[s, i * 128:(i + 1) * 128, :], 'xld%d' % i, reads=[], writes=['x%d' % i])
            for l in layers:
                if l % 2 == 0:
                    layer_ab(l)
                else:
                    layer_cd(l)
            for i in range(NT):
                P.dma('sp', yout[s, i * 128:(i + 1) * 128, :], x_sb[:, i, :], 'xst%d' % i, reads=['x%d' % i], writes=['out%d' % i])
        P.wait_all('sp', ['out%d' % i for i in range(NT)])
        P.emit()
    return nc


def make_consts():
    c = {}
    c["c_ident"] = np.eye(128, dtype=np.float32)
    bdm = np.zeros((128, 128), np.float32)
    bdm[0:64, 0:64] = 1.0 / 64
    bdm[64:128, 64:128] = 1.0 / 64
    c["c_bd"] = bdm
    c["c_a128"] = np.full((128, 128), 1.0 / 128, np.float32)
    s = np.arange(128)[:, None]
    t = np.arange(128)[None, :]
    c["c_tric"] = np.where(s <= t, 0.0, NEG).astype(np.float32)
    c["c_trip"] = np.where(s > t, 0.0, NEG).astype(np.float32)
    c["c_triu"] = (s <= t).astype(np.float32)
    pos = np.arange(S)
    kaug = np.zeros((12, S), np.float32)
    for n in range(8):
        kaug[n] = (pos // 256 == n)
    kaug[8] = 128 * (pos // 128)
    kaug[9] = pos % 128
    kaug[10] = 1.0
    kaug[11] = 1.0
    c["c_kaug"] = kaug
    qal = np.zeros((9, 4, S), np.float32)
    for i in range(1, 9):
        sl = 2.0 ** (-i)
        qal[i, 0] = sl
        qal[i, 1] = sl
        qal[i, 2] = -sl * 128 * (pos // 128)
        qal[i, 3] = -sl * (pos % 128)
    c["c_qalibi"] = qal
    cm = np.zeros((128, 16, 8), np.float32)
    for i in range(16):
        qb = i // 2
        for n in range(8):
            cm[:, i, n] = 0.0 if n < qb else (1e30 if n == qb else -1e30)
    c["c_cmask"] = cm.reshape(128, 128)
    return c


PARAMS = ["norm_gain", "w_in_ab", "b_forget", "moba_q_gain", "moba_k_gain", "fox_q_gain", "fox_k_gain", "w_out_ab", "w_in_cd",
          "diff_q_gain", "diff_k_gain", "diff_lambda", "diff_subln_gain", "swa_q_gain", "swa_k_gain", "swa_sinks", "w_out_cd"]


LAUNCH_GROUPS = [(0, 1, 2, 3)]


def kernel(**inputs):
    x = np.ascontiguousarray(np.asarray(inputs["x"], dtype=np.float32))
    n = 8
    per = x.shape[0] // n
    consts = make_consts()
    base = {k: np.ascontiguousarray(np.asarray(inputs[k], dtype=np.float32)) for k in PARAMS}
    base.update(consts)
    cur = x
    for grp in LAUNCH_GROUPS:
        nc = build(nseq=per, layers=grp)
        in_maps = []
        for c in range(n):
            m = dict(base)
            m["xin"] = np.ascontiguousarray(cur[c * per:(c + 1) * per])
            in_maps.append(m)
        res = run_bass_kernel_spmd(nc, in_maps, core_ids=list(range(n)))
        cur = np.concatenate([r["yout"] for r in res.results], axis=0)
    return cur
```
